# Optimizing a Trainium2 kernel written in Bass

```python
import math
import jax, jax.numpy as jnp
from jax import lax
import numpy as np

D_MODEL = 2048
BATCH = 4
SEQ = 4096
DEPTH = 1

CHUNK = 64
N_META = 16
Q_BLOCK = 128
ROPE_THETA = 10000.0
LN_EPS = 1e-5
ATT_HALF_DIM = 64
N_ATT_HEADS = D_MODEL // (4 * ATT_HALF_DIM)
ATT_QK_WIDTH = N_ATT_HEADS * 2 * ATT_HALF_DIM
ATT_V_WIDTH = N_ATT_HEADS * 2 * ATT_HALF_DIM
CONV_DIM = D_MODEL // 2
CONV_WIDTH = 3
IN_COLS = 2 * ATT_QK_WIDTH + ATT_V_WIDTH + 3 * CONV_DIM + 2 * D_MODEL
N_EXPERTS = 64
TOP_K = 8
N_GROUPS = 8
TOPK_GROUPS = 4
D_EXPERT = D_MODEL // 4
ROUTED_SCALE = 2.5
EXPERT_BLOCK = 128
DEEPNORM_ALPHA = (2 * DEPTH) ** 0.25
DEEPNORM_BETA = (8 * DEPTH) ** -0.25

kernel_name = "chunk_causal_diffattn_shortconv_moe_hybrid"


def _layer_norm(x, g, b):
    xf = x.astype(jnp.float32)
    mu = jnp.mean(xf, axis=-1, keepdims=True)
    var = jnp.mean(jnp.square(xf - mu), axis=-1, keepdims=True)
    return ((xf - mu) * lax.rsqrt(var + LN_EPS) * g.astype(jnp.float32) + b.astype(jnp.float32)).astype(x.dtype)


def _rms_norm(x, g):
    xf = x.astype(jnp.float32)
    return (xf * lax.rsqrt(jnp.mean(xf * xf, axis=-1, keepdims=True) + LN_EPS) * g.astype(jnp.float32)).astype(x.dtype)


def _rope(x, pos):
    half = x.shape[-1] // 2
    inv_freq = ROPE_THETA ** (-jnp.arange(half, dtype=jnp.float32) / half)
    ang = pos.astype(jnp.float32)[:, None] * inv_freq[None, :]
    cos = jnp.cos(ang)[None, :, None, None, :]
    sin = jnp.sin(ang)[None, :, None, None, :]
    x1 = x[..., :half].astype(jnp.float32)
    x2 = x[..., half:].astype(jnp.float32)
    return jnp.concatenate([x1 * cos - x2 * sin, x2 * cos + x1 * sin], axis=-1).astype(x.dtype)


def _diff_attn_block(q, k, v, q_cid, k_cid, lam):
    s = jnp.einsum('bqhmd,bkhmd->bhmqk', q, k, preferred_element_type=jnp.float32) * (ATT_HALF_DIM ** -0.5)
    visible = k_cid[None, :] <= q_cid[:, None]
    p = jax.nn.softmax(jnp.where(visible, s, -jnp.inf), axis=-1)
    a = p[:, :, 0] - lam * p[:, :, 1]
    return jnp.einsum('bhqk,bkhe->bqhe', a.astype(v.dtype), v)


def _mixer(h, q0, lam_init, w_in, lambda_q1, lambda_k1, lambda_q2, lambda_k2, subln_g,
           w_conv, w_proj_attn, w_proj_conv, w_out):
    B, L, _ = h.shape
    u = h @ w_in
    splits = [int(c) for c in np.cumsum([ATT_QK_WIDTH, ATT_QK_WIDTH, ATT_V_WIDTH,
                                          CONV_DIM, CONV_DIM, CONV_DIM, D_MODEL])]
    q, k, v, cx, cc, cb, ga, gc = jnp.split(u, splits, axis=-1)

    pos = jnp.arange(L, dtype=jnp.int32)
    cid = jnp.where(pos < N_META, -1, (pos - N_META) // CHUNK)

    q = _rope(q.reshape(B, L, N_ATT_HEADS, 2, ATT_HALF_DIM), pos)
    k = _rope(k.reshape(B, L, N_ATT_HEADS, 2, ATT_HALF_DIM), pos)
    v = v.reshape(B, L, N_ATT_HEADS, 2 * ATT_HALF_DIM)
    f32 = jnp.float32
    lam = (jnp.exp(jnp.sum(lambda_q1.astype(f32) * lambda_k1.astype(f32)))
           - jnp.exp(jnp.sum(lambda_q2.astype(f32) * lambda_k2.astype(f32))) + lam_init)
    blocks = [(0, N_META)] if q0 == 0 else []
    blocks = blocks + [(s, s + Q_BLOCK) for s in range(N_META, L, Q_BLOCK)]
    outs = [_diff_attn_block(q[:, s:e], k[:, :e], v[:, :e], cid[s:e], cid[:e], lam) for s, e in blocks]
    o = jnp.concatenate(outs, axis=1)
    o = (_rms_norm(o, subln_g) * (1.0 - lam_init)).reshape(B, L - q0, ATT_V_WIDTH)

    z = cc * cx
    y = lax.conv_general_dilated(z, w_conv[:, None, :].astype(z.dtype), window_strides=(1,),
                                 padding=[(CONV_WIDTH - 1, 0)],
                                 dimension_numbers=('NWC', 'WIO', 'NWC'),
                                 feature_group_count=CONV_DIM)
    y = cb[:, q0:] * y[:, q0:]

    merged = (jax.nn.sigmoid(ga[:, q0:]) * (o @ w_proj_attn)
              + jax.nn.sigmoid(gc[:, q0:]) * (y @ w_proj_conv))
    return merged @ w_out


def _routed_experts(xf, eidx, ew, w_exp_gate, w_exp_up, w_exp_down):
    N, D = xf.shape
    A = N * TOP_K
    flat_e = eidx.reshape(A)
    flat_tok = jnp.arange(A, dtype=jnp.int32) // TOP_K
    flat_w = ew.reshape(A)
    order = jnp.argsort(flat_e)
    e_sorted = flat_e[order]
    counts = jnp.bincount(flat_e, length=N_EXPERTS)
    start = jnp.cumsum(counts) - counts
    padded = (counts + EXPERT_BLOCK - 1) // EXPERT_BLOCK * EXPERT_BLOCK
    pad_end = jnp.cumsum(padded)
    pad_start = pad_end - padded
    dest = pad_start[e_sorted] + jnp.arange(A, dtype=jnp.int32) - start[e_sorted]
    n_blocks = -(-A // EXPERT_BLOCK) + N_EXPERTS
    P = n_blocks * EXPERT_BLOCK
    row_tok = jnp.full((P,), N, jnp.int32).at[dest].set(flat_tok[order])
    row_w = jnp.zeros((P,), xf.dtype).at[dest].set(flat_w[order].astype(xf.dtype))
    block_e = jnp.minimum(jnp.searchsorted(pad_end, jnp.arange(n_blocks, dtype=jnp.int32) * EXPERT_BLOCK,
                                           side='right'), N_EXPERTS - 1)
    x_pad = jnp.concatenate([xf, jnp.zeros((1, D), xf.dtype)], axis=0)

    def block_fn(args):
        e, toks, wts = args
        xb = x_pad[toks]
        hb = jax.nn.silu(xb @ w_exp_gate[e]) * (xb @ w_exp_up[e])
        return (hb @ w_exp_down[e]) * wts[:, None]

    yb = lax.map(block_fn, (block_e, row_tok.reshape(n_blocks, EXPERT_BLOCK),
                            row_w.reshape(n_blocks, EXPERT_BLOCK)))
    return jax.ops.segment_sum(yb.reshape(P, D), row_tok, num_segments=N + 1)[:N]


def _moe(h, w_router, router_bias, w_exp_gate, w_exp_up, w_exp_down, w_sh_gate, w_sh_up, w_sh_down):
    B, T, D = h.shape
    xf = h.reshape(B * T, D)
    N = B * T
    logits = xf.astype(jnp.float32) @ w_router.astype(jnp.float32)
    s = jax.nn.sigmoid(logits)
    sel = s + router_bias.astype(jnp.float32)
    per_group = N_EXPERTS // N_GROUPS
    gscore = lax.top_k(sel.reshape(N, N_GROUPS, per_group), 2)[0].sum(-1)
    _, gidx = lax.top_k(gscore, TOPK_GROUPS)
    gmask = jax.nn.one_hot(gidx, N_GROUPS, dtype=jnp.float32).sum(-2) > 0
    sel = jnp.where(jnp.repeat(gmask, per_group, axis=-1), sel, -jnp.inf)
    _, eidx = lax.top_k(sel, TOP_K)
    ew = jnp.take_along_axis(s, eidx, axis=-1)
    ew = ew / jnp.sum(ew, axis=-1, keepdims=True) * ROUTED_SCALE
    shared = (jax.nn.silu(xf @ w_sh_gate) * (xf @ w_sh_up)) @ w_sh_down
    routed = _routed_experts(xf, eidx, ew, w_exp_gate, w_exp_up, w_exp_down)
    return (shared + routed).reshape(B, T, D)


def setup_inputs(seed: int = 0) -> dict:
    key = jax.random.key(seed)
    ks = jax.random.split(key, 32)

    def nrm(k, shape, scale):
        return jax.random.normal(k, shape, jnp.float32) * scale

    D, L_ = D_MODEL, DEPTH
    beta = DEEPNORM_BETA
    col_scale = jnp.concatenate([
        jnp.ones((2 * ATT_QK_WIDTH,), jnp.float32),
        jnp.full((ATT_V_WIDTH + CONV_DIM,), beta, jnp.float32),
        jnp.ones((2 * CONV_DIM + 2 * D_MODEL,), jnp.float32)])
    return {
        "x": nrm(ks[0], (BATCH, SEQ, D), 1.0),
        "meta_tokens": nrm(ks[1], (N_META, D), 1.0),
        "ln0_g": 1.0 + nrm(ks[2], (D,), 0.02),
        "ln0_b": nrm(ks[3], (D,), 0.02),
        "w_in": nrm(ks[4], (L_, D, IN_COLS), D ** -0.5) * col_scale,
        "lambda_q1": nrm(ks[5], (L_, ATT_HALF_DIM), 0.1),
        "lambda_k1": nrm(ks[6], (L_, ATT_HALF_DIM), 0.1),
        "lambda_q2": nrm(ks[7], (L_, ATT_HALF_DIM), 0.1),
        "lambda_k2": nrm(ks[8], (L_, ATT_HALF_DIM), 0.1),
        "subln_g": 1.0 + nrm(ks[9], (L_, 2 * ATT_HALF_DIM), 0.02),
        "w_conv": nrm(ks[10], (L_, CONV_WIDTH, CONV_DIM), CONV_WIDTH ** -0.5),
        "w_proj_attn": nrm(ks[11], (L_, ATT_V_WIDTH, D), ATT_V_WIDTH ** -0.5 * beta),
        "w_proj_conv": nrm(ks[12], (L_, CONV_DIM, D), CONV_DIM ** -0.5 * beta),
        "w_out": nrm(ks[13], (L_, D, D), D ** -0.5 * beta),
        "ln1_g": 1.0 + nrm(ks[14], (L_, D), 0.02),
        "ln1_b": nrm(ks[15], (L_, D), 0.02),
        "w_router": nrm(ks[16], (L_, D, N_EXPERTS), D ** -0.5),
        "router_bias": nrm(ks[17], (L_, N_EXPERTS), 0.01),
        "w_exp_gate": nrm(ks[18], (L_, N_EXPERTS, D, D_EXPERT), D ** -0.5 * beta),
        "w_exp_up": nrm(ks[19], (L_, N_EXPERTS, D, D_EXPERT), D ** -0.5 * beta),
        "w_exp_down": nrm(ks[20], (L_, N_EXPERTS, D_EXPERT, D), D_EXPERT ** -0.5 * beta),
        "w_sh_gate": nrm(ks[21], (L_, D, D_EXPERT), D ** -0.5 * beta),
        "w_sh_up": nrm(ks[22], (L_, D, D_EXPERT), D ** -0.5 * beta),
        "w_sh_down": nrm(ks[23], (L_, D_EXPERT, D), D_EXPERT ** -0.5 * beta),
        "ln2_g": 1.0 + nrm(ks[24], (L_, D), 0.02),
        "ln2_b": nrm(ks[25], (L_, D), 0.02),
    }


def reference(x, meta_tokens, ln0_g, ln0_b, w_in, lambda_q1, lambda_k1, lambda_q2, lambda_k2,
              subln_g, w_conv, w_proj_attn, w_proj_conv, w_out, ln1_g, ln1_b, w_router,
              router_bias, w_exp_gate, w_exp_up, w_exp_down, w_sh_gate, w_sh_up, w_sh_down,
              ln2_g, ln2_b):
    B = x.shape[0]
    meta = jnp.broadcast_to(meta_tokens[None].astype(x.dtype), (B, N_META, D_MODEL))
    h = _layer_norm(jnp.concatenate([meta, x], axis=1), ln0_g, ln0_b)
    for l in range(DEPTH):
        q0 = N_META if l == DEPTH - 1 else 0
        lam_init = 0.8 - 0.6 * math.exp(-0.3 * l)
        m = _mixer(h, q0, lam_init, w_in[l], lambda_q1[l], lambda_k1[l], lambda_q2[l], lambda_k2[l],
                   subln_g[l], w_conv[l], w_proj_attn[l], w_proj_conv[l], w_out[l])
        h1 = _layer_norm(DEEPNORM_ALPHA * h[:, q0:] + m, ln1_g[l], ln1_b[l])
        f = _moe(h1, w_router[l], router_bias[l], w_exp_gate[l], w_exp_up[l], w_exp_down[l],
                 w_sh_gate[l], w_sh_up[l], w_sh_down[l])
        h = _layer_norm(DEEPNORM_ALPHA * h1 + f, ln2_g[l], ln2_b[l])
    return h
```

```python
import numpy as np
import ml_dtypes
import concourse.bass as bass
import concourse.mybir as mybir
from concourse.bass_utils import run_bass_kernel_spmd

F32 = mybir.dt.float32
BF16 = mybir.dt.bfloat16
AF = mybir.ActivationFunctionType
ALU = mybir.AluOpType
AX = mybir.AxisListType

D = 2048
SEQ = 4096
NMETA = 16
L = SEQ + NMETA
NH = 8
NE = 64
DE = 512
EPS = 1e-5
ALPHA = 2.0 ** 0.25
LAM_INIT = 0.2
OFFS = ((0, 3, 4, 7), (1, 2, 5, 6))


class _Op:
    __slots__ = ("eng", "fn", "deps", "is_dma", "signal", "sem", "val", "idx", "pos")


class Sched:
    ENGS = ("pe", "act", "dve", "pool", "sp")
    RING = 12

    def __init__(self):
        self.ops = {e: [] for e in self.ENGS}
        self.last_w = {}
        self.readers = {}
        self.ndma = {"sp": 0, "pool": 0}
        self.dma_ops = {"sp": [], "pool": []}
        self.pending_barrier = {}
        self.n = 0

    def _add(self, eng, fn, r, w, is_dma, s=()):
        op = _Op()
        op.eng, op.fn, op.is_dma, op.signal, op.sem, op.val = eng, fn, is_dma, False, None, 0
        op.idx = self.n
        op.pos = len(self.ops[eng])
        self.n += 1
        deps = set()
        hard = set()
        for k in tuple(r) + tuple(s):
            lw = self.last_w.get(k)
            if lw is not None:
                deps.add(lw)
                if (not lw.is_dma) and (not is_dma) and lw.eng == eng and eng != "pe" and op.pos - lw.pos <= 2:
                    hard.add(lw)
        for k in w:
            lw = self.last_w.get(k)
            if lw is not None:
                deps.add(lw)
            rd = self.readers.get(k)
            if rd:
                deps.update(rd.values())
        r = tuple(r) + tuple(s)
        if eng in self.pending_barrier:
            deps.update(self.pending_barrier.pop(eng))
        if is_dma:
            n = self.ndma[eng]
            self.ndma[eng] = n + 1
            op.sem = (eng, n % self.RING)
            op.val = 16 * (n // self.RING + 1)
            if n >= self.RING:
                deps.add(self.dma_ops[eng][n - self.RING])
            self.dma_ops[eng].append(op)
        for k in r:
            d = self.readers.setdefault(k, {})
            d[("dma", op.idx) if is_dma else eng] = op
        for k in w:
            self.last_w[k] = op
            self.readers[k] = {}
        op.deps = [d for d in deps if (d.is_dma or is_dma or d.eng != eng)] + [d for d in hard if not (d.is_dma or is_dma or d.eng != eng)]
        for d in op.deps:
            if not d.is_dma:
                d.signal = True
        self.ops[eng].append(op)
        return op

    def pe(self, fn, r=(), w=(), s=()):
        return self._add("pe", fn, r, w, False, s)

    def act(self, fn, r=(), w=(), s=()):
        return self._add("act", fn, r, w, False, s)

    def dve(self, fn, r=(), w=(), s=()):
        return self._add("dve", fn, r, w, False, s)

    def pool(self, fn, r=(), w=(), s=()):
        return self._add("pool", fn, r, w, False, s)

    def dma(self, q, fn, r=(), w=()):
        return self._add(q, fn, r, w, True)

    def barrier(self):
        deps = []
        for e in self.ENGS:
            if self.ops[e]:
                deps.append(self.ops[e][-1])
        for q in ("sp", "pool"):
            deps.extend(self.dma_ops[q][-self.RING:])
        for e in self.ENGS:
            self.pending_barrier[e] = list(deps)

    def emit(self, nc, sems):
        for e in ("pe", "act", "dve", "pool"):
            c = 0
            for op in self.ops[e]:
                if op.is_dma:
                    continue
                if op.signal:
                    c += 1
                    op.sem = e
                    op.val = c
        final = {}
        for q in ("sp", "pool"):
            for op in self.dma_ops[q]:
                final[op.sem] = max(final.get(op.sem, 0), op.val)

        def run(eng_name, e):
            waited = {}
            for op in self.ops[eng_name]:
                need = {}
                for d in op.deps:
                    if d.val > need.get(d.sem, 0):
                        need[d.sem] = d.val
                for s, v in need.items():
                    if waited.get(s, 0) < v:
                        e.wait_ge(sems[s], v)
                        waited[s] = v
                ins = op.fn(e)
                if op.is_dma:
                    ins.then_inc(sems[op.sem], 16)
                elif op.signal:
                    ins.then_inc(sems[op.sem], 1)
            if eng_name == "sp":
                for s, v in final.items():
                    e.wait_ge(sems[s], v)

        with nc.Block() as block:
            @block.tensor
            def _(e):
                run("pe", e)

            @block.scalar
            def _(e):
                run("act", e)

            @block.vector
            def _(e):
                run("dve", e)

            @block.gpsimd
            def _(e):
                run("pool", e)

            @block.sync
            def _(e):
                run("sp", e)


class Arena:
    def __init__(self, t, nwords):
        self.t, self.n, self.off = t, nwords, 0

    def reset(self):
        self.off = 0

    def f32(self, n):
        assert self.off + n <= self.n, ("arena overflow", self.off, n, self.n)
        ap = self.t[:, self.off:self.off + n]
        self.off += n
        return ap

    def bf16(self, n):
        w = (n + 1) // 2
        assert self.off + w <= self.n, ("arena overflow", self.off, w, self.n)
        ap = self.t[:, self.off:self.off + w].bitcast(BF16)
        self.off += w
        return ap


ARENA_WORDS = 51800
NEXP = 65
SUB6 = 3
SUB6B = 7
STAGE = 99
DEBUG = False
NCHK = 9
PASSES = (0, 1)


def build_program():
    nc = bass.Bass("TRN2", target_bir_lowering=False)
    S = Sched()

    def dram(name, shape, dt, kind="ExternalInput"):
        if kind == "ExternalInput":
            if STAGE < 7 and name.startswith(("w_e", "w_s")):
                return None
            _DECL.append(name)
        return nc.dram_tensor(name, list(shape), dt, kind=kind).ap()

    scratch_kind = "ExternalOutput" if DEBUG else "Internal"
    xs = dram("xs", [SEQ, D], F32)
    meta = dram("meta", [NMETA, D], F32)
    w_in = dram("w_in", [D, 10240], F32)
    w_qkp = dram("w_qkp", [D, 2048], F32)
    par = dram("par", [128, 64], F32)
    ident_d = dram("ident", [128, 128], F32)
    cosk = dram("cosk", [128, L], F32)
    sink = dram("sink", [128, L], F32)
    xo = dram("xo", [2048, D], F32)
    xh = dram("xh", [32, D], F32)
    cosq = dram("cosq", [128, 2048], F32)
    sinq = dram("sinq", [128, 2048], F32)
    maskd = dram("maskd", [128, 8, 512], BF16)
    lamv = dram("lamv", [128, 256], F32)
    w_pa = dram("w_pa", [1024, D], F32)
    w_pc = dram("w_pc", [1024, D], F32)
    w_out = dram("w_out", [D, D], F32)
    w_router = dram("w_router", [D, NE], F32)
    rbd = dram("rbd", [128, NE], F32)
    bcd = dram("bcd", [6, 128, D], F32)
    w_eg = dram("w_eg", [NE, D, DE], F32)
    w_eu = dram("w_eu", [NE, D, DE], F32)
    w_ed = dram("w_ed", [NE, DE, D], F32)
    w_sg = dram("w_sg", [D, DE], F32)
    w_su = dram("w_su", [D, DE], F32)
    w_sd = dram("w_sd", [DE, D], F32)
    KT_d = dram("KT_d", [NH, 128, L], BF16, scratch_kind)
    V_d = dram("V_d", [L, 1024], BF16, scratch_kind)
    hTo_d = dram("hTo_d", [16, 128, 2048], BF16, scratch_kind)
    hTh_d = dram("hTh_d", [16, 128, 32], BF16, scratch_kind)
    QT_d = dram("QT_d", [NH, 128, 2048], BF16, scratch_kind)
    yT_d = dram("yT_d", [8, 128, 2048], BF16, scratch_kind)
    OnT_d = dram("OnT_d", [NH, 128, 2048], BF16, scratch_kind)
    mT_d = dram("mT_d", [16, 128, 2048], BF16, scratch_kind)
    h1_d = dram("h1_d", [2048, D], F32, scratch_kind)
    h1T_d = dram("h1T_d", [16, 128, 2048], BF16, scratch_kind)
    ew_d = dram("ew_d", [128, 16 * 65], F32, scratch_kind)
    out = dram("out", [2048, D], F32, "ExternalOutput")

    sem_names = ["pe", "act", "dve", "pool"] + [(q, i) for q in ("sp", "pool") for i in range(Sched.RING)]

    import contextlib
    with contextlib.ExitStack() as es:
        arena_t = es.enter_context(nc.sbuf_tensor("arena", [128, ARENA_WORDS], F32))
        A = Arena(arena_t, ARENA_WORDS)
        cst = es.enter_context(nc.sbuf_tensor("cst", [128, 64 + 128], F32))
        idb_t = es.enter_context(nc.sbuf_tensor("idb", [128, 128], BF16))
        ewt_t = es.enter_context(nc.sbuf_tensor("ewt", [128, 16 * 65], F32))
        ewt = ewt_t[:]
        banks = [es.enter_context(nc.psum_tensor(f"ps{i}", [128, 512], F32)) for i in range(8)]
        sems = {}
        for sn in sem_names:
            nm = sn if isinstance(sn, str) else f"{sn[0]}{sn[1]}"
            sems[sn] = es.enter_context(nc.semaphore("s_" + nm))

        parv = cst[:, 0:64]
        idf = cst[:, 64:192]
        idb = idb_t[:]
        G0T, B0T = 0, 16

        S.dma("sp", lambda e: e.dma_start(out=parv, in_=par[:, :]), w=["par"])
        S.dma("sp", lambda e: e.dma_start(out=idf, in_=ident_d[:, :]), w=["idf"])
        S.dve(lambda e: e.tensor_copy(out=idb, in_=idf), r=["idf"], w=["idb"])

        def ln_tile(src_ap, R, xr, xn, st, mv, rstd, tpb, hT_dst, keys, c0):
            S.dma("sp", lambda e: e.dma_start(out=xr[:R, :], in_=src_ap), w=[keys["xr"]])

            def stats(e):
                for i in range(4):
                    ins = e.bn_stats(out=st[:R, i, :], in_=xr[:R, i * 512:(i + 1) * 512])
                return ins
            S.dve(stats, r=[keys["xr"]], w=[keys["st"]])
            S.dve(lambda e: e.bn_aggr(out=mv[:R, :], in_=st[:R, :, :]), r=[keys["st"]], w=[keys["mv"]])
            S.act(lambda e: e.activation(out=rstd[:R, :], in_=mv[:R, 1:2], func=AF.Sqrt, bias=EPS, scale=1.0),
                  r=[keys["mv"]], w=[keys["rstd"]])

            S.dve(lambda e: e.reciprocal(out=rstd[:R, :], in_=rstd[:R, :]), r=[keys["rstd"]], w=[keys["rstd"]])
            S.dve(lambda e: e.tensor_scalar(out=xn[:R, :], in0=xr[:R, :], scalar1=mv[:R, 0:1], scalar2=rstd[:R, 0:1],
                                            op0=ALU.subtract, op1=ALU.mult),
                  r=[keys["xr"]], s=[keys["rstd"], keys["mv"]], w=[keys["xn"]])

            def tr(e):
                for kt in range(16):
                    bk = tpb[kt // 8]
                    ins = e.transpose(out=bk[:, (kt % 8) * 128:(kt % 8) * 128 + R], in_=xn[:R, kt * 128:(kt + 1) * 128],
                                      identity=idb[:R, :R])
                return ins
            S.pe(tr, r=[keys["xn"], "idb"], w=[keys["tp"]])

            def ev(e):
                for kt in range(16):
                    bk = tpb[kt // 8]
                    ins = e.activation(out=hT_dst(kt), in_=bk[:, (kt % 8) * 128:(kt % 8) * 128 + R], func=AF.Identity,
                                       scale=parv[:, G0T + kt:G0T + kt + 1], bias=parv[:, B0T + kt:B0T + kt + 1])
                return ins
            S.act(ev, r=[keys["tp"], "par"], w=[keys["hT"]])

        bank_bf = [b[:].bitcast(BF16) for b in banks]
        bank_f = [b[:] for b in banks]


        def V3(ap, a):
            return ap.rearrange("p (a b) -> p a b", a=a)

        def qk_pass(pfx, chunk_list, wcol0, cos_d, sin_d, out_d, hT_store=None, vmode=False):
            A.reset()
            xr_s = [A.f32(D) for _ in range(2)]
            xn_s = [A.bf16(D) for _ in range(2)]
            hT_s = [A.bf16(16 * 512).rearrange("p (k n) -> p k n", n=512) for _ in range(2)]
            st = A.f32(24).rearrange("p (a b) -> p a b", b=6)
            mv = A.f32(2)
            rstd = A.f32(1)
            W = A.bf16(16 * 2048).rearrange("p (k n) -> p k n", n=2048)
            cs_s = [(A.f32(512), A.f32(512)) for _ in range(2)]
            t_s = [(A.f32(512), A.f32(512)) for _ in range(2)]
            ko_s = [A.bf16(512) for _ in range(2)]
            vo_s = [A.bf16(1024) for _ in range(2)]
            S.barrier()
            if out_d is not None:
                S.dma("pool", lambda e: e.dma_start(out=W[:, :, 0:1024],
                                                    in_=w_in[:, wcol0:wcol0 + 1024].rearrange("(k p) n -> p k n", p=128)), w=[pfx + "W0"])
            if not vmode and out_d is not None:
                qc = wcol0
                S.dma("pool", lambda e: e.dma_start(out=W[:, :, 1024:2048],
                                                    in_=w_qkp[:, qc:qc + 1024].rearrange("(k p) n -> p k n", p=128)), w=[pfx + "W1"])
            tile_ctr = 0
            for ci, (col0, ntok, tiles) in enumerate(chunk_list):
                hT = hT_s[ci % 2]
                hk = f"{pfx}hT{ci % 2}"
                for tl, (src, R) in enumerate(tiles):
                    sl = tile_ctr % 2
                    keys = dict(xr=f"{pfx}xr{sl}", xn=f"{pfx}xn{sl}", mv=pfx + "mv", rstd=pfx + "rstd", st=pfx + "st",
                                tp=f"{pfx}tp{sl}", hT=hk)
                    tpb = (bank_bf[2 * sl], bank_bf[2 * sl + 1])
                    ln_tile(src, R, xr_s[sl], xn_s[sl], st, mv, rstd, tpb,
                            (lambda kt, hT=hT, tl=tl, R=R: hT[:, kt, tl * 128: tl * 128 + R]), keys, 0)
                    tile_ctr += 1
                N = ntok
                if hT_store is not None:
                    hT_store(hT, hk, col0, N)
                if out_d is None:
                    continue
                if not vmode:
                    cs = cs_s[ci % 2]
                    ck = f"{pfx}cs{ci % 2}"
                    S.dma("sp", lambda e, cs=cs, col0=col0, N=N: e.dma_start(out=cs[0][:, :N], in_=cos_d[:, col0:col0 + N]), w=[ck + "c"])
                    S.dma("sp", lambda e, cs=cs, col0=col0, N=N: e.dma_start(out=cs[1][:, :N], in_=sin_d[:, col0:col0 + N]), w=[ck + "s"])
                    for h in range(NH):
                        par2 = h % 2
                        pk, pkp = bank_f[4 + 2 * par2], bank_f[5 + 2 * par2]

                        def mm(e, h=h, hT=hT, N=N, pk=pk, pkp=pkp):
                            for v, pb in ((0, pk), (1, pkp)):
                                for kt in range(16):
                                    ins = e.matmul(pb[:, :N], lhsT=W[:, kt, v * 1024 + h * 128: v * 1024 + (h + 1) * 128],
                                                   rhs=hT[:, kt, :N], start=(kt == 0), stop=(kt == 15))
                            return ins
                        S.pe(mm, r=[hk, pfx + "W0", pfx + "W1"], w=[f"{pfx}pk{par2}"])
                        t1, t2 = t_s[par2]
                        ko = ko_s[par2]

                        def rope(e, pk=pk, pkp=pkp, cs=cs, N=N, t1=t1, t2=t2):
                            e.tensor_tensor(out=t1[:, :N], in0=pk[:, :N], in1=cs[0][:, :N], op=ALU.mult)
                            return e.tensor_tensor(out=t2[:, :N], in0=pkp[:, :N], in1=cs[1][:, :N], op=ALU.mult)
                        S.dve(rope, r=[f"{pfx}pk{par2}", ck + "c", ck + "s"], w=[f"{pfx}t{par2}"])
                        S.pool(lambda e, t1=t1, t2=t2, ko=ko, N=N: e.tensor_tensor(out=ko[:, :N], in0=t1[:, :N], in1=t2[:, :N], op=ALU.add),
                               r=[f"{pfx}t{par2}"], w=[f"{pfx}ko{par2}"])
                        S.dma("sp", lambda e, ko=ko, h=h, col0=col0, N=N: e.dma_start(out=out_d[h, :, col0:col0 + N], in_=ko[:, :N]),
                              r=[f"{pfx}ko{par2}"], w=[(pfx + "out", h, ci)])
                else:
                    for tl, (src, R) in enumerate(tiles):
                        vo = vo_s[tl % 2]
                        for half in range(2):
                            pb = bank_f[4 + half + 2 * (tl % 2)]

                            def mmv(e, hT=hT, tl=tl, R=R, half=half, pb=pb):
                                for kt in range(16):
                                    ins = e.matmul(pb[:R, :], lhsT=hT[:, kt, tl * 128: tl * 128 + R],
                                                   rhs=W[:, kt, half * 512:(half + 1) * 512], start=(kt == 0), stop=(kt == 15))
                                return ins
                            S.pe(mmv, r=[hk, pfx + "W0"], w=[f"{pfx}pv{half}{tl % 2}"])
                            S.act(lambda e, vo=vo, pb=pb, R=R, half=half: e.activation(out=vo[:R, half * 512:(half + 1) * 512], in_=pb[:R, :], func=AF.Copy),
                                  r=[f"{pfx}pv{half}{tl % 2}"], w=[f"{pfx}vo{tl % 2}{half}"])
                        p0 = col0 + tl * 128
                        S.dma("sp", lambda e, vo=vo, p0=p0, R=R: e.dma_start(out=out_d[p0:p0 + R, :], in_=vo[:R, :]),
                              r=[f"{pfx}vo{tl % 2}0", f"{pfx}vo{tl % 2}1"], w=[(pfx + "out", p0)])

        seq_chunks = [(0, NMETA, [(meta[:, :], NMETA)])] + \
                     [(NMETA + 512 * i, 512, [(xs[i * 512 + t * 128: i * 512 + (t + 1) * 128, :], 128) for t in range(4)]) for i in range(8)]
        own_chunks = [(512 * g, 512, [(xo[g * 512 + t * 128: g * 512 + (t + 1) * 128, :], 128) for t in range(4)]) for g in range(4)]
        halo_chunk = [(0, 32, [(xh[:, :], 32)])]

        if STAGE >= 1:
            qk_pass("A0", seq_chunks, 1024, cosk, sink, KT_d)
            qk_pass("A1", seq_chunks, 2048, None, None, V_d, vmode=True)

        def store_hTo(hT, hk, col0, N):
            S.dma("sp", lambda e: e.dma_start(out=hTo_d[:, :, col0:col0 + N].rearrange("k p n -> p k n"), in_=hT[:, :, :N]),
                  r=[hk], w=[("hTo_d", col0)])

        def store_hTh(hT, hk, col0, N):
            S.dma("sp", lambda e: e.dma_start(out=hTh_d[:, :, 0:32].rearrange("k p n -> p k n"), in_=hT[:, :, :32]),
                  r=[hk], w=["hTh_d"])

        if STAGE >= 2:
            qk_pass("B0", own_chunks, 0, cosq, sinq, QT_d, hT_store=store_hTo)
            qk_pass("Bh", halo_chunk, 0, None, None, None, hT_store=store_hTh)

        if STAGE >= 3:
            A.reset()
            Wc = A.bf16(16 * 3072).rearrange("p (k n) -> p k n", n=3072)
            hT_s = [A.bf16(16 * 512).rearrange("p (k n) -> p k n", n=512) for _ in range(2)]
            hTh = A.bf16(16 * 32).rearrange("p (k n) -> p k n", n=32)
            cxs = A.f32(512)
            cxhs = A.f32(8)
            zext = A.f32(4 * 130).rearrange("p (a b) -> p a b", b=130)
            c1 = A.f32(512)
            yT_s = [A.bf16(8 * 512).rearrange("p (k n) -> p k n", n=512) for _ in range(2)]
            S.barrier()
            for j in range(3):
                S.dma("pool", lambda e, j=j: e.dma_start(out=Wc[:, :, j * 1024:(j + 1) * 1024],
                                                         in_=w_in[:, 3072 + j * 1024: 4096 + j * 1024].rearrange("(k p) n -> p k n", p=128)),
                      w=[f"Wc{j}"])
            S.dma("sp", lambda e: e.dma_start(out=hTh, in_=hTh_d[:, :, :].rearrange("k p n -> p k n")), r=["hTh_d"], w=["hTh"])
            for g in range(4):
                hT = hT_s[g % 2]
                hk = f"C_hT{g % 2}"
                S.dma("sp", lambda e, hT=hT, g=g: e.dma_start(out=hT, in_=hTo_d[:, :, g * 512:(g + 1) * 512].rearrange("k p n -> p k n")),
                      r=[("hTo_d", g * 512)], w=[hk])
                yT = yT_s[g % 2]
                yk = f"C_yT{g % 2}"
                for ct in range(8):
                    b0 = 4 * (ct % 2)
                    bx, bc_, bb, bh = bank_f[b0], bank_f[b0 + 1], bank_f[b0 + 2], bank_f[b0 + 3]
                    bkk = f"C_bk{ct % 2}"

                    def mmc(e, hT=hT, ct=ct, g=g, bx=bx, bc_=bc_, bb=bb, bh=bh):
                        for j, pb in ((0, bx), (1, bc_), (2, bb)):
                            for kt in range(16):
                                ins = e.matmul(pb[:, :], lhsT=Wc[:, kt, j * 1024 + ct * 128: j * 1024 + (ct + 1) * 128],
                                               rhs=hT[:, kt, :], start=(kt == 0), stop=(kt == 15))
                        for j in (0, 1):
                            for kt in range(16):
                                ins = e.matmul(bh[:, j * 8:(j + 1) * 8], lhsT=Wc[:, kt, j * 1024 + ct * 128: j * 1024 + (ct + 1) * 128],
                                               rhs=hTh[:, kt, g * 8:(g + 1) * 8], start=(kt == 0), stop=(kt == 15))
                        return ins
                    S.pe(mmc, r=[hk, "hTh", "Wc0", "Wc1", "Wc2"], w=[bkk])

                    def cpx(e, bx=bx, bh=bh):
                        e.activation(out=cxs, in_=bx[:, :], func=AF.Copy)
                        return e.activation(out=cxhs, in_=bh[:, 0:8], func=AF.Copy)
                    S.act(cpx, r=[bkk], w=["C_cxs"])

                    def zmk(e, bc_=bc_, bh=bh):
                        e.tensor_tensor(out=zext[:, :, 2:130], in0=V3(bc_[:, :], 4), in1=V3(cxs, 4), op=ALU.mult)
                        return e.tensor_tensor(out=zext[:, :, 0:2], in0=V3(bh[:, 8:16], 4), in1=V3(cxhs, 4), op=ALU.mult)
                    S.dve(zmk, r=[bkk, "C_cxs"], w=["C_z"])
                    wc = 33
                    S.dve(lambda e, ct=ct: e.tensor_scalar(out=V3(c1, 4), in0=zext[:, :, 0:128], scalar1=parv[:, 33 + ct:34 + ct], scalar2=None,
                                                           op0=ALU.mult), r=["C_z", "par"], w=["C_c1"])
                    S.dve(lambda e, ct=ct: e.scalar_tensor_tensor(out=V3(c1, 4), in0=zext[:, :, 1:129], scalar=parv[:, 41 + ct:42 + ct],
                                                                  in1=V3(c1, 4), op0=ALU.mult, op1=ALU.add), r=["C_z", "C_c1", "par"], w=["C_c1"])
                    S.dve(lambda e, ct=ct: e.scalar_tensor_tensor(out=V3(c1, 4), in0=zext[:, :, 2:130], scalar=parv[:, 49 + ct:50 + ct],
                                                                  in1=V3(c1, 4), op0=ALU.mult, op1=ALU.add), r=["C_z", "C_c1", "par"], w=["C_c1"])
                    S.dve(lambda e, bb=bb, yT=yT, ct=ct: e.tensor_tensor(out=yT[:, ct, :], in0=bb[:, :], in1=c1, op=ALU.mult),
                          r=[bkk, "C_c1"], w=[yk])
                S.dma("sp", lambda e, yT=yT, g=g: e.dma_start(out=yT_d[:, :, g * 512:(g + 1) * 512].rearrange("k p n -> p k n"), in_=yT),
                      r=[yk], w=[("yT_d", g)])

        if STAGE >= 4:
            A.reset()
            lamt = A.f32(256)
            lprod = A.f32(128)
            lsum = A.f32(2)
            lex = A.f32(2)
            nlam = A.f32(1)
            sgs = A.f32(1)
            ones_b = A.bf16(128)
            ones_f = A.f32(128)
            mk = A.bf16(8 * 512).rearrange("p (k n) -> p k n", n=512)
            QT_s = [A.bf16(512) for _ in range(2)]
            KT_s = [A.bf16(L) for _ in range(2)]
            V_s = [A.bf16(33 * 128).rearrange("p (k n) -> p k n", n=128) for _ in range(2)]
            E_s = [[A.bf16(512) for _ in range(2)] for _ in range(2)]
            rz = [A.f32(512) for _ in range(2)]
            tt_ = [A.f32(512) for _ in range(2)]
            ocomb = A.f32(512)
            sq = A.f32(512)
            rst = A.f32(512)
            on_s = [A.bf16(512) for _ in range(2)]
            S.barrier()
            S.dma("sp", lambda e: e.dma_start(out=lamt, in_=lamv[:, :]), w=["lamt"])
            S.dma("sp", lambda e: e.dma_start(out=mk, in_=maskd[:, :, :]), w=["mk"])
            S.pool(lambda e: e.memset(ones_b, 1.0), w=["ones_b"])
            S.pool(lambda e: e.memset(ones_f, 1.0), w=["ones_f"])
            S.dve(lambda e: e.tensor_tensor(out=lprod, in0=lamt[:, 0:128], in1=lamt[:, 128:256], op=ALU.mult),
                  r=["lamt"], w=["lprod"])
            S.dve(lambda e: e.tensor_reduce(out=lsum, in_=V3(lprod, 2), axis=AX.X, op=ALU.add), r=["lprod"], w=["lsum"])
            S.act(lambda e: e.activation(out=lex, in_=lsum, func=AF.Exp), r=["lsum"], w=["lex"])
            S.dve(lambda e: e.tensor_tensor(out=nlam, in0=lex[:, 1:2], in1=lex[:, 0:1], op=ALU.subtract), r=["lex"], w=["nlam"])
            S.pool(lambda e: e.tensor_scalar(out=nlam, in0=nlam, scalar1=-LAM_INIT, scalar2=1.0, op0=ALU.add, op1=ALU.mult), r=["nlam"], w=["nlam"])
            S.pool(lambda e: e.tensor_scalar(out=sgs, in0=parv[:, 32:33], scalar1=1.0 - LAM_INIT, scalar2=1.0, op0=ALU.mult, op1=ALU.mult),
                   r=["par"], w=["sgs"])
            heads = [(g, h) for g in range(4) for h in range(NH)]

            def slot_of(n):
                sl = n % 2
                return sl, QT_s[sl], KT_s[sl], V_s[sl], f"D_Q{sl}", f"D_K{sl}", f"D_V{sl}"

            def issue_loads(n):
                g, h = heads[n]
                ntile = 8 * g + 8
                nk = NMETA + 128 * ntile
                sl, QT, KT, Vh, kq, kk, kv = slot_of(n)
                S.dma("sp", lambda e: e.dma_start(out=QT, in_=QT_d[h, :, g * 512:(g + 1) * 512]), r=[("B0out", h, g)], w=[kq])
                S.dma("sp", lambda e: e.dma_start(out=KT[:, :nk], in_=KT_d[h, :, 0:nk]), r=[("A0out", h, c) for c in range(9)], w=[kk])
                S.dma("sp", lambda e: e.dma_start(out=Vh[:NMETA, 0, :], in_=V_d[0:NMETA, h * 128:(h + 1) * 128]), r=[("A1out", 0)], w=[kv + "m"])
                S.dma("sp", lambda e: e.dma_start(
                    out=Vh[:, 1:1 + ntile, :], in_=V_d[NMETA:NMETA + 128 * ntile, h * 128:(h + 1) * 128].rearrange("(t p) n -> p t n", p=128)),
                    r=[("A1out", NMETA + 128 * t) for t in range(32)], w=[kv])

            def s_part(n, t):
                g, h = heads[n]
                sl, QT, KT, Vh, kq, kk, kv = slot_of(n)
                R = NMETA if t == 0 else 128
                kc0 = 0 if t == 0 else NMETA + 128 * (t - 1)
                tp_ = t % 2
                jj = t - 1 - 8 * g
                masked = (t >= 1 and jj >= 0)
                for m in range(2):
                    sb = bank_f[2 * tp_ + m]
                    Em = E_s[tp_][m]

                    def smm(e, sb=sb, m=m):
                        ins = e.matmul(sb[:R, :], lhsT=KT[m * 64:(m + 1) * 64, kc0:kc0 + R], rhs=QT[m * 64:(m + 1) * 64, :],
                                       start=True, stop=(not masked))
                        if masked:
                            ins = e.matmul(sb[:R, :], lhsT=idb, rhs=mk[:, jj, :], start=False, stop=True)
                        return ins
                    S.pe(smm, r=[kq, kk, "mk", "idb"], w=[f"D_S{tp_}{m}"])
                    S.act(lambda e, sb=sb, Em=Em: e.activation(out=Em[:R, :], in_=sb[:R, :], func=AF.Exp, scale=0.125),
                          r=[f"D_S{tp_}{m}"], w=[f"D_E{tp_}{m}"])

            def av_part(n, t):
                g, h = heads[n]
                ntile = 8 * g + 8
                sl, QT, KT, Vh, kq, kk, kv = slot_of(n)
                R = NMETA if t == 0 else 128
                tp_ = t % 2
                for m in range(2):
                    Em = E_s[tp_][m]

                    def av(e, Em=Em, m=m):
                        e.matmul(bank_f[4 + m][:, :], lhsT=Vh[:R, t, :], rhs=Em[:R, :], start=(t == 0), stop=(t == ntile))
                        return e.matmul(bank_f[6 + m][:, :], lhsT=ones_b[:R, :], rhs=Em[:R, :], start=(t == 0), stop=(t == ntile))
                    S.pe(av, r=[f"D_E{tp_}{m}", kv, kv + "m", "ones_b"], w=[f"D_O{m}"])

            def fin_a(n):
                for m in range(2):
                    S.dve(lambda e, m=m: e.reciprocal(out=rz[m], in_=bank_f[6 + m][:, :]), r=[f"D_O{m}"], w=[f"D_rz{m}"])
                    S.dve(lambda e, m=m: e.tensor_tensor(out=tt_[m], in0=bank_f[4 + m][:, :], in1=rz[m], op=ALU.mult),
                          r=[f"D_O{m}", f"D_rz{m}"], w=[f"D_t{m}"])

            def fin_b(n):
                g, h = heads[n]
                sl = n % 2
                S.dve(lambda e: e.scalar_tensor_tensor(out=ocomb, in0=tt_[1], scalar=nlam[:, 0:1], in1=tt_[0], op0=ALU.mult, op1=ALU.add),
                      r=["D_t0", "D_t1", "nlam"], w=["D_oc"])
                S.pool(lambda e: e.tensor_tensor(out=sq, in0=ocomb, in1=ocomb, op=ALU.mult), r=["D_oc"], w=["D_sq"])
                S.pe(lambda e: e.matmul(bank_f[2][:, :], lhsT=ones_f, rhs=sq, start=True, stop=True), r=["D_sq", "ones_f"], w=["D_S10"])
                S.act(lambda e: e.activation(out=rst, in_=bank_f[2][:, :], func=AF.Sqrt, scale=1.0 / 128.0, bias=EPS), r=["D_S10"], w=["D_rst"])
                S.dve(lambda e: e.reciprocal(out=rst, in_=rst), r=["D_rst"], w=["D_rst"])
                S.dve(lambda e: e.tensor_tensor(out=ocomb, in0=ocomb, in1=rst, op=ALU.mult), r=["D_oc", "D_rst"], w=["D_oc"])
                on = on_s[sl]
                S.pool(lambda e: e.tensor_scalar(out=on, in0=ocomb, scalar1=sgs[:, 0:1], scalar2=1.0, op0=ALU.mult, op1=ALU.mult),
                       r=["D_oc", "sgs"], w=[f"D_on{sl}"])
                S.dma("sp", lambda e: e.dma_start(out=OnT_d[h, :, g * 512:(g + 1) * 512], in_=on), r=[f"D_on{sl}"], w=[("OnT_d", h, g)])

            issue_loads(0)
            for n in range(len(heads)):
                if n + 1 < len(heads):
                    issue_loads(n + 1)
                nt = 8 * heads[n][0] + 8
                s_part(n, 0)
                for t in range(nt + 1):
                    if t + 1 <= nt:
                        s_part(n, t + 1)
                    av_part(n, t)
                    if t == 2 and n >= 1:
                        fin_b(n - 1)
                fin_a(n)
            fin_b(len(heads) - 1)

        if STAGE >= 5:
            A.reset()
            hT_a = A.bf16(16 * 2048).rearrange("p (k n) -> p k n", n=2048)
            on_a = A.bf16(8 * 2048).rearrange("p (k n) -> p k n", n=2048)
            y_a = A.bf16(8 * 2048).rearrange("p (k n) -> p k n", n=2048)
            Wg_s = [A.bf16(16 * 512).rearrange("p (k n) -> p k n", n=512) for _ in range(2)]
            Wp_s = [A.bf16(8 * 512).rearrange("p (k n) -> p k n", n=512) for _ in range(2)]
            mT_s = [A.bf16(2 * 512).rearrange("p (k n) -> p k n", n=512) for _ in range(2)]
            sg_t = [A.f32(512) for _ in range(2)]
            t12 = [A.f32(512) for _ in range(2)]
            S.barrier()
            for g in range(4):
                S.dma("sp", lambda e, g=g: e.dma_start(out=hT_a[:, :, g * 512:(g + 1) * 512], in_=hTo_d[:, :, g * 512:(g + 1) * 512].rearrange("k p n -> p k n")),
                      r=[("hTo_d", g * 512)], w=[f"E_hT{g}"])
                S.dma("sp", lambda e, g=g: e.dma_start(out=on_a[:, :, g * 512:(g + 1) * 512], in_=OnT_d[:, :, g * 512:(g + 1) * 512].rearrange("k p n -> p k n")),
                      r=[("OnT_d", h, g) for h in range(NH)], w=[f"E_on{g}"])
                S.dma("sp", lambda e, g=g: e.dma_start(out=y_a[:, :, g * 512:(g + 1) * 512], in_=yT_d[:, :, g * 512:(g + 1) * 512].rearrange("k p n -> p k n")),
                      r=[("yT_d", g)], w=[f"E_y{g}"])
            octr = 0
            for c8 in range(8):
                ws = c8 % 2
                Wg, Wp = Wg_s[ws], Wp_s[ws]
                S.dma("pool", lambda e, Wg=Wg, c8=c8: e.dma_start(out=Wg[:, :, 0:256], in_=w_in[:, 6144 + c8 * 256: 6144 + (c8 + 1) * 256].rearrange("(k p) n -> p k n", p=128)), w=[f"E_Wga{ws}"])
                S.dma("pool", lambda e, Wg=Wg, c8=c8: e.dma_start(out=Wg[:, :, 256:512], in_=w_in[:, 8192 + c8 * 256: 8192 + (c8 + 1) * 256].rearrange("(k p) n -> p k n", p=128)), w=[f"E_Wgc{ws}"])
                S.dma("pool", lambda e, Wp=Wp, c8=c8: e.dma_start(out=Wp[:, :, 0:256], in_=w_pa[:, c8 * 256:(c8 + 1) * 256].rearrange("(k p) n -> p k n", p=128)), w=[f"E_Wpa{ws}"])
                S.dma("pool", lambda e, Wp=Wp, c8=c8: e.dma_start(out=Wp[:, :, 256:512], in_=w_pc[:, c8 * 256:(c8 + 1) * 256].rearrange("(k p) n -> p k n", p=128)), w=[f"E_Wpc{ws}"])
                for g in range(4):
                    mT = mT_s[octr % 2]
                    mk_ = f"E_mT{octr % 2}"
                    octr += 1
                    for k2 in range(2):
                        b0 = 4 * (k2 % 2)
                        bk = f"E_bk{k2 % 2}"

                        def mmg(e, Wg=Wg, Wp=Wp, g=g, k2=k2, b0=b0):
                            for j in range(2):
                                for k in range(16):
                                    e.matmul(bank_f[b0 + j][:, :], lhsT=Wg[:, k, j * 256 + k2 * 128: j * 256 + (k2 + 1) * 128],
                                             rhs=hT_a[:, k, g * 512:(g + 1) * 512], start=(k == 0), stop=(k == 15))
                            for k in range(8):
                                e.matmul(bank_f[b0 + 2][:, :], lhsT=Wp[:, k, k2 * 128:(k2 + 1) * 128], rhs=on_a[:, k, g * 512:(g + 1) * 512],
                                         start=(k == 0), stop=(k == 7))
                            for k in range(8):
                                ins = e.matmul(bank_f[b0 + 3][:, :], lhsT=Wp[:, k, 256 + k2 * 128: 256 + (k2 + 1) * 128],
                                               rhs=y_a[:, k, g * 512:(g + 1) * 512], start=(k == 0), stop=(k == 7))
                            return ins
                        S.pe(mmg, r=[f"E_Wga{ws}", f"E_Wgc{ws}", f"E_Wpa{ws}", f"E_Wpc{ws}", f"E_hT{g}", f"E_on{g}", f"E_y{g}"], w=[bk])
                        for j in range(2):
                            S.act(lambda e, j=j, b0=b0: e.activation(out=sg_t[j], in_=bank_f[b0 + j][:, :], func=AF.Sigmoid), r=[bk], w=[f"E_sg{j}"])
                            S.dve(lambda e, j=j, b0=b0: e.tensor_tensor(out=t12[j], in0=bank_f[b0 + 2 + j][:, :], in1=sg_t[j], op=ALU.mult),
                                  r=[bk, f"E_sg{j}"], w=[f"E_t{j}"])
                        S.pool(lambda e, mT=mT, k2=k2: e.tensor_tensor(out=mT[:, k2, :], in0=t12[0], in1=t12[1], op=ALU.add), r=["E_t0", "E_t1"], w=[mk_])
                    S.dma("sp", lambda e, mT=mT, g=g, c8=c8: e.dma_start(
                        out=mT_d[2 * c8:2 * c8 + 2, :, g * 512:(g + 1) * 512].rearrange("k p n -> p k n"), in_=mT),
                        r=[mk_], w=[("mT_d", g, c8)])

        if STAGE >= 6:
            A.reset()
            Wo = A.bf16(16 * 2048).rearrange("p (k n) -> p k n", n=2048)
            mt_s = [A.bf16(16 * 128).rearrange("p (k n) -> p k n", n=128) for _ in range(2)]
            xr_s = [A.f32(D) for _ in range(2)]
            r_s = [A.f32(D) for _ in range(2)]
            G0, B0, G1, B1 = A.f32(D), A.f32(D), A.f32(D), A.f32(D)
            h1Tf = A.f32(16 * 128).rearrange("p (k n) -> p k n", n=128)
            h1Tb_s = [A.bf16(16 * 512).rearrange("p (k n) -> p k n", n=512) for _ in range(2)]
            wr = A.f32(16 * 64).rearrange("p (k n) -> p k n", n=64)
            rbt = A.f32(64)
            st = A.f32(24).rearrange("p (a b) -> p a b", b=6)
            mv = A.f32(2)
            rstd = A.f32(1)
            sgm, sel, eq, sel2, selm, msk, smm = [A.f32(64) for _ in range(7)]
            m1, m2, gs, top8, gm, pen, top8e = [A.f32(8) for _ in range(7)]
            ssum = A.f32(1)
            S.barrier()
            S.dma("pool", lambda e: e.dma_start(out=Wo, in_=w_out[:, :].rearrange("(k p) n -> p k n", p=128)), w=["F_Wo"])
            for i, t_ in enumerate((G0, B0, G1, B1)):
                S.dma("sp", lambda e, i=i, t_=t_: e.dma_start(out=t_, in_=bcd[i, :, :]), w=[f"F_bc{i}"])
            S.dma("sp", lambda e: e.dma_start(out=wr, in_=w_router[:, :].rearrange("(k p) n -> p k n", p=128)), w=["F_wr"])
            S.dma("sp", lambda e: e.dma_start(out=rbt, in_=rbd[:, :]), w=["F_rb"])
            S.pool(lambda e: e.memset(ewt[:, :], 1.0), w=["ew"])

            def ln_stats(src, keyin, pf):
                def stats(e):
                    for i in range(4):
                        ins = e.bn_stats(out=st[:, i, :], in_=src[:, i * 512:(i + 1) * 512])
                    return ins
                S.dve(stats, r=[keyin], w=[pf + "st"])
                S.dve(lambda e: e.bn_aggr(out=mv, in_=st), r=[pf + "st"], w=[pf + "mv"])
                S.act(lambda e: e.activation(out=rstd, in_=mv[:, 1:2], func=AF.Sqrt, bias=EPS, scale=1.0), r=[pf + "mv"], w=[pf + "rstd"])
                S.dve(lambda e: e.reciprocal(out=rstd, in_=rstd), r=[pf + "rstd"], w=[pf + "rstd"])
                S.dve(lambda e: e.tensor_scalar(out=src, in0=src, scalar1=mv[:, 0:1], scalar2=rstd[:, 0:1], op0=ALU.subtract, op1=ALU.mult),
                      r=[keyin], s=[pf + "rstd", pf + "mv"], w=[keyin])

            for tt in range(16):
                sl = tt % 2
                mt, xr, rr, h1Tb = mt_s[sl], xr_s[sl], r_s[sl], h1Tb_s[(tt // 4) % 2]
                g4, t4 = tt // 4, tt % 4
                S.dma("sp", lambda e, mt=mt, tt=tt: e.dma_start(out=mt, in_=mT_d[:, :, tt * 128:(tt + 1) * 128].rearrange("k p n -> p k n")),
                      r=[("mT_d", tt // 4, c8) for c8 in range(8)], w=[f"F_mt{sl}"])
                S.dma("sp", lambda e, xr=xr, tt=tt: e.dma_start(out=xr, in_=xo[tt * 128:(tt + 1) * 128, :]), w=[f"F_xr{sl}"])

                def mmo(e, mt=mt):
                    for cc in range(4):
                        for k in range(16):
                            ins = e.matmul(bank_f[cc][:, :], lhsT=mt[:, k, :], rhs=Wo[:, k, cc * 512:(cc + 1) * 512], start=(k == 0), stop=(k == 15))
                    return ins
                S.pe(mmo, r=[f"F_mt{sl}", "F_Wo"], w=["F_po"])
                ln_stats(xr, f"F_xr{sl}", "F_a")
                S.dve(lambda e, xr=xr: e.tensor_tensor(out=xr, in0=xr, in1=G0, op=ALU.mult), r=[f"F_xr{sl}", "F_bc0"], w=[f"F_xr{sl}"])
                S.pool(lambda e, xr=xr: e.tensor_tensor(out=xr, in0=xr, in1=B0, op=ALU.add), r=[f"F_xr{sl}", "F_bc1"], w=[f"F_xr{sl}"])

                def resid(e, xr=xr, rr=rr):
                    for cc in range(4):
                        ins = e.scalar_tensor_tensor(out=rr[:, cc * 512:(cc + 1) * 512], in0=xr[:, cc * 512:(cc + 1) * 512], scalar=ALPHA,
                                                     in1=bank_f[cc][:, :], op0=ALU.mult, op1=ALU.add)
                    return ins
                S.dve(resid, r=[f"F_xr{sl}", "F_po"], w=[f"F_r{sl}"])
                ln_stats(rr, f"F_r{sl}", "F_b")
                S.dve(lambda e, rr=rr: e.tensor_tensor(out=rr, in0=rr, in1=G1, op=ALU.mult), r=[f"F_r{sl}", "F_bc2"], w=[f"F_r{sl}"])
                S.pool(lambda e, rr=rr: e.tensor_tensor(out=rr, in0=rr, in1=B1, op=ALU.add), r=[f"F_r{sl}", "F_bc3"], w=[f"F_r{sl}"])
                S.dma("sp", lambda e, rr=rr, tt=tt: e.dma_start(out=h1_d[tt * 128:(tt + 1) * 128, :], in_=rr), r=[f"F_r{sl}"], w=[("h1_d", tt)])

                if SUB6 >= 1:
                    def trf(e, rr=rr):
                        for k in range(16):
                            ins = e.matmul(bank_f[4 + k // 4][:, (k % 4) * 128:(k % 4 + 1) * 128], lhsT=rr[:, k * 128:(k + 1) * 128], rhs=idf, start=True, stop=True)
                        return ins
                    S.pe(trf, r=[f"F_r{sl}", "idf"], w=["F_pt"])

                    def evb(e, h1Tb=h1Tb, t4=t4):
                        for q in range(4):
                            ins = e.activation(out=h1Tb[:, 4 * q:4 * q + 4, t4 * 128:(t4 + 1) * 128], in_=V3(bank_f[4 + q][:, :], 4), func=AF.Copy)
                        return ins
                    if SUB6B & 1:
                        S.act(evb, r=["F_pt"], w=[f"F_h1Tb{g4 % 2}"])

                    def evf(e):
                        for q in range(4):
                            ins = e.tensor_copy(out=h1Tf[:, 4 * q:4 * q + 4, :], in_=V3(bank_f[4 + q][:, :], 4))
                        return ins
                    if SUB6B & 2:
                        S.dve(evf, r=["F_pt", f"F_h1Tb{g4 % 2}"], w=["F_h1Tf"])
                    if (SUB6B & 4) and t4 == 3:
                        S.dma("sp", lambda e, h1Tb=h1Tb, g4=g4: e.dma_start(out=h1T_d[:, :, g4 * 512:(g4 + 1) * 512].rearrange("k p n -> p k n"), in_=h1Tb),
                              r=[f"F_h1Tb{g4 % 2}"], w=[("h1T_d", g4)])

                if SUB6 >= 2:
                    def mmr(e):
                        for k in range(16):
                            ins = e.matmul(bank_f[0][:, 0:64], lhsT=h1Tf[:, k, :], rhs=wr[:, k, :], start=(k == 0), stop=(k == 15))
                        return ins
                    S.pe(mmr, r=["F_h1Tf", "F_wr"], w=["F_po"])
                if SUB6 >= 3:
                    S.act(lambda e: e.activation(out=sgm, in_=bank_f[0][:, 0:64], func=AF.Sigmoid), r=["F_po"], w=["R_sgm"])
                    S.dve(lambda e: e.tensor_tensor(out=sel, in0=sgm, in1=rbt, op=ALU.add), r=["R_sgm", "F_rb"], w=["R_sel"])
                    S.dve(lambda e: e.tensor_reduce(out=m1, in_=V3(sel, 8), axis=AX.X, op=ALU.max), r=["R_sel"], w=["R_m1"])
                    S.dve(lambda e: e.tensor_tensor(out=V3(eq, 8), in0=V3(sel, 8), in1=m1.unsqueeze(2).to_broadcast([128, 8, 8]), op=ALU.is_equal),
                          r=["R_sel", "R_m1"], w=["R_eq"])
                    S.dve(lambda e: e.scalar_tensor_tensor(out=sel2, in0=eq, scalar=-1e30, in1=sel, op0=ALU.mult, op1=ALU.add),
                          r=["R_eq", "R_sel"], w=["R_sel2"])
                    S.dve(lambda e: e.tensor_reduce(out=m2, in_=V3(sel2, 8), axis=AX.X, op=ALU.max), r=["R_sel2"], w=["R_m2"])
                    S.dve(lambda e: e.tensor_tensor(out=gs, in0=m1, in1=m2, op=ALU.add), r=["R_m1", "R_m2"], w=["R_gs"])
                    S.dve(lambda e: e.max(out=top8, in_=gs), r=["R_gs"], w=["R_top8"])
                    S.dve(lambda e: e.tensor_tensor(out=gm, in0=gs, in1=top8[:, 3:4].to_broadcast([128, 8]), op=ALU.is_ge), r=["R_gs", "R_top8"], w=["R_gm"])
                    S.dve(lambda e: e.tensor_scalar(out=pen, in0=gm, scalar1=-1.0, scalar2=1e30, op0=ALU.add, op1=ALU.mult), r=["R_gm"], w=["R_pen"])
                    S.dve(lambda e: e.tensor_tensor(out=V3(selm, 8), in0=V3(sel, 8), in1=pen.unsqueeze(2).to_broadcast([128, 8, 8]), op=ALU.add),
                          r=["R_sel", "R_pen"], w=["R_selm"])
                    S.dve(lambda e: e.max(out=top8e, in_=selm), r=["R_selm"], w=["R_top8e"])
                    S.dve(lambda e: e.tensor_tensor(out=msk, in0=selm, in1=top8e[:, 7:8].to_broadcast([128, 64]), op=ALU.is_ge),
                          r=["R_selm", "R_top8e"], w=["R_msk"])
                    S.dve(lambda e: e.tensor_tensor(out=smm, in0=sgm, in1=msk, op=ALU.mult), r=["R_sgm", "R_msk"], w=["R_smm"])
                    S.dve(lambda e: e.tensor_reduce(out=ssum, in_=smm, axis=AX.X, op=ALU.add), r=["R_smm"], w=["R_ssum"])
                    S.dve(lambda e: e.reciprocal(out=ssum, in_=ssum), r=["R_ssum"], w=["R_ssum"])
                    S.dve(lambda e, tt=tt: e.scalar_tensor_tensor(out=ewt[:, tt * 65: tt * 65 + 64], in0=smm, scalar=2.5,
                                                                  in1=ssum[:, 0:1].to_broadcast([128, 64]), op0=ALU.mult, op1=ALU.mult),
                          r=["R_smm", "R_ssum"], w=["ew"])

            if DEBUG:
                S.dma("sp", lambda e: e.dma_start(out=ew_d[:, :], in_=ewt), r=["ew"], w=["ew_d"])

        if STAGE >= 7:
            experts = [(w_eg[e_], w_eu[e_], w_ed[e_]) for e_ in range(NE)] + [(w_sg, w_su, w_sd)]
            def moe_half(hf):
                A.reset()
                acc = A.f32(8 * D).rearrange("p (t n) -> p t n", n=D)
                h1T = A.bf16(16 * 1024).rearrange("p (k n) -> p k n", n=1024)
                Wgu_s = [A.bf16(16 * 1024).rearrange("p (k n) -> p k n", n=1024) for _ in range(2)]
                Wd_s = [A.bf16(4 * 2048).rearrange("p (k n) -> p k n", n=2048) for _ in range(2)]
                hid = A.bf16(4 * 1024).rearrange("p (k n) -> p k n", n=1024)
                sgt_one = A.f32(512)
                sgt = [sgt_one, sgt_one]
                pf = f"M{hf}"
                S.barrier()
                for t in range(8):
                    S.dma("sp", lambda e, t=t: e.dma_start(out=acc[:, t, :], in_=h1_d[(8 * hf + t) * 128:(8 * hf + t + 1) * 128, :]),
                          r=[("h1_d", 8 * hf + t)], w=[(pf + "acc", t)])
                    S.pool(lambda e, t=t: e.tensor_scalar(out=acc[:, t, :], in0=acc[:, t, :], scalar1=ALPHA, scalar2=1.0, op0=ALU.mult, op1=ALU.mult),
                           r=[(pf + "acc", t)], w=[(pf + "acc", t)])
                S.dma("sp", lambda e: e.dma_start(out=h1T, in_=h1T_d[:, :, hf * 1024:(hf + 1) * 1024].rearrange("k p n -> p k n")),
                      r=[("h1T_d", 2 * hf), ("h1T_d", 2 * hf + 1)], w=[pf + "h1T"])
                for ei, (wg_, wu_, wd_) in enumerate(experts[:NEXP]):
                    ws = ei % 2
                    Wgu, Wd = Wgu_s[ws], Wd_s[ws]
                    S.dma("pool", lambda e, Wgu=Wgu, wg_=wg_: e.dma_start(out=Wgu[:, :, 0:512], in_=wg_.rearrange("(k p) n -> p k n", p=128)), w=[f"{pf}Wg{ws}"])
                    S.dma("pool", lambda e, Wgu=Wgu, wu_=wu_: e.dma_start(out=Wgu[:, :, 512:1024], in_=wu_.rearrange("(k p) n -> p k n", p=128)), w=[f"{pf}Wu{ws}"])
                    S.dma("pool", lambda e, Wd=Wd, wd_=wd_: e.dma_start(out=Wd, in_=wd_.rearrange("(k p) n -> p k n", p=128)), w=[f"{pf}Wd{ws}"])
                    for m in range(4):
                        for n in range(2):
                            b0 = 2 * ((2 * m + n) % 2)
                            bk = f"{pf}gu{b0}"

                            def mmgu(e, Wgu=Wgu, m=m, n=n, b0=b0):
                                for j in range(2):
                                    for k in range(16):
                                        ins = e.matmul(bank_f[b0 + j][:, :], lhsT=Wgu[:, k, j * 512 + m * 128: j * 512 + (m + 1) * 128],
                                                       rhs=h1T[:, k, n * 512:(n + 1) * 512], start=(k == 0), stop=(k == 15))
                                return ins
                            S.pe(mmgu, r=[f"{pf}Wg{ws}", f"{pf}Wu{ws}", pf + "h1T"], w=[bk])
                            sg_ = sgt[(2 * m + n) % 2]
                            S.act(lambda e, sg_=sg_, b0=b0: e.activation(out=sg_, in_=bank_f[b0][:, :], func=AF.Silu), r=[bk], w=[f"{pf}sg"])
                            S.dve(lambda e, sg_=sg_, b0=b0, m=m, n=n: e.tensor_tensor(out=hid[:, m, n * 512:(n + 1) * 512], in0=bank_f[b0 + 1][:, :],
                                                                                      in1=sg_, op=ALU.mult), r=[bk, f"{pf}sg"], w=[(pf + "hid", m, n)])
                    for t in range(8):
                        for cc in range(4):
                            yb = 4 + cc
                            S.pe(lambda e, t=t, cc=cc, yb=yb, Wd=Wd: [e.matmul(bank_f[yb][:, :], lhsT=hid[:, m, t * 128:(t + 1) * 128],
                                                                              rhs=Wd[:, m, cc * 512:(cc + 1) * 512], start=(m == 0), stop=(m == 3))
                                                                     for m in range(4)][-1],
                                 r=[(pf + "hid", m, t // 4) for m in range(4)] + [f"{pf}Wd{ws}"], w=[f"{pf}y{cc}"])
                            S.dve(lambda e, t=t, cc=cc, yb=yb, ei=ei: e.scalar_tensor_tensor(
                                out=acc[:, t, cc * 512:(cc + 1) * 512], in0=bank_f[yb][:, :],
                                scalar=ewt[:, (8 * hf + t) * 65 + ei:(8 * hf + t) * 65 + ei + 1], in1=acc[:, t, cc * 512:(cc + 1) * 512],
                                op0=ALU.mult, op1=ALU.add), r=[f"{pf}y{cc}", (pf + "acc", t), "ew"], w=[(pf + "acc", t)])
                S.barrier()
                A.off = 8 * D
                G2, B2 = A.f32(D), A.f32(D)
                st = A.f32(24).rearrange("p (a b) -> p a b", b=6)
                mv = A.f32(2)
                rstd = A.f32(1)
                S.dma("sp", lambda e: e.dma_start(out=G2, in_=bcd[4, :, :]), w=[pf + "G2"])
                S.dma("sp", lambda e: e.dma_start(out=B2, in_=bcd[5, :, :]), w=[pf + "B2"])
                for t in range(8):
                    src = acc[:, t, :]
                    kin = (pf + "acc", t)

                    def stats(e, src=src):
                        for i in range(4):
                            ins = e.bn_stats(out=st[:, i, :], in_=src[:, i * 512:(i + 1) * 512])
                        return ins
                    S.dve(stats, r=[kin], w=[pf + "st"])
                    S.dve(lambda e: e.bn_aggr(out=mv, in_=st), r=[pf + "st"], w=[pf + "mv"])
                    S.act(lambda e: e.activation(out=rstd, in_=mv[:, 1:2], func=AF.Sqrt, bias=EPS, scale=1.0), r=[pf + "mv"], w=[pf + "rstd"])
                    S.dve(lambda e: e.reciprocal(out=rstd, in_=rstd), r=[pf + "rstd"], w=[pf + "rstd"])
                    S.dve(lambda e, src=src: e.tensor_scalar(out=src, in0=src, scalar1=mv[:, 0:1], scalar2=rstd[:, 0:1], op0=ALU.subtract, op1=ALU.mult),
                          r=[kin], s=[pf + "rstd", pf + "mv"], w=[kin])
                    S.dve(lambda e, src=src: e.tensor_tensor(out=src, in0=src, in1=G2, op=ALU.mult), r=[kin, pf + "G2"], w=[kin])
                    S.pool(lambda e, src=src: e.tensor_tensor(out=src, in0=src, in1=B2, op=ALU.add), r=[kin, pf + "B2"], w=[kin])
                    S.dma("sp", lambda e, src=src, t=t: e.dma_start(out=out[(8 * hf + t) * 128:(8 * hf + t + 1) * 128, :], in_=src),
                          r=[kin], w=[("out", hf, t)])

            if STAGE >= 7:
                for hf_ in range(2):
                    moe_half(hf_)

        S.barrier()
        S.emit(nc, sems)
    return nc


def _rope_tables():
    half = 32
    inv = (10000.0 ** (-np.arange(half, dtype=np.float32) / half)).astype(np.float32)
    pos = np.arange(L, dtype=np.float32)
    ang = pos[None, :] * inv[:, None]
    cos = np.cos(ang).astype(np.float32)
    sin = np.sin(ang).astype(np.float32)
    cos64 = np.concatenate([cos, cos], 0)
    sin64 = np.concatenate([-sin, sin], 0)
    return np.concatenate([cos64, cos64], 0), np.concatenate([sin64, sin64], 0)


def _perm_cols():
    idx = np.arange(1024).reshape(NH, 2, 2, 32)[:, :, ::-1, :].reshape(1024)
    return idx


_NC_CACHE = {}
_DECL = []


def kernel(**inputs):
    f = lambda k: np.asarray(inputs[k], np.float32)
    x = f("x")
    w_in = np.ascontiguousarray(f("w_in")[0])
    pidx = _perm_cols()
    w_qkp = np.ascontiguousarray(np.concatenate([w_in[:, 0:1024][:, pidx], w_in[:, 1024:2048][:, pidx]], axis=1))
    cosk, sink = _rope_tables()
    par = np.zeros((128, 64), np.float32)
    par[:, 0:16] = f("ln0_g").reshape(16, 128).T
    par[:, 16:32] = f("ln0_b").reshape(16, 128).T
    par[:, 32] = f("subln_g")[0]
    wc = f("w_conv")[0]
    for k in range(3):
        par[:, 33 + 8 * k: 41 + 8 * k] = wc[k].reshape(8, 128).T
    ident = np.eye(128, dtype=np.float32)
    meta = np.ascontiguousarray(f("meta_tokens"))
    lamv = np.ascontiguousarray(np.broadcast_to(np.concatenate(
        [f("lambda_q1")[0], f("lambda_q2")[0], f("lambda_k1")[0], f("lambda_k2")[0]])[None, :], (128, 256)))
    rbd = np.ascontiguousarray(np.broadcast_to(f("router_bias")[0][None, :], (128, NE)))
    bcd = np.ascontiguousarray(np.stack([np.broadcast_to(v[None, :], (128, D)) for v in
                                         (f("ln0_g"), f("ln0_b"), f("ln1_g")[0], f("ln1_b")[0], f("ln2_g")[0], f("ln2_b")[0])]))
    shared = dict(meta=meta, w_in=w_in, w_qkp=w_qkp, par=par, ident=ident, cosk=cosk, sink=sink, lamv=lamv, rbd=rbd, bcd=bcd,
                  w_pa=np.ascontiguousarray(f("w_proj_attn")[0]), w_pc=np.ascontiguousarray(f("w_proj_conv")[0]),
                  w_out=np.ascontiguousarray(f("w_out")[0]), w_router=np.ascontiguousarray(f("w_router")[0]),
                  w_eg=np.ascontiguousarray(f("w_exp_gate")[0]), w_eu=np.ascontiguousarray(f("w_exp_up")[0]),
                  w_ed=np.ascontiguousarray(f("w_exp_down")[0]), w_sg=np.ascontiguousarray(f("w_sh_gate")[0]),
                  w_su=np.ascontiguousarray(f("w_sh_up")[0]), w_sd=np.ascontiguousarray(f("w_sh_down")[0]))
    if "nc" not in _NC_CACHE:
        _NC_CACHE["nc"] = build_program()
    nc = _NC_CACHE["nc"]
    in_maps = []
    blocks_of = []
    for c in range(8):
        b, p = c // 2, c % 2
        blks = [8 * g + OFFS[p][i] for g in range(4) for i in range(4)]
        blocks_of.append(blks)
        xo = np.ascontiguousarray(np.concatenate([x[b, 128 * j:128 * (j + 1)] for j in blks], 0))
        seqx = x[b]
        xh = np.ascontiguousarray(np.concatenate([(meta[14:16] if j == 0 else seqx[128 * j - 2:128 * j]) for j in blks], 0))
        pos = np.concatenate([NMETA + 128 * j + np.arange(128) for j in blks])
        mask = np.zeros((128, 8, 4, 128), np.float32)
        r = np.arange(128)
        diag = ((r[:, None] // 64) <= (r[None, :] // 64)).astype(np.float32)
        for o in range(8):
            for i in range(4):
                oo = OFFS[p][i]
                if o < oo:
                    mask[:, o, i, :] = 1.0
                elif o == oo:
                    mask[:, o, i, :] = diag
        m = dict(shared)
        m.update(xs=np.ascontiguousarray(x[b]), xo=xo, xh=xh, cosq=np.ascontiguousarray(cosk[:, pos]),
                 sinq=np.ascontiguousarray(sink[:, pos]), maskd=((mask.reshape(128, 8, 512) - 1.0) * 10000.0).astype(ml_dtypes.bfloat16))
        in_maps.append({k: v for k, v in m.items() if k in _DECL})
    res = run_bass_kernel_spmd(nc, in_maps, core_ids=list(range(8)))
    kernel.last = res
    outp = np.zeros((4, SEQ, D), np.float32)
    for c in range(8):
        b = c // 2
        o = np.asarray(res.results[c]["out"], np.float32)
        for i, j in enumerate(blocks_of[c]):
            outp[b, 128 * j:128 * (j + 1)] = o[128 * i:128 * (i + 1)]
    return outp
```

```python
import numpy as np
import ml_dtypes
import concourse.bass as bass
import concourse.mybir as mybir
from concourse.bass_utils import run_bass_kernel_spmd

F32 = mybir.dt.float32
BF16 = mybir.dt.bfloat16
AF = mybir.ActivationFunctionType
ALU = mybir.AluOpType
AX = mybir.AxisListType

D = 2048
SEQ = 4096
NMETA = 16
L = SEQ + NMETA
NH = 8
NE = 64
DE = 512
EPS = 1e-5
ALPHA = 2.0 ** 0.25
LAM_INIT = 0.2
OFFS = ((0, 3, 4, 7), (1, 2, 5, 6))


class _Op:
    __slots__ = ("eng", "fn", "deps", "is_dma", "signal", "sem", "val", "idx", "pos")


class Sched:
    ENGS = ("pe", "act", "dve", "pool", "sp")
    RING = 12

    def __init__(self):
        self.ops = {e: [] for e in self.ENGS}
        self.last_w = {}
        self.readers = {}
        self.ndma = {"sp": 0, "pool": 0}
        self.dma_ops = {"sp": [], "pool": []}
        self.pending_barrier = {}
        self.n = 0

    def _add(self, eng, fn, r, w, is_dma, s=()):
        op = _Op()
        op.eng, op.fn, op.is_dma, op.signal, op.sem, op.val = eng, fn, is_dma, False, None, 0
        op.idx = self.n
        op.pos = len(self.ops[eng])
        self.n += 1
        deps = set()
        hard = set()
        for k in tuple(r) + tuple(s):
            lw = self.last_w.get(k)
            if lw is not None:
                deps.add(lw)
                if (not lw.is_dma) and (not is_dma) and lw.eng == eng and eng != "pe" and op.pos - lw.pos <= 2:
                    hard.add(lw)
        for k in w:
            lw = self.last_w.get(k)
            if lw is not None:
                deps.add(lw)
            rd = self.readers.get(k)
            if rd:
                deps.update(rd.values())
        r = tuple(r) + tuple(s)
        if eng in self.pending_barrier:
            deps.update(self.pending_barrier.pop(eng))
        if is_dma:
            n = self.ndma[eng]
            self.ndma[eng] = n + 1
            op.sem = (eng, n % self.RING)
            op.val = 16 * (n // self.RING + 1)
            if n >= self.RING:
                deps.add(self.dma_ops[eng][n - self.RING])
            self.dma_ops[eng].append(op)
        for k in r:
            d = self.readers.setdefault(k, {})
            d[("dma", op.idx) if is_dma else eng] = op
        for k in w:
            self.last_w[k] = op
            self.readers[k] = {}
        op.deps = [d for d in deps if (d.is_dma or is_dma or d.eng != eng)] + [d for d in hard if not (d.is_dma or is_dma or d.eng != eng)]
        for d in op.deps:
            if not d.is_dma:
                d.signal = True
        self.ops[eng].append(op)
        return op

    def pe(self, fn, r=(), w=(), s=()):
        return self._add("pe", fn, r, w, False, s)

    def act(self, fn, r=(), w=(), s=()):
        return self._add("act", fn, r, w, False, s)

    def dve(self, fn, r=(), w=(), s=()):
        return self._add("dve", fn, r, w, False, s)

    def pool(self, fn, r=(), w=(), s=()):
        return self._add("pool", fn, r, w, False, s)

    def dma(self, q, fn, r=(), w=()):
        return self._add(q, fn, r, w, True)

    def barrier(self):
        deps = []
        for e in self.ENGS:
            if self.ops[e]:
                deps.append(self.ops[e][-1])
        for q in ("sp", "pool"):
            deps.extend(self.dma_ops[q][-self.RING:])
        for e in self.ENGS:
            self.pending_barrier[e] = list(deps)

    def emit(self, nc, sems):
        for e in ("pe", "act", "dve", "pool"):
            c = 0
            for op in self.ops[e]:
                if op.is_dma:
                    continue
                if op.signal:
                    c += 1
                    op.sem = e
                    op.val = c
        final = {}
        for q in ("sp", "pool"):
            for op in self.dma_ops[q]:
                final[op.sem] = max(final.get(op.sem, 0), op.val)

        def run(eng_name, e):
            waited = {}
            for op in self.ops[eng_name]:
                need = {}
                for d in op.deps:
                    if d.val > need.get(d.sem, 0):
                        need[d.sem] = d.val
                for s, v in need.items():
                    if waited.get(s, 0) < v:
                        e.wait_ge(sems[s], v)
                        waited[s] = v
                ins = op.fn(e)
                if op.is_dma:
                    ins.then_inc(sems[op.sem], 16)
                elif op.signal:
                    ins.then_inc(sems[op.sem], 1)
            if eng_name == "sp":
                for s, v in final.items():
                    e.wait_ge(sems[s], v)

        with nc.Block() as block:
            @block.tensor
            def _(e):
                run("pe", e)

            @block.scalar
            def _(e):
                run("act", e)

            @block.vector
            def _(e):
                run("dve", e)

            @block.gpsimd
            def _(e):
                run("pool", e)

            @block.sync
            def _(e):
                run("sp", e)


class Arena:
    def __init__(self, t, nwords):
        self.t, self.n, self.off = t, nwords, 0

    def reset(self):
        self.off = 0

    def f32(self, n):
        assert self.off + n <= self.n, ("arena overflow", self.off, n, self.n)
        ap = self.t[:, self.off:self.off + n]
        self.off += n
        return ap

    def bf16(self, n):
        w = (n + 1) // 2
        assert self.off + w <= self.n, ("arena overflow", self.off, w, self.n)
        ap = self.t[:, self.off:self.off + w].bitcast(BF16)
        self.off += w
        return ap


ARENA_WORDS = 51800
NEXP = 65
SUB6 = 3
SUB6B = 7
STAGE = 99
DEBUG = False
NCHK = 9
PASSES = (0, 1)


def build_program():
    nc = bass.Bass("TRN2", target_bir_lowering=False)
    S = Sched()

    def dram(name, shape, dt, kind="ExternalInput"):
        if kind == "ExternalInput":
            if STAGE < 7 and name.startswith(("w_e", "w_s")):
                return None
            _DECL.append(name)
        return nc.dram_tensor(name, list(shape), dt, kind=kind).ap()

    scratch_kind = "ExternalOutput" if DEBUG else "Internal"
    xs = dram("xs", [SEQ, D], F32)
    meta = dram("meta", [NMETA, D], F32)
    w_in = dram("w_in", [D, 10240], F32)
    w_qkp = dram("w_qkp", [D, 2048], F32)
    par = dram("par", [128, 64], F32)
    ident_d = dram("ident", [128, 128], F32)
    cosk = dram("cosk", [128, L], F32)
    sink = dram("sink", [128, L], F32)
    xo = dram("xo", [2048, D], F32)
    xh = dram("xh", [32, D], F32)
    cosq = dram("cosq", [128, 2048], F32)
    sinq = dram("sinq", [128, 2048], F32)
    maskd = dram("maskd", [128, 8, 512], BF16)
    lamv = dram("lamv", [128, 256], F32)
    w_pa = dram("w_pa", [1024, D], F32)
    w_pc = dram("w_pc", [1024, D], F32)
    w_out = dram("w_out", [D, D], F32)
    w_router = dram("w_router", [D, NE], F32)
    rbd = dram("rbd", [128, NE], F32)
    bcd = dram("bcd", [6, 128, D], F32)
    w_eg = dram("w_eg", [NE, D, DE], F32)
    w_eu = dram("w_eu", [NE, D, DE], F32)
    w_ed = dram("w_ed", [NE, DE, D], F32)
    w_sg = dram("w_sg", [D, DE], F32)
    w_su = dram("w_su", [D, DE], F32)
    w_sd = dram("w_sd", [DE, D], F32)
    KT_d = dram("KT_d", [NH, 128, L], BF16, scratch_kind)
    V_d = dram("V_d", [L, 1024], BF16, scratch_kind)
    hTo_d = dram("hTo_d", [16, 128, 2048], BF16, scratch_kind)
    hTh_d = dram("hTh_d", [16, 128, 32], BF16, scratch_kind)
    QT_d = dram("QT_d", [NH, 128, 2048], BF16, scratch_kind)
    yT_d = dram("yT_d", [8, 128, 2048], BF16, scratch_kind)
    OnT_d = dram("OnT_d", [NH, 128, 2048], BF16, scratch_kind)
    mT_d = dram("mT_d", [16, 128, 2048], BF16, scratch_kind)
    h1_d = dram("h1_d", [2048, D], F32, scratch_kind)
    h1T_d = dram("h1T_d", [16, 128, 2048], BF16, scratch_kind)
    ew_d = dram("ew_d", [128, 16 * 65], F32, scratch_kind)
    out = dram("out", [2048, D], F32, "ExternalOutput")

    sem_names = ["pe", "act", "dve", "pool"] + [(q, i) for q in ("sp", "pool") for i in range(Sched.RING)]

    import contextlib
    with contextlib.ExitStack() as es:
        arena_t = es.enter_context(nc.sbuf_tensor("arena", [128, ARENA_WORDS], F32))
        A = Arena(arena_t, ARENA_WORDS)
        cst = es.enter_context(nc.sbuf_tensor("cst", [128, 64 + 128], F32))
        idb_t = es.enter_context(nc.sbuf_tensor("idb", [128, 128], BF16))
        ewt_t = es.enter_context(nc.sbuf_tensor("ewt", [128, 16 * 65], F32))
        ewt = ewt_t[:]
        banks = [es.enter_context(nc.psum_tensor(f"ps{i}", [128, 512], F32)) for i in range(8)]
        sems = {}
        for sn in sem_names:
            nm = sn if isinstance(sn, str) else f"{sn[0]}{sn[1]}"
            sems[sn] = es.enter_context(nc.semaphore("s_" + nm))

        parv = cst[:, 0:64]
        idf = cst[:, 64:192]
        idb = idb_t[:]
        G0T, B0T = 0, 16

        S.dma("sp", lambda e: e.dma_start(out=parv, in_=par[:, :]), w=["par"])
        S.dma("sp", lambda e: e.dma_start(out=idf, in_=ident_d[:, :]), w=["idf"])
        S.dve(lambda e: e.tensor_copy(out=idb, in_=idf), r=["idf"], w=["idb"])

        def ln_tile(src_ap, R, xr, xn, st, mv, rstd, tpb, hT_dst, keys, c0):
            S.dma("sp", lambda e: e.dma_start(out=xr[:R, :], in_=src_ap), w=[keys["xr"]])

            def stats(e):
                for i in range(4):
                    ins = e.bn_stats(out=st[:R, i, :], in_=xr[:R, i * 512:(i + 1) * 512])
                return ins
            S.dve(stats, r=[keys["xr"]], w=[keys["st"]])
            S.dve(lambda e: e.bn_aggr(out=mv[:R, :], in_=st[:R, :, :]), r=[keys["st"]], w=[keys["mv"]])
            S.act(lambda e: e.activation(out=rstd[:R, :], in_=mv[:R, 1:2], func=AF.Sqrt, bias=EPS, scale=1.0),
                  r=[keys["mv"]], w=[keys["rstd"]])

            S.dve(lambda e: e.reciprocal(out=rstd[:R, :], in_=rstd[:R, :]), r=[keys["rstd"]], w=[keys["rstd"]])
            S.dve(lambda e: e.tensor_scalar(out=xn[:R, :], in0=xr[:R, :], scalar1=mv[:R, 0:1], scalar2=rstd[:R, 0:1],
                                            op0=ALU.subtract, op1=ALU.mult),
                  r=[keys["xr"]], s=[keys["rstd"], keys["mv"]], w=[keys["xn"]])

            def tr(e):
                for kt in range(16):
                    bk = tpb[kt // 8]
                    ins = e.transpose(out=bk[:, (kt % 8) * 128:(kt % 8) * 128 + R], in_=xn[:R, kt * 128:(kt + 1) * 128],
                                      identity=idb[:R, :R])
                return ins
            S.pe(tr, r=[keys["xn"], "idb"], w=[keys["tp"]])

            def ev(e):
                for kt in range(16):
                    bk = tpb[kt // 8]
                    ins = e.activation(out=hT_dst(kt), in_=bk[:, (kt % 8) * 128:(kt % 8) * 128 + R], func=AF.Identity,
                                       scale=parv[:, G0T + kt:G0T + kt + 1], bias=parv[:, B0T + kt:B0T + kt + 1])
                return ins
            S.act(ev, r=[keys["tp"], "par"], w=[keys["hT"]])

        bank_bf = [b[:].bitcast(BF16) for b in banks]
        bank_f = [b[:] for b in banks]


        def V3(ap, a):
            return ap.rearrange("p (a b) -> p a b", a=a)

        def qk_pass(pfx, chunk_list, wcol0, cos_d, sin_d, out_d, hT_store=None, vmode=False):
            A.reset()
            xr_s = [A.f32(D) for _ in range(3)]
            xn_s = [A.bf16(D) for _ in range(2)]
            hT_s = [A.bf16(16 * 512).rearrange("p (k n) -> p k n", n=512) for _ in range(2)]
            st = A.f32(24).rearrange("p (a b) -> p a b", b=6)
            mv = A.f32(2)
            rstd = A.f32(1)
            W = A.bf16(16 * 2048).rearrange("p (k n) -> p k n", n=2048)
            cs_s = [(A.f32(512), A.f32(512)) for _ in range(2)]
            t_s = [(A.f32(512), A.f32(512)) for _ in range(2)]
            ko_s = [A.bf16(512) for _ in range(2)]
            vo_s = [A.bf16(1024) for _ in range(2)]
            S.barrier()
            if out_d is not None:
                S.dma("pool", lambda e: e.dma_start(out=W[:, :, 0:1024],
                                                    in_=w_in[:, wcol0:wcol0 + 1024].rearrange("(k p) n -> p k n", p=128)), w=[pfx + "W0"])
            if not vmode and out_d is not None:
                qc = wcol0
                S.dma("pool", lambda e: e.dma_start(out=W[:, :, 1024:2048],
                                                    in_=w_qkp[:, qc:qc + 1024].rearrange("(k p) n -> p k n", p=128)), w=[pfx + "W1"])
            tile_ctr = 0
            for ci, (col0, ntok, tiles) in enumerate(chunk_list):
                hT = hT_s[ci % 2]
                hk = f"{pfx}hT{ci % 2}"
                for tl, (src, R) in enumerate(tiles):
                    sl = tile_ctr % 2
                    keys = dict(xr=f"{pfx}xr{tile_ctr % 3}", xn=f"{pfx}xn{sl}", mv=pfx + "mv", rstd=pfx + "rstd", st=pfx + "st",
                                tp=f"{pfx}tp{sl}", hT=hk)
                    tpb = (bank_bf[2 * sl], bank_bf[2 * sl + 1])
                    ln_tile(src, R, xr_s[tile_ctr % 3], xn_s[sl], st, mv, rstd, tpb,
                            (lambda kt, hT=hT, tl=tl, R=R: hT[:, kt, tl * 128: tl * 128 + R]), keys, 0)
                    tile_ctr += 1
                N = ntok
                if hT_store is not None:
                    hT_store(hT, hk, col0, N)
                if out_d is None:
                    continue
                if not vmode:
                    cs = cs_s[ci % 2]
                    ck = f"{pfx}cs{ci % 2}"
                    S.dma("sp", lambda e, cs=cs, col0=col0, N=N: e.dma_start(out=cs[0][:, :N], in_=cos_d[:, col0:col0 + N]), w=[ck + "c"])
                    S.dma("sp", lambda e, cs=cs, col0=col0, N=N: e.dma_start(out=cs[1][:, :N], in_=sin_d[:, col0:col0 + N]), w=[ck + "s"])
                    for h in range(NH):
                        par2 = h % 2
                        pk, pkp = bank_f[4 + 2 * par2], bank_f[5 + 2 * par2]

                        def mm(e, h=h, hT=hT, N=N, pk=pk, pkp=pkp):
                            for v, pb in ((0, pk), (1, pkp)):
                                for kt in range(16):
                                    ins = e.matmul(pb[:, :N], lhsT=W[:, kt, v * 1024 + h * 128: v * 1024 + (h + 1) * 128],
                                                   rhs=hT[:, kt, :N], start=(kt == 0), stop=(kt == 15))
                            return ins
                        S.pe(mm, r=[hk, pfx + "W0", pfx + "W1"], w=[f"{pfx}pk{par2}"])
                        t1, t2 = t_s[par2]
                        ko = ko_s[par2]

                        def rope(e, pk=pk, pkp=pkp, cs=cs, N=N, t1=t1, t2=t2):
                            e.tensor_tensor(out=t1[:, :N], in0=pk[:, :N], in1=cs[0][:, :N], op=ALU.mult)
                            return e.tensor_tensor(out=t2[:, :N], in0=pkp[:, :N], in1=cs[1][:, :N], op=ALU.mult)
                        S.dve(rope, r=[f"{pfx}pk{par2}", ck + "c", ck + "s"], w=[f"{pfx}t{par2}"])
                        S.pool(lambda e, t1=t1, t2=t2, ko=ko, N=N: e.tensor_tensor(out=ko[:, :N], in0=t1[:, :N], in1=t2[:, :N], op=ALU.add),
                               r=[f"{pfx}t{par2}"], w=[f"{pfx}ko{par2}"])
                        S.dma("pool", lambda e, ko=ko, h=h, col0=col0, N=N: e.dma_start(out=out_d[h, :, col0:col0 + N], in_=ko[:, :N]),
                              r=[f"{pfx}ko{par2}"], w=[(pfx + "out", h, ci)])
                else:
                    for tl, (src, R) in enumerate(tiles):
                        vo = vo_s[tl % 2]
                        for half in range(2):
                            pb = bank_f[4 + half + 2 * (tl % 2)]

                            def mmv(e, hT=hT, tl=tl, R=R, half=half, pb=pb):
                                for kt in range(16):
                                    ins = e.matmul(pb[:R, :], lhsT=hT[:, kt, tl * 128: tl * 128 + R],
                                                   rhs=W[:, kt, half * 512:(half + 1) * 512], start=(kt == 0), stop=(kt == 15))
                                return ins
                            S.pe(mmv, r=[hk, pfx + "W0"], w=[f"{pfx}pv{half}{tl % 2}"])
                            S.act(lambda e, vo=vo, pb=pb, R=R, half=half: e.activation(out=vo[:R, half * 512:(half + 1) * 512], in_=pb[:R, :], func=AF.Copy),
                                  r=[f"{pfx}pv{half}{tl % 2}"], w=[f"{pfx}vo{tl % 2}{half}"])
                        p0 = col0 + tl * 128
                        S.dma("pool", lambda e, vo=vo, p0=p0, R=R: e.dma_start(out=out_d[p0:p0 + R, :], in_=vo[:R, :]),
                              r=[f"{pfx}vo{tl % 2}0", f"{pfx}vo{tl % 2}1"], w=[(pfx + "out", p0)])

        seq_chunks = [(0, NMETA, [(meta[:, :], NMETA)])] + \
                     [(NMETA + 512 * i, 512, [(xs[i * 512 + t * 128: i * 512 + (t + 1) * 128, :], 128) for t in range(4)]) for i in range(8)]
        own_chunks = [(512 * g, 512, [(xo[g * 512 + t * 128: g * 512 + (t + 1) * 128, :], 128) for t in range(4)]) for g in range(4)]
        halo_chunk = [(0, 32, [(xh[:, :], 32)])]

        if STAGE >= 1:
            qk_pass("A0", seq_chunks, 1024, cosk, sink, KT_d)
            qk_pass("A1", seq_chunks, 2048, None, None, V_d, vmode=True)

        def store_hTo(hT, hk, col0, N):
            S.dma("sp", lambda e: e.dma_start(out=hTo_d[:, :, col0:col0 + N].rearrange("k p n -> p k n"), in_=hT[:, :, :N]),
                  r=[hk], w=[("hTo_d", col0)])

        def store_hTh(hT, hk, col0, N):
            S.dma("sp", lambda e: e.dma_start(out=hTh_d[:, :, 0:32].rearrange("k p n -> p k n"), in_=hT[:, :, :32]),
                  r=[hk], w=["hTh_d"])

        if STAGE >= 2:
            qk_pass("B0", own_chunks, 0, cosq, sinq, QT_d, hT_store=store_hTo)
            qk_pass("Bh", halo_chunk, 0, None, None, None, hT_store=store_hTh)

        if STAGE >= 3:
            A.reset()
            Wc = A.bf16(16 * 3072).rearrange("p (k n) -> p k n", n=3072)
            hT_s = [A.bf16(16 * 512).rearrange("p (k n) -> p k n", n=512) for _ in range(2)]
            hTh = A.bf16(16 * 32).rearrange("p (k n) -> p k n", n=32)
            cxs = A.f32(512)
            cxhs = A.f32(8)
            zext = A.f32(4 * 130).rearrange("p (a b) -> p a b", b=130)
            c1 = A.f32(512)
            yT_s = [A.bf16(8 * 512).rearrange("p (k n) -> p k n", n=512) for _ in range(2)]
            S.barrier()
            for j in range(3):
                S.dma("pool", lambda e, j=j: e.dma_start(out=Wc[:, :, j * 1024:(j + 1) * 1024],
                                                         in_=w_in[:, 3072 + j * 1024: 4096 + j * 1024].rearrange("(k p) n -> p k n", p=128)),
                      w=[f"Wc{j}"])
            S.dma("sp", lambda e: e.dma_start(out=hTh, in_=hTh_d[:, :, :].rearrange("k p n -> p k n")), r=["hTh_d"], w=["hTh"])
            for g in range(4):
                hT = hT_s[g % 2]
                hk = f"C_hT{g % 2}"
                S.dma("sp", lambda e, hT=hT, g=g: e.dma_start(out=hT, in_=hTo_d[:, :, g * 512:(g + 1) * 512].rearrange("k p n -> p k n")),
                      r=[("hTo_d", g * 512)], w=[hk])
                yT = yT_s[g % 2]
                yk = f"C_yT{g % 2}"
                for ct in range(8):
                    b0 = 4 * (ct % 2)
                    bx, bc_, bb, bh = bank_f[b0], bank_f[b0 + 1], bank_f[b0 + 2], bank_f[b0 + 3]
                    bkk = f"C_bk{ct % 2}"

                    def mmc(e, hT=hT, ct=ct, g=g, bx=bx, bc_=bc_, bb=bb, bh=bh):
                        for j, pb in ((0, bx), (1, bc_), (2, bb)):
                            for kt in range(16):
                                ins = e.matmul(pb[:, :], lhsT=Wc[:, kt, j * 1024 + ct * 128: j * 1024 + (ct + 1) * 128],
                                               rhs=hT[:, kt, :], start=(kt == 0), stop=(kt == 15))
                        for j in (0, 1):
                            for kt in range(16):
                                ins = e.matmul(bh[:, j * 8:(j + 1) * 8], lhsT=Wc[:, kt, j * 1024 + ct * 128: j * 1024 + (ct + 1) * 128],
                                               rhs=hTh[:, kt, g * 8:(g + 1) * 8], start=(kt == 0), stop=(kt == 15))
                        return ins
                    S.pe(mmc, r=[hk, "hTh", "Wc0", "Wc1", "Wc2"], w=[bkk])

                    def cpx(e, bx=bx, bh=bh):
                        e.activation(out=cxs, in_=bx[:, :], func=AF.Copy)
                        return e.activation(out=cxhs, in_=bh[:, 0:8], func=AF.Copy)
                    S.act(cpx, r=[bkk], w=["C_cxs"])

                    def zmk(e, bc_=bc_, bh=bh):
                        e.tensor_tensor(out=zext[:, :, 2:130], in0=V3(bc_[:, :], 4), in1=V3(cxs, 4), op=ALU.mult)
                        return e.tensor_tensor(out=zext[:, :, 0:2], in0=V3(bh[:, 8:16], 4), in1=V3(cxhs, 4), op=ALU.mult)
                    S.dve(zmk, r=[bkk, "C_cxs"], w=["C_z"])
                    wc = 33
                    S.dve(lambda e, ct=ct: e.tensor_scalar(out=V3(c1, 4), in0=zext[:, :, 0:128], scalar1=parv[:, 33 + ct:34 + ct], scalar2=None,
                                                           op0=ALU.mult), r=["C_z", "par"], w=["C_c1"])
                    S.dve(lambda e, ct=ct: e.scalar_tensor_tensor(out=V3(c1, 4), in0=zext[:, :, 1:129], scalar=parv[:, 41 + ct:42 + ct],
                                                                  in1=V3(c1, 4), op0=ALU.mult, op1=ALU.add), r=["C_z", "C_c1", "par"], w=["C_c1"])
                    S.dve(lambda e, ct=ct: e.scalar_tensor_tensor(out=V3(c1, 4), in0=zext[:, :, 2:130], scalar=parv[:, 49 + ct:50 + ct],
                                                                  in1=V3(c1, 4), op0=ALU.mult, op1=ALU.add), r=["C_z", "C_c1", "par"], w=["C_c1"])
                    S.dve(lambda e, bb=bb, yT=yT, ct=ct: e.tensor_tensor(out=yT[:, ct, :], in0=bb[:, :], in1=c1, op=ALU.mult),
                          r=[bkk, "C_c1"], w=[yk])
                S.dma("sp", lambda e, yT=yT, g=g: e.dma_start(out=yT_d[:, :, g * 512:(g + 1) * 512].rearrange("k p n -> p k n"), in_=yT),
                      r=[yk], w=[("yT_d", g)])

        if STAGE >= 4:
            A.reset()
            lamt = A.f32(256)
            lprod = A.f32(128)
            lsum = A.f32(2)
            lex = A.f32(2)
            nlam = A.f32(1)
            sgs = A.f32(1)
            ones_b = A.bf16(128)
            ones_f = A.f32(128)
            mk = A.bf16(8 * 512).rearrange("p (k n) -> p k n", n=512)
            QT_s = [A.bf16(512) for _ in range(2)]
            KT_s = [A.bf16(L) for _ in range(2)]
            V_s = [A.bf16(33 * 128).rearrange("p (k n) -> p k n", n=128) for _ in range(2)]
            E_s = [[A.bf16(512) for _ in range(2)] for _ in range(2)]
            rz = [A.f32(512) for _ in range(2)]
            tt_ = [A.f32(512) for _ in range(2)]
            ocomb = A.f32(512)
            sq = A.f32(512)
            rst = A.f32(512)
            on_s = [A.bf16(512) for _ in range(2)]
            S.barrier()
            S.dma("sp", lambda e: e.dma_start(out=lamt, in_=lamv[:, :]), w=["lamt"])
            S.dma("sp", lambda e: e.dma_start(out=mk, in_=maskd[:, :, :]), w=["mk"])
            S.pool(lambda e: e.memset(ones_b, 1.0), w=["ones_b"])
            S.pool(lambda e: e.memset(ones_f, 1.0), w=["ones_f"])
            S.dve(lambda e: e.tensor_tensor(out=lprod, in0=lamt[:, 0:128], in1=lamt[:, 128:256], op=ALU.mult),
                  r=["lamt"], w=["lprod"])
            S.dve(lambda e: e.tensor_reduce(out=lsum, in_=V3(lprod, 2), axis=AX.X, op=ALU.add), r=["lprod"], w=["lsum"])
            S.act(lambda e: e.activation(out=lex, in_=lsum, func=AF.Exp), r=["lsum"], w=["lex"])
            S.dve(lambda e: e.tensor_tensor(out=nlam, in0=lex[:, 1:2], in1=lex[:, 0:1], op=ALU.subtract), r=["lex"], w=["nlam"])
            S.pool(lambda e: e.tensor_scalar(out=nlam, in0=nlam, scalar1=-LAM_INIT, scalar2=1.0, op0=ALU.add, op1=ALU.mult), r=["nlam"], w=["nlam"])
            S.pool(lambda e: e.tensor_scalar(out=sgs, in0=parv[:, 32:33], scalar1=1.0 - LAM_INIT, scalar2=1.0, op0=ALU.mult, op1=ALU.mult),
                   r=["par"], w=["sgs"])
            heads = [(g, h) for g in range(4) for h in range(NH)]

            def slot_of(n):
                sl = n % 2
                return sl, QT_s[sl], KT_s[sl], V_s[sl], f"D_Q{sl}", f"D_K{sl}", f"D_V{sl}"

            def issue_loads(n):
                g, h = heads[n]
                ntile = 8 * g + 8
                nk = NMETA + 128 * ntile
                sl, QT, KT, Vh, kq, kk, kv = slot_of(n)
                S.dma("sp", lambda e: e.dma_start(out=QT, in_=QT_d[h, :, g * 512:(g + 1) * 512]), r=[("B0out", h, g)], w=[kq])
                S.dma("sp", lambda e: e.dma_start(out=KT[:, :nk], in_=KT_d[h, :, 0:nk]), r=[("A0out", h, c) for c in range(9)], w=[kk])
                S.dma("sp", lambda e: e.dma_start(out=Vh[:NMETA, 0, :], in_=V_d[0:NMETA, h * 128:(h + 1) * 128]), r=[("A1out", 0)], w=[kv + "m"])
                S.dma("sp", lambda e: e.dma_start(
                    out=Vh[:, 1:1 + ntile, :], in_=V_d[NMETA:NMETA + 128 * ntile, h * 128:(h + 1) * 128].rearrange("(t p) n -> p t n", p=128)),
                    r=[("A1out", NMETA + 128 * t) for t in range(32)], w=[kv])

            def s_part(n, t):
                g, h = heads[n]
                sl, QT, KT, Vh, kq, kk, kv = slot_of(n)
                R = NMETA if t == 0 else 128
                kc0 = 0 if t == 0 else NMETA + 128 * (t - 1)
                tp_ = t % 2
                jj = t - 1 - 8 * g
                masked = (t >= 1 and jj >= 0)
                for m in range(2):
                    sb = bank_f[2 * tp_ + m]
                    Em = E_s[tp_][m]

                    def smm(e, sb=sb, m=m):
                        ins = e.matmul(sb[:R, :], lhsT=KT[m * 64:(m + 1) * 64, kc0:kc0 + R], rhs=QT[m * 64:(m + 1) * 64, :],
                                       start=True, stop=(not masked))
                        if masked:
                            ins = e.matmul(sb[:R, :], lhsT=idb, rhs=mk[:, jj, :], start=False, stop=True)
                        return ins
                    S.pe(smm, r=[kq, kk, "mk", "idb"], w=[f"D_S{tp_}{m}"])
                    S.act(lambda e, sb=sb, Em=Em: e.activation(out=Em[:R, :], in_=sb[:R, :], func=AF.Exp, scale=0.125),
                          r=[f"D_S{tp_}{m}"], w=[f"D_E{tp_}{m}"])

            def av_part(n, t):
                g, h = heads[n]
                ntile = 8 * g + 8
                sl, QT, KT, Vh, kq, kk, kv = slot_of(n)
                R = NMETA if t == 0 else 128
                tp_ = t % 2
                for m in range(2):
                    Em = E_s[tp_][m]

                    def av(e, Em=Em, m=m):
                        e.matmul(bank_f[4 + m][:, :], lhsT=Vh[:R, t, :], rhs=Em[:R, :], start=(t == 0), stop=(t == ntile))
                        return e.matmul(bank_f[6 + m][:, :], lhsT=ones_b[:R, :], rhs=Em[:R, :], start=(t == 0), stop=(t == ntile))
                    S.pe(av, r=[f"D_E{tp_}{m}", kv, kv + "m", "ones_b"], w=[f"D_O{m}"])

            def fin_a(n):
                for m in range(2):
                    S.dve(lambda e, m=m: e.reciprocal(out=rz[m], in_=bank_f[6 + m][:, :]), r=[f"D_O{m}"], w=[f"D_rz{m}"])
                    S.dve(lambda e, m=m: e.tensor_tensor(out=tt_[m], in0=bank_f[4 + m][:, :], in1=rz[m], op=ALU.mult),
                          r=[f"D_O{m}", f"D_rz{m}"], w=[f"D_t{m}"])

            def fin_b(n):
                g, h = heads[n]
                sl = n % 2
                S.dve(lambda e: e.scalar_tensor_tensor(out=ocomb, in0=tt_[1], scalar=nlam[:, 0:1], in1=tt_[0], op0=ALU.mult, op1=ALU.add),
                      r=["D_t0", "D_t1", "nlam"], w=["D_oc"])
                S.pool(lambda e: e.tensor_tensor(out=sq, in0=ocomb, in1=ocomb, op=ALU.mult), r=["D_oc"], w=["D_sq"])
                S.pe(lambda e: e.matmul(bank_f[2][:, :], lhsT=ones_f, rhs=sq, start=True, stop=True), r=["D_sq", "ones_f"], w=["D_S10"])
                S.act(lambda e: e.activation(out=rst, in_=bank_f[2][:, :], func=AF.Sqrt, scale=1.0 / 128.0, bias=EPS), r=["D_S10"], w=["D_rst"])
                S.dve(lambda e: e.reciprocal(out=rst, in_=rst), r=["D_rst"], w=["D_rst"])
                S.dve(lambda e: e.tensor_tensor(out=ocomb, in0=ocomb, in1=rst, op=ALU.mult), r=["D_oc", "D_rst"], w=["D_oc"])
                on = on_s[sl]
                S.pool(lambda e: e.tensor_scalar(out=on, in0=ocomb, scalar1=sgs[:, 0:1], scalar2=1.0, op0=ALU.mult, op1=ALU.mult),
                       r=["D_oc", "sgs"], w=[f"D_on{sl}"])
                S.dma("sp", lambda e: e.dma_start(out=OnT_d[h, :, g * 512:(g + 1) * 512], in_=on), r=[f"D_on{sl}"], w=[("OnT_d", h, g)])

            issue_loads(0)
            for n in range(len(heads)):
                if n + 1 < len(heads):
                    issue_loads(n + 1)
                nt = 8 * heads[n][0] + 8
                s_part(n, 0)
                for t in range(nt + 1):
                    if t + 1 <= nt:
                        s_part(n, t + 1)
                    av_part(n, t)
                    if t == 2 and n >= 1:
                        fin_b(n - 1)
                fin_a(n)
            fin_b(len(heads) - 1)

        if STAGE >= 5:
            A.reset()
            hT_a = A.bf16(16 * 2048).rearrange("p (k n) -> p k n", n=2048)
            on_a = A.bf16(8 * 2048).rearrange("p (k n) -> p k n", n=2048)
            y_a = A.bf16(8 * 2048).rearrange("p (k n) -> p k n", n=2048)
            Wg_s = [A.bf16(16 * 512).rearrange("p (k n) -> p k n", n=512) for _ in range(2)]
            Wp_s = [A.bf16(8 * 512).rearrange("p (k n) -> p k n", n=512) for _ in range(2)]
            mT_s = [A.bf16(2 * 512).rearrange("p (k n) -> p k n", n=512) for _ in range(2)]
            sg_t = [A.f32(512) for _ in range(2)]
            t12 = [A.f32(512) for _ in range(2)]
            S.barrier()
            for g in range(4):
                S.dma("sp", lambda e, g=g: e.dma_start(out=hT_a[:, :, g * 512:(g + 1) * 512], in_=hTo_d[:, :, g * 512:(g + 1) * 512].rearrange("k p n -> p k n")),
                      r=[("hTo_d", g * 512)], w=[f"E_hT{g}"])
                S.dma("sp", lambda e, g=g: e.dma_start(out=on_a[:, :, g * 512:(g + 1) * 512], in_=OnT_d[:, :, g * 512:(g + 1) * 512].rearrange("k p n -> p k n")),
                      r=[("OnT_d", h, g) for h in range(NH)], w=[f"E_on{g}"])
                S.dma("sp", lambda e, g=g: e.dma_start(out=y_a[:, :, g * 512:(g + 1) * 512], in_=yT_d[:, :, g * 512:(g + 1) * 512].rearrange("k p n -> p k n")),
                      r=[("yT_d", g)], w=[f"E_y{g}"])
            octr = 0
            for c8 in range(8):
                ws = c8 % 2
                Wg, Wp = Wg_s[ws], Wp_s[ws]
                S.dma("pool", lambda e, Wg=Wg, c8=c8: e.dma_start(out=Wg[:, :, 0:256], in_=w_in[:, 6144 + c8 * 256: 6144 + (c8 + 1) * 256].rearrange("(k p) n -> p k n", p=128)), w=[f"E_Wga{ws}"])
                S.dma("pool", lambda e, Wg=Wg, c8=c8: e.dma_start(out=Wg[:, :, 256:512], in_=w_in[:, 8192 + c8 * 256: 8192 + (c8 + 1) * 256].rearrange("(k p) n -> p k n", p=128)), w=[f"E_Wgc{ws}"])
                S.dma("pool", lambda e, Wp=Wp, c8=c8: e.dma_start(out=Wp[:, :, 0:256], in_=w_pa[:, c8 * 256:(c8 + 1) * 256].rearrange("(k p) n -> p k n", p=128)), w=[f"E_Wpa{ws}"])
                S.dma("pool", lambda e, Wp=Wp, c8=c8: e.dma_start(out=Wp[:, :, 256:512], in_=w_pc[:, c8 * 256:(c8 + 1) * 256].rearrange("(k p) n -> p k n", p=128)), w=[f"E_Wpc{ws}"])
                for g in range(4):
                    mT = mT_s[octr % 2]
                    mk_ = f"E_mT{octr % 2}"
                    octr += 1
                    for k2 in range(2):
                        b0 = 4 * (k2 % 2)
                        bk = f"E_bk{k2 % 2}"

                        def mmg(e, Wg=Wg, Wp=Wp, g=g, k2=k2, b0=b0):
                            for j in range(2):
                                for k in range(16):
                                    e.matmul(bank_f[b0 + j][:, :], lhsT=Wg[:, k, j * 256 + k2 * 128: j * 256 + (k2 + 1) * 128],
                                             rhs=hT_a[:, k, g * 512:(g + 1) * 512], start=(k == 0), stop=(k == 15))
                            for k in range(8):
                                e.matmul(bank_f[b0 + 2][:, :], lhsT=Wp[:, k, k2 * 128:(k2 + 1) * 128], rhs=on_a[:, k, g * 512:(g + 1) * 512],
                                         start=(k == 0), stop=(k == 7))
                            for k in range(8):
                                ins = e.matmul(bank_f[b0 + 3][:, :], lhsT=Wp[:, k, 256 + k2 * 128: 256 + (k2 + 1) * 128],
                                               rhs=y_a[:, k, g * 512:(g + 1) * 512], start=(k == 0), stop=(k == 7))
                            return ins
                        S.pe(mmg, r=[f"E_Wga{ws}", f"E_Wgc{ws}", f"E_Wpa{ws}", f"E_Wpc{ws}", f"E_hT{g}", f"E_on{g}", f"E_y{g}"], w=[bk])
                        for j in range(2):
                            S.act(lambda e, j=j, b0=b0: e.activation(out=sg_t[j], in_=bank_f[b0 + j][:, :], func=AF.Sigmoid), r=[bk], w=[f"E_sg{j}"])
                            S.dve(lambda e, j=j, b0=b0: e.tensor_tensor(out=t12[j], in0=bank_f[b0 + 2 + j][:, :], in1=sg_t[j], op=ALU.mult),
                                  r=[bk, f"E_sg{j}"], w=[f"E_t{j}"])
                        S.pool(lambda e, mT=mT, k2=k2: e.tensor_tensor(out=mT[:, k2, :], in0=t12[0], in1=t12[1], op=ALU.add), r=["E_t0", "E_t1"], w=[mk_])
                    S.dma("sp", lambda e, mT=mT, g=g, c8=c8: e.dma_start(
                        out=mT_d[2 * c8:2 * c8 + 2, :, g * 512:(g + 1) * 512].rearrange("k p n -> p k n"), in_=mT),
                        r=[mk_], w=[("mT_d", g, c8)])

        if STAGE >= 6:
            A.reset()
            Wo = A.bf16(16 * 2048).rearrange("p (k n) -> p k n", n=2048)
            mt_s = [A.bf16(16 * 128).rearrange("p (k n) -> p k n", n=128) for _ in range(2)]
            xr_s = [A.f32(D) for _ in range(2)]
            r_s = [A.f32(D) for _ in range(2)]
            G0, B0, G1, B1 = A.f32(D), A.f32(D), A.f32(D), A.f32(D)
            h1Tf = A.f32(16 * 128).rearrange("p (k n) -> p k n", n=128)
            h1Tb_s = [A.bf16(16 * 512).rearrange("p (k n) -> p k n", n=512) for _ in range(2)]
            wr = A.f32(16 * 64).rearrange("p (k n) -> p k n", n=64)
            rbt = A.f32(64)
            st = A.f32(24).rearrange("p (a b) -> p a b", b=6)
            mv = A.f32(2)
            rstd = A.f32(1)
            sgm, sel, eq, sel2, selm, msk, smm = [A.f32(64) for _ in range(7)]
            m1, m2, gs, top8, gm, pen, top8e = [A.f32(8) for _ in range(7)]
            ssum = A.f32(1)
            S.barrier()
            S.dma("pool", lambda e: e.dma_start(out=Wo, in_=w_out[:, :].rearrange("(k p) n -> p k n", p=128)), w=["F_Wo"])
            for i, t_ in enumerate((G0, B0, G1, B1)):
                S.dma("sp", lambda e, i=i, t_=t_: e.dma_start(out=t_, in_=bcd[i, :, :]), w=[f"F_bc{i}"])
            S.dma("sp", lambda e: e.dma_start(out=wr, in_=w_router[:, :].rearrange("(k p) n -> p k n", p=128)), w=["F_wr"])
            S.dma("sp", lambda e: e.dma_start(out=rbt, in_=rbd[:, :]), w=["F_rb"])
            S.pool(lambda e: e.memset(ewt[:, :], 1.0), w=["ew"])

            def ln_stats(src, keyin, pf):
                def stats(e):
                    for i in range(4):
                        ins = e.bn_stats(out=st[:, i, :], in_=src[:, i * 512:(i + 1) * 512])
                    return ins
                S.dve(stats, r=[keyin], w=[pf + "st"])
                S.dve(lambda e: e.bn_aggr(out=mv, in_=st), r=[pf + "st"], w=[pf + "mv"])
                S.act(lambda e: e.activation(out=rstd, in_=mv[:, 1:2], func=AF.Sqrt, bias=EPS, scale=1.0), r=[pf + "mv"], w=[pf + "rstd"])
                S.dve(lambda e: e.reciprocal(out=rstd, in_=rstd), r=[pf + "rstd"], w=[pf + "rstd"])
                S.dve(lambda e: e.tensor_scalar(out=src, in0=src, scalar1=mv[:, 0:1], scalar2=rstd[:, 0:1], op0=ALU.subtract, op1=ALU.mult),
                      r=[keyin], s=[pf + "rstd", pf + "mv"], w=[keyin])

            for tt in range(16):
                sl = tt % 2
                mt, xr, rr, h1Tb = mt_s[sl], xr_s[sl], r_s[sl], h1Tb_s[(tt // 4) % 2]
                g4, t4 = tt // 4, tt % 4
                S.dma("sp", lambda e, mt=mt, tt=tt: e.dma_start(out=mt, in_=mT_d[:, :, tt * 128:(tt + 1) * 128].rearrange("k p n -> p k n")),
                      r=[("mT_d", tt // 4, c8) for c8 in range(8)], w=[f"F_mt{sl}"])
                S.dma("sp", lambda e, xr=xr, tt=tt: e.dma_start(out=xr, in_=xo[tt * 128:(tt + 1) * 128, :]), w=[f"F_xr{sl}"])

                def mmo(e, mt=mt):
                    for cc in range(4):
                        for k in range(16):
                            ins = e.matmul(bank_f[cc][:, :], lhsT=mt[:, k, :], rhs=Wo[:, k, cc * 512:(cc + 1) * 512], start=(k == 0), stop=(k == 15))
                    return ins
                S.pe(mmo, r=[f"F_mt{sl}", "F_Wo"], w=["F_po"])
                ln_stats(xr, f"F_xr{sl}", "F_a")
                S.dve(lambda e, xr=xr: e.tensor_tensor(out=xr, in0=xr, in1=G0, op=ALU.mult), r=[f"F_xr{sl}", "F_bc0"], w=[f"F_xr{sl}"])
                S.pool(lambda e, xr=xr: e.tensor_tensor(out=xr, in0=xr, in1=B0, op=ALU.add), r=[f"F_xr{sl}", "F_bc1"], w=[f"F_xr{sl}"])

                def resid(e, xr=xr, rr=rr):
                    for cc in range(4):
                        ins = e.scalar_tensor_tensor(out=rr[:, cc * 512:(cc + 1) * 512], in0=xr[:, cc * 512:(cc + 1) * 512], scalar=ALPHA,
                                                     in1=bank_f[cc][:, :], op0=ALU.mult, op1=ALU.add)
                    return ins
                S.dve(resid, r=[f"F_xr{sl}", "F_po"], w=[f"F_r{sl}"])
                ln_stats(rr, f"F_r{sl}", "F_b")
                S.dve(lambda e, rr=rr: e.tensor_tensor(out=rr, in0=rr, in1=G1, op=ALU.mult), r=[f"F_r{sl}", "F_bc2"], w=[f"F_r{sl}"])
                S.pool(lambda e, rr=rr: e.tensor_tensor(out=rr, in0=rr, in1=B1, op=ALU.add), r=[f"F_r{sl}", "F_bc3"], w=[f"F_r{sl}"])
                S.dma("sp", lambda e, rr=rr, tt=tt: e.dma_start(out=h1_d[tt * 128:(tt + 1) * 128, :], in_=rr), r=[f"F_r{sl}"], w=[("h1_d", tt)])

                if SUB6 >= 1:
                    def trf(e, rr=rr):
                        for k in range(16):
                            ins = e.matmul(bank_f[4 + k // 4][:, (k % 4) * 128:(k % 4 + 1) * 128], lhsT=rr[:, k * 128:(k + 1) * 128], rhs=idf, start=True, stop=True)
                        return ins
                    S.pe(trf, r=[f"F_r{sl}", "idf"], w=["F_pt"])

                    def evb(e, h1Tb=h1Tb, t4=t4):
                        for q in range(4):
                            ins = e.activation(out=h1Tb[:, 4 * q:4 * q + 4, t4 * 128:(t4 + 1) * 128], in_=V3(bank_f[4 + q][:, :], 4), func=AF.Copy)
                        return ins
                    if SUB6B & 1:
                        S.act(evb, r=["F_pt"], w=[f"F_h1Tb{g4 % 2}"])

                    def evf(e):
                        for q in range(4):
                            ins = e.tensor_copy(out=h1Tf[:, 4 * q:4 * q + 4, :], in_=V3(bank_f[4 + q][:, :], 4))
                        return ins
                    if SUB6B & 2:
                        S.dve(evf, r=["F_pt", f"F_h1Tb{g4 % 2}"], w=["F_h1Tf"])
                    if (SUB6B & 4) and t4 == 3:
                        S.dma("sp", lambda e, h1Tb=h1Tb, g4=g4: e.dma_start(out=h1T_d[:, :, g4 * 512:(g4 + 1) * 512].rearrange("k p n -> p k n"), in_=h1Tb),
                              r=[f"F_h1Tb{g4 % 2}"], w=[("h1T_d", g4)])

                if SUB6 >= 2:
                    def mmr(e):
                        for k in range(16):
                            ins = e.matmul(bank_f[0][:, 0:64], lhsT=h1Tf[:, k, :], rhs=wr[:, k, :], start=(k == 0), stop=(k == 15))
                        return ins
                    S.pe(mmr, r=["F_h1Tf", "F_wr"], w=["F_po"])
                if SUB6 >= 3:
                    S.act(lambda e: e.activation(out=sgm, in_=bank_f[0][:, 0:64], func=AF.Sigmoid), r=["F_po"], w=["R_sgm"])
                    S.dve(lambda e: e.tensor_tensor(out=sel, in0=sgm, in1=rbt, op=ALU.add), r=["R_sgm", "F_rb"], w=["R_sel"])
                    S.dve(lambda e: e.tensor_reduce(out=m1, in_=V3(sel, 8), axis=AX.X, op=ALU.max), r=["R_sel"], w=["R_m1"])
                    S.dve(lambda e: e.tensor_tensor(out=V3(eq, 8), in0=V3(sel, 8), in1=m1.unsqueeze(2).to_broadcast([128, 8, 8]), op=ALU.is_equal),
                          r=["R_sel", "R_m1"], w=["R_eq"])
                    S.dve(lambda e: e.scalar_tensor_tensor(out=sel2, in0=eq, scalar=-1e30, in1=sel, op0=ALU.mult, op1=ALU.add),
                          r=["R_eq", "R_sel"], w=["R_sel2"])
                    S.dve(lambda e: e.tensor_reduce(out=m2, in_=V3(sel2, 8), axis=AX.X, op=ALU.max), r=["R_sel2"], w=["R_m2"])
                    S.dve(lambda e: e.tensor_tensor(out=gs, in0=m1, in1=m2, op=ALU.add), r=["R_m1", "R_m2"], w=["R_gs"])
                    S.dve(lambda e: e.max(out=top8, in_=gs), r=["R_gs"], w=["R_top8"])
                    S.dve(lambda e: e.tensor_tensor(out=gm, in0=gs, in1=top8[:, 3:4].to_broadcast([128, 8]), op=ALU.is_ge), r=["R_gs", "R_top8"], w=["R_gm"])
                    S.dve(lambda e: e.tensor_scalar(out=pen, in0=gm, scalar1=-1.0, scalar2=1e30, op0=ALU.add, op1=ALU.mult), r=["R_gm"], w=["R_pen"])
                    S.dve(lambda e: e.tensor_tensor(out=V3(selm, 8), in0=V3(sel, 8), in1=pen.unsqueeze(2).to_broadcast([128, 8, 8]), op=ALU.add),
                          r=["R_sel", "R_pen"], w=["R_selm"])
                    S.dve(lambda e: e.max(out=top8e, in_=selm), r=["R_selm"], w=["R_top8e"])
                    S.dve(lambda e: e.tensor_tensor(out=msk, in0=selm, in1=top8e[:, 7:8].to_broadcast([128, 64]), op=ALU.is_ge),
                          r=["R_selm", "R_top8e"], w=["R_msk"])
                    S.dve(lambda e: e.tensor_tensor(out=smm, in0=sgm, in1=msk, op=ALU.mult), r=["R_sgm", "R_msk"], w=["R_smm"])
                    S.dve(lambda e: e.tensor_reduce(out=ssum, in_=smm, axis=AX.X, op=ALU.add), r=["R_smm"], w=["R_ssum"])
                    S.dve(lambda e: e.reciprocal(out=ssum, in_=ssum), r=["R_ssum"], w=["R_ssum"])
                    S.dve(lambda e, tt=tt: e.scalar_tensor_tensor(out=ewt[:, tt * 65: tt * 65 + 64], in0=smm, scalar=2.5,
                                                                  in1=ssum[:, 0:1].to_broadcast([128, 64]), op0=ALU.mult, op1=ALU.mult),
                          r=["R_smm", "R_ssum"], w=["ew"])

            if DEBUG:
                S.dma("sp", lambda e: e.dma_start(out=ew_d[:, :], in_=ewt), r=["ew"], w=["ew_d"])

        if STAGE >= 7:
            experts = [(w_eg[e_], w_eu[e_], w_ed[e_]) for e_ in range(NE)] + [(w_sg, w_su, w_sd)]
            def moe_half(hf):
                A.reset()
                acc = A.f32(8 * D).rearrange("p (t n) -> p t n", n=D)
                h1T = A.bf16(16 * 1024).rearrange("p (k n) -> p k n", n=1024)
                Wgu_s = [A.bf16(16 * 1024).rearrange("p (k n) -> p k n", n=1024) for _ in range(2)]
                Wd_s = [A.bf16(4 * 2048).rearrange("p (k n) -> p k n", n=2048) for _ in range(2)]
                hid = A.bf16(4 * 1024).rearrange("p (k n) -> p k n", n=1024)
                sgt_one = A.f32(512)
                sgt = [sgt_one, sgt_one]
                pf = f"M{hf}"
                S.barrier()
                for t in range(8):
                    S.dma("sp", lambda e, t=t: e.dma_start(out=acc[:, t, :], in_=h1_d[(8 * hf + t) * 128:(8 * hf + t + 1) * 128, :]),
                          r=[("h1_d", 8 * hf + t)], w=[(pf + "acc", t)])
                    S.pool(lambda e, t=t: e.tensor_scalar(out=acc[:, t, :], in0=acc[:, t, :], scalar1=ALPHA, scalar2=1.0, op0=ALU.mult, op1=ALU.mult),
                           r=[(pf + "acc", t)], w=[(pf + "acc", t)])
                S.dma("sp", lambda e: e.dma_start(out=h1T, in_=h1T_d[:, :, hf * 1024:(hf + 1) * 1024].rearrange("k p n -> p k n")),
                      r=[("h1T_d", 2 * hf), ("h1T_d", 2 * hf + 1)], w=[pf + "h1T"])
                for ei, (wg_, wu_, wd_) in enumerate(experts[:NEXP]):
                    ws = ei % 2
                    Wgu, Wd = Wgu_s[ws], Wd_s[ws]
                    S.dma("pool", lambda e, Wgu=Wgu, wg_=wg_: e.dma_start(out=Wgu[:, :, 0:512], in_=wg_.rearrange("(k p) n -> p k n", p=128)), w=[f"{pf}Wg{ws}"])
                    S.dma("pool", lambda e, Wgu=Wgu, wu_=wu_: e.dma_start(out=Wgu[:, :, 512:1024], in_=wu_.rearrange("(k p) n -> p k n", p=128)), w=[f"{pf}Wu{ws}"])
                    S.dma("pool", lambda e, Wd=Wd, wd_=wd_: e.dma_start(out=Wd, in_=wd_.rearrange("(k p) n -> p k n", p=128)), w=[f"{pf}Wd{ws}"])
                    for m in range(4):
                        for n in range(2):
                            b0 = 2 * ((2 * m + n) % 2)
                            bk = f"{pf}gu{b0}"

                            def mmgu(e, Wgu=Wgu, m=m, n=n, b0=b0):
                                for j in range(2):
                                    for k in range(16):
                                        ins = e.matmul(bank_f[b0 + j][:, :], lhsT=Wgu[:, k, j * 512 + m * 128: j * 512 + (m + 1) * 128],
                                                       rhs=h1T[:, k, n * 512:(n + 1) * 512], start=(k == 0), stop=(k == 15))
                                return ins
                            S.pe(mmgu, r=[f"{pf}Wg{ws}", f"{pf}Wu{ws}", pf + "h1T"], w=[bk])
                            sg_ = sgt[(2 * m + n) % 2]
                            S.act(lambda e, sg_=sg_, b0=b0: e.activation(out=sg_, in_=bank_f[b0][:, :], func=AF.Silu), r=[bk], w=[f"{pf}sg"])
                            S.dve(lambda e, sg_=sg_, b0=b0, m=m, n=n: e.tensor_tensor(out=hid[:, m, n * 512:(n + 1) * 512], in0=bank_f[b0 + 1][:, :],
                                                                                      in1=sg_, op=ALU.mult), r=[bk, f"{pf}sg"], w=[(pf + "hid", m, n)])
                    for t in range(8):
                        for cc in range(4):
                            yb = 4 + cc
                            S.pe(lambda e, t=t, cc=cc, yb=yb, Wd=Wd: [e.matmul(bank_f[yb][:, :], lhsT=hid[:, m, t * 128:(t + 1) * 128],
                                                                              rhs=Wd[:, m, cc * 512:(cc + 1) * 512], start=(m == 0), stop=(m == 3))
                                                                     for m in range(4)][-1],
                                 r=[(pf + "hid", m, t // 4) for m in range(4)] + [f"{pf}Wd{ws}"], w=[f"{pf}y{cc}"])
                            S.dve(lambda e, t=t, cc=cc, yb=yb, ei=ei: e.scalar_tensor_tensor(
                                out=acc[:, t, cc * 512:(cc + 1) * 512], in0=bank_f[yb][:, :],
                                scalar=ewt[:, (8 * hf + t) * 65 + ei:(8 * hf + t) * 65 + ei + 1], in1=acc[:, t, cc * 512:(cc + 1) * 512],
                                op0=ALU.mult, op1=ALU.add), r=[f"{pf}y{cc}", (pf + "acc", t), "ew"], w=[(pf + "acc", t)])
                S.barrier()
                A.off = 8 * D
                G2, B2 = A.f32(D), A.f32(D)
                st = A.f32(24).rearrange("p (a b) -> p a b", b=6)
                mv = A.f32(2)
                rstd = A.f32(1)
                S.dma("sp", lambda e: e.dma_start(out=G2, in_=bcd[4, :, :]), w=[pf + "G2"])
                S.dma("sp", lambda e: e.dma_start(out=B2, in_=bcd[5, :, :]), w=[pf + "B2"])
                for t in range(8):
                    src = acc[:, t, :]
                    kin = (pf + "acc", t)

                    def stats(e, src=src):
                        for i in range(4):
                            ins = e.bn_stats(out=st[:, i, :], in_=src[:, i * 512:(i + 1) * 512])
                        return ins
                    S.dve(stats, r=[kin], w=[pf + "st"])
                    S.dve(lambda e: e.bn_aggr(out=mv, in_=st), r=[pf + "st"], w=[pf + "mv"])
                    S.act(lambda e: e.activation(out=rstd, in_=mv[:, 1:2], func=AF.Sqrt, bias=EPS, scale=1.0), r=[pf + "mv"], w=[pf + "rstd"])
                    S.dve(lambda e: e.reciprocal(out=rstd, in_=rstd), r=[pf + "rstd"], w=[pf + "rstd"])
                    S.dve(lambda e, src=src: e.tensor_scalar(out=src, in0=src, scalar1=mv[:, 0:1], scalar2=rstd[:, 0:1], op0=ALU.subtract, op1=ALU.mult),
                          r=[kin], s=[pf + "rstd", pf + "mv"], w=[kin])
                    S.dve(lambda e, src=src: e.tensor_tensor(out=src, in0=src, in1=G2, op=ALU.mult), r=[kin, pf + "G2"], w=[kin])
                    S.pool(lambda e, src=src: e.tensor_tensor(out=src, in0=src, in1=B2, op=ALU.add), r=[kin, pf + "B2"], w=[kin])
                    S.dma("sp", lambda e, src=src, t=t: e.dma_start(out=out[(8 * hf + t) * 128:(8 * hf + t + 1) * 128, :], in_=src),
                          r=[kin], w=[("out", hf, t)])

            if STAGE >= 7:
                for hf_ in range(2):
                    moe_half(hf_)

        S.barrier()
        S.emit(nc, sems)
    return nc


def _rope_tables():
    half = 32
    inv = (10000.0 ** (-np.arange(half, dtype=np.float32) / half)).astype(np.float32)
    pos = np.arange(L, dtype=np.float32)
    ang = pos[None, :] * inv[:, None]
    cos = np.cos(ang).astype(np.float32)
    sin = np.sin(ang).astype(np.float32)
    cos64 = np.concatenate([cos, cos], 0)
    sin64 = np.concatenate([-sin, sin], 0)
    return np.concatenate([cos64, cos64], 0), np.concatenate([sin64, sin64], 0)


def _perm_cols():
    idx = np.arange(1024).reshape(NH, 2, 2, 32)[:, :, ::-1, :].reshape(1024)
    return idx


_NC_CACHE = {}
_DECL = []


def kernel(**inputs):
    f = lambda k: np.asarray(inputs[k], np.float32)
    x = f("x")
    w_in = np.ascontiguousarray(f("w_in")[0])
    pidx = _perm_cols()
    w_qkp = np.ascontiguousarray(np.concatenate([w_in[:, 0:1024][:, pidx], w_in[:, 1024:2048][:, pidx]], axis=1))
    cosk, sink = _rope_tables()
    par = np.zeros((128, 64), np.float32)
    par[:, 0:16] = f("ln0_g").reshape(16, 128).T
    par[:, 16:32] = f("ln0_b").reshape(16, 128).T
    par[:, 32] = f("subln_g")[0]
    wc = f("w_conv")[0]
    for k in range(3):
        par[:, 33 + 8 * k: 41 + 8 * k] = wc[k].reshape(8, 128).T
    ident = np.eye(128, dtype=np.float32)
    meta = np.ascontiguousarray(f("meta_tokens"))
    lamv = np.ascontiguousarray(np.broadcast_to(np.concatenate(
        [f("lambda_q1")[0], f("lambda_q2")[0], f("lambda_k1")[0], f("lambda_k2")[0]])[None, :], (128, 256)))
    rbd = np.ascontiguousarray(np.broadcast_to(f("router_bias")[0][None, :], (128, NE)))
    bcd = np.ascontiguousarray(np.stack([np.broadcast_to(v[None, :], (128, D)) for v in
                                         (f("ln0_g"), f("ln0_b"), f("ln1_g")[0], f("ln1_b")[0], f("ln2_g")[0], f("ln2_b")[0])]))
    shared = dict(meta=meta, w_in=w_in, w_qkp=w_qkp, par=par, ident=ident, cosk=cosk, sink=sink, lamv=lamv, rbd=rbd, bcd=bcd,
                  w_pa=np.ascontiguousarray(f("w_proj_attn")[0]), w_pc=np.ascontiguousarray(f("w_proj_conv")[0]),
                  w_out=np.ascontiguousarray(f("w_out")[0]), w_router=np.ascontiguousarray(f("w_router")[0]),
                  w_eg=np.ascontiguousarray(f("w_exp_gate")[0]), w_eu=np.ascontiguousarray(f("w_exp_up")[0]),
                  w_ed=np.ascontiguousarray(f("w_exp_down")[0]), w_sg=np.ascontiguousarray(f("w_sh_gate")[0]),
                  w_su=np.ascontiguousarray(f("w_sh_up")[0]), w_sd=np.ascontiguousarray(f("w_sh_down")[0]))
    if "nc" not in _NC_CACHE:
        _NC_CACHE["nc"] = build_program()
    nc = _NC_CACHE["nc"]
    in_maps = []
    blocks_of = []
    for c in range(8):
        b, p = c // 2, c % 2
        blks = [8 * g + OFFS[p][i] for g in range(4) for i in range(4)]
        blocks_of.append(blks)
        xo = np.ascontiguousarray(np.concatenate([x[b, 128 * j:128 * (j + 1)] for j in blks], 0))
        seqx = x[b]
        xh = np.ascontiguousarray(np.concatenate([(meta[14:16] if j == 0 else seqx[128 * j - 2:128 * j]) for j in blks], 0))
        pos = np.concatenate([NMETA + 128 * j + np.arange(128) for j in blks])
        mask = np.zeros((128, 8, 4, 128), np.float32)
        r = np.arange(128)
        diag = ((r[:, None] // 64) <= (r[None, :] // 64)).astype(np.float32)
        for o in range(8):
            for i in range(4):
                oo = OFFS[p][i]
                if o < oo:
                    mask[:, o, i, :] = 1.0
                elif o == oo:
                    mask[:, o, i, :] = diag
        m = dict(shared)
        m.update(xs=np.ascontiguousarray(x[b]), xo=xo, xh=xh, cosq=np.ascontiguousarray(cosk[:, pos]),
                 sinq=np.ascontiguousarray(sink[:, pos]), maskd=((mask.reshape(128, 8, 512) - 1.0) * 10000.0).astype(ml_dtypes.bfloat16))
        in_maps.append({k: v for k, v in m.items() if k in _DECL})
    res = run_bass_kernel_spmd(nc, in_maps, core_ids=list(range(8)))
    kernel.last = res
    outp = np.zeros((4, SEQ, D), np.float32)
    for c in range(8):
        b = c // 2
        o = np.asarray(res.results[c]["out"], np.float32)
        for i, j in enumerate(blocks_of[c]):
            outp[b, 128 * j:128 * (j + 1)] = o[128 * i:128 * (i + 1)]
    return outp
```

```python
import numpy as np
import ml_dtypes
import concourse.bass as bass
import concourse.mybir as mybir
from concourse.bass_utils import run_bass_kernel_spmd

F32 = mybir.dt.float32
BF16 = mybir.dt.bfloat16
AF = mybir.ActivationFunctionType
ALU = mybir.AluOpType
AX = mybir.AxisListType

D = 2048
SEQ = 4096
NMETA = 16
L = SEQ + NMETA
NH = 8
NE = 64
DE = 512
EPS = 1e-5
ALPHA = 2.0 ** 0.25
LAM_INIT = 0.2
OFFS = ((0, 3, 4, 7), (1, 2, 5, 6))


class _Op:
    __slots__ = ("eng", "fn", "deps", "is_dma", "signal", "sem", "val", "idx", "pos")


class Sched:
    ENGS = ("pe", "act", "dve", "pool", "sp")
    RING = 12

    def __init__(self):
        self.ops = {e: [] for e in self.ENGS}
        self.last_w = {}
        self.readers = {}
        self.ndma = {"sp": 0, "pool": 0}
        self.dma_ops = {"sp": [], "pool": []}
        self.pending_barrier = {}
        self.n = 0

    def _add(self, eng, fn, r, w, is_dma, s=()):
        op = _Op()
        op.eng, op.fn, op.is_dma, op.signal, op.sem, op.val = eng, fn, is_dma, False, None, 0
        op.idx = self.n
        op.pos = len(self.ops[eng])
        self.n += 1
        deps = set()
        hard = set()
        for k in tuple(r) + tuple(s):
            lw = self.last_w.get(k)
            if lw is not None:
                deps.add(lw)
                if (not lw.is_dma) and (not is_dma) and lw.eng == eng and eng != "pe" and op.pos - lw.pos <= 2:
                    hard.add(lw)
        for k in w:
            lw = self.last_w.get(k)
            if lw is not None:
                deps.add(lw)
            rd = self.readers.get(k)
            if rd:
                deps.update(rd.values())
        r = tuple(r) + tuple(s)
        if eng in self.pending_barrier:
            deps.update(self.pending_barrier.pop(eng))
        if is_dma:
            n = self.ndma[eng]
            self.ndma[eng] = n + 1
            op.sem = (eng, n % self.RING)
            op.val = 16 * (n // self.RING + 1)
            if n >= self.RING:
                deps.add(self.dma_ops[eng][n - self.RING])
            self.dma_ops[eng].append(op)
        for k in r:
            d = self.readers.setdefault(k, {})
            d[("dma", op.idx) if is_dma else eng] = op
        for k in w:
            self.last_w[k] = op
            self.readers[k] = {}
        op.deps = [d for d in deps if (d.is_dma or is_dma or d.eng != eng)] + [d for d in hard if not (d.is_dma or is_dma or d.eng != eng)]
        for d in op.deps:
            if not d.is_dma:
                d.signal = True
        self.ops[eng].append(op)
        return op

    def pe(self, fn, r=(), w=(), s=()):
        return self._add("pe", fn, r, w, False, s)

    def act(self, fn, r=(), w=(), s=()):
        return self._add("act", fn, r, w, False, s)

    def dve(self, fn, r=(), w=(), s=()):
        return self._add("dve", fn, r, w, False, s)

    def pool(self, fn, r=(), w=(), s=()):
        return self._add("pool", fn, r, w, False, s)

    def dma(self, q, fn, r=(), w=()):
        return self._add(q, fn, r, w, True)

    def barrier(self):
        deps = []
        for e in self.ENGS:
            if self.ops[e]:
                deps.append(self.ops[e][-1])
        for q in ("sp", "pool"):
            deps.extend(self.dma_ops[q][-self.RING:])
        for e in self.ENGS:
            self.pending_barrier[e] = list(deps)

    def emit(self, nc, sems):
        for e in ("pe", "act", "dve", "pool"):
            c = 0
            for op in self.ops[e]:
                if op.is_dma:
                    continue
                if op.signal:
                    c += 1
                    op.sem = e
                    op.val = c
        final = {}
        for q in ("sp", "pool"):
            for op in self.dma_ops[q]:
                final[op.sem] = max(final.get(op.sem, 0), op.val)

        def run(eng_name, e):
            waited = {}
            for op in self.ops[eng_name]:
                need = {}
                for d in op.deps:
                    if d.val > need.get(d.sem, 0):
                        need[d.sem] = d.val
                for s, v in need.items():
                    if waited.get(s, 0) < v:
                        e.wait_ge(sems[s], v)
                        waited[s] = v
                ins = op.fn(e)
                if op.is_dma:
                    ins.then_inc(sems[op.sem], 16)
                elif op.signal:
                    ins.then_inc(sems[op.sem], 1)
            if eng_name == "sp":
                for s, v in final.items():
                    e.wait_ge(sems[s], v)

        with nc.Block() as block:
            @block.tensor
            def _(e):
                run("pe", e)

            @block.scalar
            def _(e):
                run("act", e)

            @block.vector
            def _(e):
                run("dve", e)

            @block.gpsimd
            def _(e):
                run("pool", e)

            @block.sync
            def _(e):
                run("sp", e)


class Arena:
    def __init__(self, t, nwords):
        self.t, self.n, self.off = t, nwords, 0

    def reset(self):
        self.off = 0

    def f32(self, n):
        assert self.off + n <= self.n, ("arena overflow", self.off, n, self.n)
        ap = self.t[:, self.off:self.off + n]
        self.off += n
        return ap

    def bf16(self, n):
        w = (n + 1) // 2
        assert self.off + w <= self.n, ("arena overflow", self.off, w, self.n)
        ap = self.t[:, self.off:self.off + w].bitcast(BF16)
        self.off += w
        return ap


ARENA_WORDS = 51800
NEXP = 65
SUB6 = 3
SUB6B = 7
STAGE = 99
DEBUG = False
NCHK = 9
PASSES = (0, 1)


def build_program():
    nc = bass.Bass("TRN2", target_bir_lowering=False)
    S = Sched()

    def dram(name, shape, dt, kind="ExternalInput"):
        if kind == "ExternalInput":
            if STAGE < 7 and name.startswith(("w_e", "w_s")):
                return None
            _DECL.append(name)
        return nc.dram_tensor(name, list(shape), dt, kind=kind).ap()

    scratch_kind = "ExternalOutput" if DEBUG else "Internal"
    xs = dram("xs", [SEQ, D], F32)
    meta = dram("meta", [NMETA, D], F32)
    w_in = dram("w_in", [D, 10240], F32)
    w_qkp = dram("w_qkp", [D, 2048], F32)
    par = dram("par", [128, 64], F32)
    ident_d = dram("ident", [128, 128], F32)
    cosk = dram("cosk", [128, L], F32)
    sink = dram("sink", [128, L], F32)
    xo = dram("xo", [2048, D], F32)
    xh = dram("xh", [32, D], F32)
    cosq = dram("cosq", [128, 2048], F32)
    sinq = dram("sinq", [128, 2048], F32)
    maskd = dram("maskd", [128, 8, 512], BF16)
    lamv = dram("lamv", [128, 256], F32)
    w_pa = dram("w_pa", [1024, D], F32)
    w_pc = dram("w_pc", [1024, D], F32)
    w_out = dram("w_out", [D, D], F32)
    w_router = dram("w_router", [D, NE], F32)
    rbd = dram("rbd", [128, NE], F32)
    bcd = dram("bcd", [6, 128, D], F32)
    w_eg = dram("w_eg", [NE, D, DE], F32)
    w_eu = dram("w_eu", [NE, D, DE], F32)
    w_ed = dram("w_ed", [NE, DE, D], F32)
    w_sg = dram("w_sg", [D, DE], F32)
    w_su = dram("w_su", [D, DE], F32)
    w_sd = dram("w_sd", [DE, D], F32)
    KT_d = dram("KT_d", [NH, 128, L], BF16, scratch_kind)
    V_d = dram("V_d", [L, 1024], BF16, scratch_kind)
    hTo_d = dram("hTo_d", [16, 128, 2048], BF16, scratch_kind)
    hTh_d = dram("hTh_d", [16, 128, 32], BF16, scratch_kind)
    QT_d = dram("QT_d", [NH, 128, 2048], BF16, scratch_kind)
    yT_d = dram("yT_d", [8, 128, 2048], BF16, scratch_kind)
    OnT_d = dram("OnT_d", [NH, 128, 2048], BF16, scratch_kind)
    mT_d = dram("mT_d", [16, 128, 2048], BF16, scratch_kind)
    h1_d = dram("h1_d", [2048, D], F32, scratch_kind)
    h1T_d = dram("h1T_d", [16, 128, 2048], BF16, scratch_kind)
    ew_d = dram("ew_d", [128, 16 * 65], F32, scratch_kind)
    out = dram("out", [2048, D], F32, "ExternalOutput")

    sem_names = ["pe", "act", "dve", "pool"] + [(q, i) for q in ("sp", "pool") for i in range(Sched.RING)]

    import contextlib
    with contextlib.ExitStack() as es:
        arena_t = es.enter_context(nc.sbuf_tensor("arena", [128, ARENA_WORDS], F32))
        A = Arena(arena_t, ARENA_WORDS)
        cst = es.enter_context(nc.sbuf_tensor("cst", [128, 64 + 128], F32))
        idb_t = es.enter_context(nc.sbuf_tensor("idb", [128, 128], BF16))
        ewt_t = es.enter_context(nc.sbuf_tensor("ewt", [128, 16 * 65], F32))
        ewt = ewt_t[:]
        banks = [es.enter_context(nc.psum_tensor(f"ps{i}", [128, 512], F32)) for i in range(8)]
        sems = {}
        for sn in sem_names:
            nm = sn if isinstance(sn, str) else f"{sn[0]}{sn[1]}"
            sems[sn] = es.enter_context(nc.semaphore("s_" + nm))

        parv = cst[:, 0:64]
        idf = cst[:, 64:192]
        idb = idb_t[:]
        G0T, B0T = 0, 16

        S.dma("sp", lambda e: e.dma_start(out=parv, in_=par[:, :]), w=["par"])
        S.dma("sp", lambda e: e.dma_start(out=idf, in_=ident_d[:, :]), w=["idf"])
        S.dve(lambda e: e.tensor_copy(out=idb, in_=idf), r=["idf"], w=["idb"])

        def ln_tile(src_ap, R, xr, xn, st, mv, rstd, tpb, hT_dst, keys, c0):
            S.dma("sp", lambda e: e.dma_start(out=xr[:R, :], in_=src_ap), w=[keys["xr"]])

            def stats(e):
                for i in range(4):
                    ins = e.bn_stats(out=st[:R, i, :], in_=xr[:R, i * 512:(i + 1) * 512])
                return ins
            S.dve(stats, r=[keys["xr"]], w=[keys["st"]])
            S.dve(lambda e: e.bn_aggr(out=mv[:R, :], in_=st[:R, :, :]), r=[keys["st"]], w=[keys["mv"]])
            S.act(lambda e: e.activation(out=rstd[:R, :], in_=mv[:R, 1:2], func=AF.Sqrt, bias=EPS, scale=1.0),
                  r=[keys["mv"]], w=[keys["rstd"]])

            S.dve(lambda e: e.reciprocal(out=rstd[:R, :], in_=rstd[:R, :]), r=[keys["rstd"]], w=[keys["rstd"]])
            S.dve(lambda e: e.tensor_scalar(out=xn[:R, :], in0=xr[:R, :], scalar1=mv[:R, 0:1], scalar2=rstd[:R, 0:1],
                                            op0=ALU.subtract, op1=ALU.mult),
                  r=[keys["xr"]], s=[keys["rstd"], keys["mv"]], w=[keys["xn"]])

            def tr(e):
                for kt in range(16):
                    bk = tpb[kt // 8]
                    ins = e.transpose(out=bk[:, (kt % 8) * 128:(kt % 8) * 128 + R], in_=xn[:R, kt * 128:(kt + 1) * 128],
                                      identity=idb[:R, :R])
                return ins
            S.pe(tr, r=[keys["xn"], "idb"], w=[keys["tp"]])

            def ev(e):
                for kt in range(16):
                    bk = tpb[kt // 8]
                    ins = e.activation(out=hT_dst(kt), in_=bk[:, (kt % 8) * 128:(kt % 8) * 128 + R], func=AF.Identity,
                                       scale=parv[:, G0T + kt:G0T + kt + 1], bias=parv[:, B0T + kt:B0T + kt + 1])
                return ins
            S.act(ev, r=[keys["tp"], "par"], w=[keys["hT"]])

        bank_bf = [b[:].bitcast(BF16) for b in banks]
        bank_f = [b[:] for b in banks]


        def V3(ap, a):
            return ap.rearrange("p (a b) -> p a b", a=a)

        def qk_pass(pfx, chunk_list, wcol0, cos_d, sin_d, out_d, hT_store=None, vmode=False):
            A.reset()
            xr_s = [A.f32(D) for _ in range(3)]
            xn_s = [A.bf16(D) for _ in range(2)]
            hT_s = [A.bf16(16 * 512).rearrange("p (k n) -> p k n", n=512) for _ in range(2)]
            st = A.f32(24).rearrange("p (a b) -> p a b", b=6)
            mv = A.f32(2)
            rstd = A.f32(1)
            W = A.bf16(16 * 2048).rearrange("p (k n) -> p k n", n=2048)
            cs_s = [(A.f32(512), A.f32(512)) for _ in range(2)]
            t_s = [(A.f32(512), A.f32(512)) for _ in range(2)]
            ko_s = [A.bf16(512) for _ in range(2)]
            vo_s = [A.bf16(1024) for _ in range(2)]
            S.barrier()
            if out_d is not None:
                S.dma("pool", lambda e: e.dma_start(out=W[:, :, 0:1024],
                                                    in_=w_in[:, wcol0:wcol0 + 1024].rearrange("(k p) n -> p k n", p=128)), w=[pfx + "W0"])
            if not vmode and out_d is not None:
                qc = wcol0
                S.dma("pool", lambda e: e.dma_start(out=W[:, :, 1024:2048],
                                                    in_=w_qkp[:, qc:qc + 1024].rearrange("(k p) n -> p k n", p=128)), w=[pfx + "W1"])
            tile_ctr = 0
            for ci, (col0, ntok, tiles) in enumerate(chunk_list):
                hT = hT_s[ci % 2]
                hk = f"{pfx}hT{ci % 2}"
                for tl, (src, R) in enumerate(tiles):
                    sl = tile_ctr % 2
                    keys = dict(xr=f"{pfx}xr{tile_ctr % 3}", xn=f"{pfx}xn{sl}", mv=pfx + "mv", rstd=pfx + "rstd", st=pfx + "st",
                                tp=f"{pfx}tp{sl}", hT=hk)
                    tpb = (bank_bf[2 * sl], bank_bf[2 * sl + 1])
                    ln_tile(src, R, xr_s[tile_ctr % 3], xn_s[sl], st, mv, rstd, tpb,
                            (lambda kt, hT=hT, tl=tl, R=R: hT[:, kt, tl * 128: tl * 128 + R]), keys, 0)
                    tile_ctr += 1
                N = ntok
                if hT_store is not None:
                    hT_store(hT, hk, col0, N)
                if out_d is None:
                    continue
                if not vmode:
                    cs = cs_s[ci % 2]
                    ck = f"{pfx}cs{ci % 2}"
                    S.dma("sp", lambda e, cs=cs, col0=col0, N=N: e.dma_start(out=cs[0][:, :N], in_=cos_d[:, col0:col0 + N]), w=[ck + "c"])
                    S.dma("sp", lambda e, cs=cs, col0=col0, N=N: e.dma_start(out=cs[1][:, :N], in_=sin_d[:, col0:col0 + N]), w=[ck + "s"])
                    for h in range(NH):
                        par2 = h % 2
                        pk, pkp = bank_f[4 + 2 * par2], bank_f[5 + 2 * par2]

                        def mm(e, h=h, hT=hT, N=N, pk=pk, pkp=pkp):
                            for v, pb in ((0, pk), (1, pkp)):
                                for kt in range(16):
                                    ins = e.matmul(pb[:, :N], lhsT=W[:, kt, v * 1024 + h * 128: v * 1024 + (h + 1) * 128],
                                                   rhs=hT[:, kt, :N], start=(kt == 0), stop=(kt == 15))
                            return ins
                        S.pe(mm, r=[hk, pfx + "W0", pfx + "W1"], w=[f"{pfx}pk{par2}"])
                        t1, t2 = t_s[par2]
                        ko = ko_s[par2]

                        def rope(e, pk=pk, pkp=pkp, cs=cs, N=N, t1=t1, t2=t2):
                            e.tensor_tensor(out=t1[:, :N], in0=pk[:, :N], in1=cs[0][:, :N], op=ALU.mult)
                            return e.tensor_tensor(out=t2[:, :N], in0=pkp[:, :N], in1=cs[1][:, :N], op=ALU.mult)
                        S.dve(rope, r=[f"{pfx}pk{par2}", ck + "c", ck + "s"], w=[f"{pfx}t{par2}"])
                        S.pool(lambda e, t1=t1, t2=t2, ko=ko, N=N: e.tensor_tensor(out=ko[:, :N], in0=t1[:, :N], in1=t2[:, :N], op=ALU.add),
                               r=[f"{pfx}t{par2}"], w=[f"{pfx}ko{par2}"])
                        S.dma("pool", lambda e, ko=ko, h=h, col0=col0, N=N: e.dma_start(out=out_d[h, :, col0:col0 + N], in_=ko[:, :N]),
                              r=[f"{pfx}ko{par2}"], w=[(pfx + "out", h, ci)])
                else:
                    for tl, (src, R) in enumerate(tiles):
                        vo = vo_s[tl % 2]
                        for half in range(2):
                            pb = bank_f[4 + half + 2 * (tl % 2)]

                            def mmv(e, hT=hT, tl=tl, R=R, half=half, pb=pb):
                                for kt in range(16):
                                    ins = e.matmul(pb[:R, :], lhsT=hT[:, kt, tl * 128: tl * 128 + R],
                                                   rhs=W[:, kt, half * 512:(half + 1) * 512], start=(kt == 0), stop=(kt == 15))
                                return ins
                            S.pe(mmv, r=[hk, pfx + "W0"], w=[f"{pfx}pv{half}{tl % 2}"])
                            S.act(lambda e, vo=vo, pb=pb, R=R, half=half: e.activation(out=vo[:R, half * 512:(half + 1) * 512], in_=pb[:R, :], func=AF.Copy),
                                  r=[f"{pfx}pv{half}{tl % 2}"], w=[f"{pfx}vo{tl % 2}{half}"])
                        p0 = col0 + tl * 128
                        S.dma("pool", lambda e, vo=vo, p0=p0, R=R: e.dma_start(out=out_d[p0:p0 + R, :], in_=vo[:R, :]),
                              r=[f"{pfx}vo{tl % 2}0", f"{pfx}vo{tl % 2}1"], w=[(pfx + "out", p0)])

        seq_chunks = [(0, NMETA, [(meta[:, :], NMETA)])] + \
                     [(NMETA + 512 * i, 512, [(xs[i * 512 + t * 128: i * 512 + (t + 1) * 128, :], 128) for t in range(4)]) for i in range(8)]
        own_chunks = [(512 * g, 512, [(xo[g * 512 + t * 128: g * 512 + (t + 1) * 128, :], 128) for t in range(4)]) for g in range(4)]
        halo_chunk = [(0, 32, [(xh[:, :], 32)])]

        if STAGE >= 1:
            qk_pass("A0", seq_chunks, 1024, cosk, sink, KT_d)
            qk_pass("A1", seq_chunks, 2048, None, None, V_d, vmode=True)

        def store_hTo(hT, hk, col0, N):
            S.dma("sp", lambda e: e.dma_start(out=hTo_d[:, :, col0:col0 + N].rearrange("k p n -> p k n"), in_=hT[:, :, :N]),
                  r=[hk], w=[("hTo_d", col0)])

        def store_hTh(hT, hk, col0, N):
            S.dma("sp", lambda e: e.dma_start(out=hTh_d[:, :, 0:32].rearrange("k p n -> p k n"), in_=hT[:, :, :32]),
                  r=[hk], w=["hTh_d"])

        if STAGE >= 2:
            qk_pass("B0", own_chunks, 0, cosq, sinq, QT_d, hT_store=store_hTo)
            qk_pass("Bh", halo_chunk, 0, None, None, None, hT_store=store_hTh)

        if STAGE >= 3:
            A.reset()
            Wc = A.bf16(16 * 3072).rearrange("p (k n) -> p k n", n=3072)
            hT_s = [A.bf16(16 * 512).rearrange("p (k n) -> p k n", n=512) for _ in range(2)]
            hTh = A.bf16(16 * 32).rearrange("p (k n) -> p k n", n=32)
            cxs = A.f32(512)
            cxhs = A.f32(8)
            zext = A.f32(4 * 130).rearrange("p (a b) -> p a b", b=130)
            c1 = A.f32(512)
            yT_s = [A.bf16(8 * 512).rearrange("p (k n) -> p k n", n=512) for _ in range(2)]
            S.barrier()
            for j in range(3):
                S.dma("pool", lambda e, j=j: e.dma_start(out=Wc[:, :, j * 1024:(j + 1) * 1024],
                                                         in_=w_in[:, 3072 + j * 1024: 4096 + j * 1024].rearrange("(k p) n -> p k n", p=128)),
                      w=[f"Wc{j}"])
            S.dma("sp", lambda e: e.dma_start(out=hTh, in_=hTh_d[:, :, :].rearrange("k p n -> p k n")), r=["hTh_d"], w=["hTh"])
            for g in range(4):
                hT = hT_s[g % 2]
                hk = f"C_hT{g % 2}"
                S.dma("sp", lambda e, hT=hT, g=g: e.dma_start(out=hT, in_=hTo_d[:, :, g * 512:(g + 1) * 512].rearrange("k p n -> p k n")),
                      r=[("hTo_d", g * 512)], w=[hk])
                yT = yT_s[g % 2]
                yk = f"C_yT{g % 2}"
                for ct in range(8):
                    b0 = 4 * (ct % 2)
                    bx, bc_, bb, bh = bank_f[b0], bank_f[b0 + 1], bank_f[b0 + 2], bank_f[b0 + 3]
                    bkk = f"C_bk{ct % 2}"

                    def mmc(e, hT=hT, ct=ct, g=g, bx=bx, bc_=bc_, bb=bb, bh=bh):
                        for j, pb in ((0, bx), (1, bc_), (2, bb)):
                            for kt in range(16):
                                ins = e.matmul(pb[:, :], lhsT=Wc[:, kt, j * 1024 + ct * 128: j * 1024 + (ct + 1) * 128],
                                               rhs=hT[:, kt, :], start=(kt == 0), stop=(kt == 15))
                        for j in (0, 1):
                            for kt in range(16):
                                ins = e.matmul(bh[:, j * 8:(j + 1) * 8], lhsT=Wc[:, kt, j * 1024 + ct * 128: j * 1024 + (ct + 1) * 128],
                                               rhs=hTh[:, kt, g * 8:(g + 1) * 8], start=(kt == 0), stop=(kt == 15))
                        return ins
                    S.pe(mmc, r=[hk, "hTh", "Wc0", "Wc1", "Wc2"], w=[bkk])

                    def cpx(e, bx=bx, bh=bh):
                        e.activation(out=cxs, in_=bx[:, :], func=AF.Copy)
                        return e.activation(out=cxhs, in_=bh[:, 0:8], func=AF.Copy)
                    S.act(cpx, r=[bkk], w=["C_cxs"])

                    def zmk(e, bc_=bc_, bh=bh):
                        e.tensor_tensor(out=zext[:, :, 2:130], in0=V3(bc_[:, :], 4), in1=V3(cxs, 4), op=ALU.mult)
                        return e.tensor_tensor(out=zext[:, :, 0:2], in0=V3(bh[:, 8:16], 4), in1=V3(cxhs, 4), op=ALU.mult)
                    S.dve(zmk, r=[bkk, "C_cxs"], w=["C_z"])
                    wc = 33
                    S.dve(lambda e, ct=ct: e.tensor_scalar(out=V3(c1, 4), in0=zext[:, :, 0:128], scalar1=parv[:, 33 + ct:34 + ct], scalar2=None,
                                                           op0=ALU.mult), r=["C_z", "par"], w=["C_c1"])
                    S.dve(lambda e, ct=ct: e.scalar_tensor_tensor(out=V3(c1, 4), in0=zext[:, :, 1:129], scalar=parv[:, 41 + ct:42 + ct],
                                                                  in1=V3(c1, 4), op0=ALU.mult, op1=ALU.add), r=["C_z", "C_c1", "par"], w=["C_c1"])
                    S.dve(lambda e, ct=ct: e.scalar_tensor_tensor(out=V3(c1, 4), in0=zext[:, :, 2:130], scalar=parv[:, 49 + ct:50 + ct],
                                                                  in1=V3(c1, 4), op0=ALU.mult, op1=ALU.add), r=["C_z", "C_c1", "par"], w=["C_c1"])
                    S.dve(lambda e, bb=bb, yT=yT, ct=ct: e.tensor_tensor(out=yT[:, ct, :], in0=bb[:, :], in1=c1, op=ALU.mult),
                          r=[bkk, "C_c1"], w=[yk])
                S.dma("sp", lambda e, yT=yT, g=g: e.dma_start(out=yT_d[:, :, g * 512:(g + 1) * 512].rearrange("k p n -> p k n"), in_=yT),
                      r=[yk], w=[("yT_d", g)])

        if STAGE >= 4:
            A.reset()
            lamt = A.f32(256)
            lprod = A.f32(128)
            lsum = A.f32(2)
            lex = A.f32(2)
            nlam = A.f32(1)
            sgs = A.f32(1)
            ones_b = A.bf16(128)
            ones_f = A.f32(128)
            mk = A.bf16(8 * 512).rearrange("p (k n) -> p k n", n=512)
            QT_s = [A.bf16(512) for _ in range(2)]
            KT_s = [A.bf16(L) for _ in range(2)]
            V_s = [A.bf16(33 * 128).rearrange("p (k n) -> p k n", n=128) for _ in range(2)]
            E_s = [[A.bf16(512) for _ in range(2)] for _ in range(2)]
            rz = [A.f32(512) for _ in range(2)]
            tt_ = [A.f32(512) for _ in range(2)]
            ocomb = A.f32(512)
            sq = A.f32(512)
            rst = A.f32(512)
            on_s = [A.bf16(512) for _ in range(2)]
            S.barrier()
            S.dma("sp", lambda e: e.dma_start(out=lamt, in_=lamv[:, :]), w=["lamt"])
            S.dma("sp", lambda e: e.dma_start(out=mk, in_=maskd[:, :, :]), w=["mk"])
            S.pool(lambda e: e.memset(ones_b, 1.0), w=["ones_b"])
            S.pool(lambda e: e.memset(ones_f, 1.0), w=["ones_f"])
            S.dve(lambda e: e.tensor_tensor(out=lprod, in0=lamt[:, 0:128], in1=lamt[:, 128:256], op=ALU.mult),
                  r=["lamt"], w=["lprod"])
            S.dve(lambda e: e.tensor_reduce(out=lsum, in_=V3(lprod, 2), axis=AX.X, op=ALU.add), r=["lprod"], w=["lsum"])
            S.act(lambda e: e.activation(out=lex, in_=lsum, func=AF.Exp), r=["lsum"], w=["lex"])
            S.dve(lambda e: e.tensor_tensor(out=nlam, in0=lex[:, 1:2], in1=lex[:, 0:1], op=ALU.subtract), r=["lex"], w=["nlam"])
            S.pool(lambda e: e.tensor_scalar(out=nlam, in0=nlam, scalar1=-LAM_INIT, scalar2=1.0, op0=ALU.add, op1=ALU.mult), r=["nlam"], w=["nlam"])
            S.pool(lambda e: e.tensor_scalar(out=sgs, in0=parv[:, 32:33], scalar1=1.0 - LAM_INIT, scalar2=1.0, op0=ALU.mult, op1=ALU.mult),
                   r=["par"], w=["sgs"])
            heads = [(g, h) for g in range(4) for h in range(NH)]

            def slot_of(n):
                sl = n % 2
                return sl, QT_s[sl], KT_s[sl], V_s[sl], f"D_Q{sl}", f"D_K{sl}", f"D_V{sl}"

            def issue_loads(n):
                g, h = heads[n]
                ntile = 8 * g + 8
                nk = NMETA + 128 * ntile
                sl, QT, KT, Vh, kq, kk, kv = slot_of(n)
                S.dma("sp", lambda e: e.dma_start(out=QT, in_=QT_d[h, :, g * 512:(g + 1) * 512]), r=[("B0out", h, g)], w=[kq])
                S.dma("sp", lambda e: e.dma_start(out=KT[:, :nk], in_=KT_d[h, :, 0:nk]), r=[("A0out", h, c) for c in range(9)], w=[kk])
                S.dma("sp", lambda e: e.dma_start(out=Vh[:NMETA, 0, :], in_=V_d[0:NMETA, h * 128:(h + 1) * 128]), r=[("A1out", 0)], w=[kv + "m"])
                S.dma("sp", lambda e: e.dma_start(
                    out=Vh[:, 1:1 + ntile, :], in_=V_d[NMETA:NMETA + 128 * ntile, h * 128:(h + 1) * 128].rearrange("(t p) n -> p t n", p=128)),
                    r=[("A1out", NMETA + 128 * t) for t in range(32)], w=[kv])

            def s_part(n, t):
                g, h = heads[n]
                sl, QT, KT, Vh, kq, kk, kv = slot_of(n)
                R = NMETA if t == 0 else 128
                kc0 = 0 if t == 0 else NMETA + 128 * (t - 1)
                tp_ = t % 2
                jj = t - 1 - 8 * g
                masked = (t >= 1 and jj >= 0)
                for m in range(2):
                    sb = bank_f[2 * tp_ + m]
                    Em = E_s[tp_][m]

                    def smm(e, sb=sb, m=m):
                        ins = e.matmul(sb[:R, :], lhsT=KT[m * 64:(m + 1) * 64, kc0:kc0 + R], rhs=QT[m * 64:(m + 1) * 64, :],
                                       start=True, stop=(not masked))
                        if masked:
                            ins = e.matmul(sb[:R, :], lhsT=idb, rhs=mk[:, jj, :], start=False, stop=True)
                        return ins
                    S.pe(smm, r=[kq, kk, "mk", "idb"], w=[f"D_S{tp_}{m}"])
                    S.act(lambda e, sb=sb, Em=Em: e.activation(out=Em[:R, :], in_=sb[:R, :], func=AF.Exp, scale=0.125),
                          r=[f"D_S{tp_}{m}"], w=[f"D_E{tp_}{m}"])

            def av_part(n, t):
                g, h = heads[n]
                ntile = 8 * g + 8
                sl, QT, KT, Vh, kq, kk, kv = slot_of(n)
                R = NMETA if t == 0 else 128
                tp_ = t % 2
                for m in range(2):
                    Em = E_s[tp_][m]

                    def av(e, Em=Em, m=m):
                        e.matmul(bank_f[4 + m][:, :], lhsT=Vh[:R, t, :], rhs=Em[:R, :], start=(t == 0), stop=(t == ntile))
                        return e.matmul(bank_f[6 + m][:, :], lhsT=ones_b[:R, :], rhs=Em[:R, :], start=(t == 0), stop=(t == ntile))
                    S.pe(av, r=[f"D_E{tp_}{m}", kv, kv + "m", "ones_b"], w=[f"D_O{m}"])

            def fin_a(n):
                for m in range(2):
                    S.dve(lambda e, m=m: e.reciprocal(out=rz[m], in_=bank_f[6 + m][:, :]), r=[f"D_O{m}"], w=[f"D_rz{m}"])
                    S.dve(lambda e, m=m: e.tensor_tensor(out=tt_[m], in0=bank_f[4 + m][:, :], in1=rz[m], op=ALU.mult),
                          r=[f"D_O{m}", f"D_rz{m}"], w=[f"D_t{m}"])

            def fin_b(n):
                g, h = heads[n]
                sl = n % 2
                S.dve(lambda e: e.scalar_tensor_tensor(out=ocomb, in0=tt_[1], scalar=nlam[:, 0:1], in1=tt_[0], op0=ALU.mult, op1=ALU.add),
                      r=["D_t0", "D_t1", "nlam"], w=["D_oc"])
                S.pool(lambda e: e.tensor_tensor(out=sq, in0=ocomb, in1=ocomb, op=ALU.mult), r=["D_oc"], w=["D_sq"])
                S.pe(lambda e: e.matmul(bank_f[2][:, :], lhsT=ones_f, rhs=sq, start=True, stop=True), r=["D_sq", "ones_f"], w=["D_S10"])
                S.act(lambda e: e.activation(out=rst, in_=bank_f[2][:, :], func=AF.Sqrt, scale=1.0 / 128.0, bias=EPS), r=["D_S10"], w=["D_rst"])
                S.dve(lambda e: e.reciprocal(out=rst, in_=rst), r=["D_rst"], w=["D_rst"])
                S.dve(lambda e: e.tensor_tensor(out=ocomb, in0=ocomb, in1=rst, op=ALU.mult), r=["D_oc", "D_rst"], w=["D_oc"])
                on = on_s[sl]
                S.pool(lambda e: e.tensor_scalar(out=on, in0=ocomb, scalar1=sgs[:, 0:1], scalar2=1.0, op0=ALU.mult, op1=ALU.mult),
                       r=["D_oc", "sgs"], w=[f"D_on{sl}"])
                S.dma("sp", lambda e: e.dma_start(out=OnT_d[h, :, g * 512:(g + 1) * 512], in_=on), r=[f"D_on{sl}"], w=[("OnT_d", h, g)])

            issue_loads(0)
            for n in range(len(heads)):
                if n + 1 < len(heads):
                    issue_loads(n + 1)
                nt = 8 * heads[n][0] + 8
                s_part(n, 0)
                for t in range(nt + 1):
                    if t + 1 <= nt:
                        s_part(n, t + 1)
                    av_part(n, t)
                    if t == 2 and n >= 1:
                        fin_b(n - 1)
                fin_a(n)
            fin_b(len(heads) - 1)

        if STAGE >= 5:
            A.reset()
            hT_a = A.bf16(16 * 2048).rearrange("p (k n) -> p k n", n=2048)
            on_a = A.bf16(8 * 2048).rearrange("p (k n) -> p k n", n=2048)
            y_a = A.bf16(8 * 2048).rearrange("p (k n) -> p k n", n=2048)
            Wg_s = [A.bf16(16 * 512).rearrange("p (k n) -> p k n", n=512) for _ in range(2)]
            Wp_s = [A.bf16(8 * 512).rearrange("p (k n) -> p k n", n=512) for _ in range(2)]
            mT_s = [A.bf16(2 * 512).rearrange("p (k n) -> p k n", n=512) for _ in range(2)]
            sg_t = [A.f32(512) for _ in range(2)]
            t12 = [A.f32(512) for _ in range(2)]
            S.barrier()
            for g in range(4):
                S.dma("sp", lambda e, g=g: e.dma_start(out=hT_a[:, :, g * 512:(g + 1) * 512], in_=hTo_d[:, :, g * 512:(g + 1) * 512].rearrange("k p n -> p k n")),
                      r=[("hTo_d", g * 512)], w=[f"E_hT{g}"])
                S.dma("sp", lambda e, g=g: e.dma_start(out=on_a[:, :, g * 512:(g + 1) * 512], in_=OnT_d[:, :, g * 512:(g + 1) * 512].rearrange("k p n -> p k n")),
                      r=[("OnT_d", h, g) for h in range(NH)], w=[f"E_on{g}"])
                S.dma("sp", lambda e, g=g: e.dma_start(out=y_a[:, :, g * 512:(g + 1) * 512], in_=yT_d[:, :, g * 512:(g + 1) * 512].rearrange("k p n -> p k n")),
                      r=[("yT_d", g)], w=[f"E_y{g}"])
            octr = 0
            for c8 in range(8):
                ws = c8 % 2
                Wg, Wp = Wg_s[ws], Wp_s[ws]
                S.dma("pool", lambda e, Wg=Wg, c8=c8: e.dma_start(out=Wg[:, :, 0:256], in_=w_in[:, 6144 + c8 * 256: 6144 + (c8 + 1) * 256].rearrange("(k p) n -> p k n", p=128)), w=[f"E_Wga{ws}"])
                S.dma("pool", lambda e, Wg=Wg, c8=c8: e.dma_start(out=Wg[:, :, 256:512], in_=w_in[:, 8192 + c8 * 256: 8192 + (c8 + 1) * 256].rearrange("(k p) n -> p k n", p=128)), w=[f"E_Wgc{ws}"])
                S.dma("pool", lambda e, Wp=Wp, c8=c8: e.dma_start(out=Wp[:, :, 0:256], in_=w_pa[:, c8 * 256:(c8 + 1) * 256].rearrange("(k p) n -> p k n", p=128)), w=[f"E_Wpa{ws}"])
                S.dma("pool", lambda e, Wp=Wp, c8=c8: e.dma_start(out=Wp[:, :, 256:512], in_=w_pc[:, c8 * 256:(c8 + 1) * 256].rearrange("(k p) n -> p k n", p=128)), w=[f"E_Wpc{ws}"])
                for g in range(4):
                    mT = mT_s[octr % 2]
                    mk_ = f"E_mT{octr % 2}"
                    octr += 1
                    for k2 in range(2):
                        b0 = 4 * (k2 % 2)
                        bk = f"E_bk{k2 % 2}"

                        def mmg(e, Wg=Wg, Wp=Wp, g=g, k2=k2, b0=b0):
                            for j in range(2):
                                for k in range(16):
                                    e.matmul(bank_f[b0 + j][:, :], lhsT=Wg[:, k, j * 256 + k2 * 128: j * 256 + (k2 + 1) * 128],
                                             rhs=hT_a[:, k, g * 512:(g + 1) * 512], start=(k == 0), stop=(k == 15))
                            for k in range(8):
                                e.matmul(bank_f[b0 + 2][:, :], lhsT=Wp[:, k, k2 * 128:(k2 + 1) * 128], rhs=on_a[:, k, g * 512:(g + 1) * 512],
                                         start=(k == 0), stop=(k == 7))
                            for k in range(8):
                                ins = e.matmul(bank_f[b0 + 3][:, :], lhsT=Wp[:, k, 256 + k2 * 128: 256 + (k2 + 1) * 128],
                                               rhs=y_a[:, k, g * 512:(g + 1) * 512], start=(k == 0), stop=(k == 7))
                            return ins
                        S.pe(mmg, r=[f"E_Wga{ws}", f"E_Wgc{ws}", f"E_Wpa{ws}", f"E_Wpc{ws}", f"E_hT{g}", f"E_on{g}", f"E_y{g}"], w=[bk])
                        for j in range(2):
                            S.act(lambda e, j=j, b0=b0: e.activation(out=sg_t[j], in_=bank_f[b0 + j][:, :], func=AF.Sigmoid), r=[bk], w=[f"E_sg{j}"])
                            S.dve(lambda e, j=j, b0=b0: e.tensor_tensor(out=t12[j], in0=bank_f[b0 + 2 + j][:, :], in1=sg_t[j], op=ALU.mult),
                                  r=[bk, f"E_sg{j}"], w=[f"E_t{j}"])
                        S.pool(lambda e, mT=mT, k2=k2: e.tensor_tensor(out=mT[:, k2, :], in0=t12[0], in1=t12[1], op=ALU.add), r=["E_t0", "E_t1"], w=[mk_])
                    S.dma("sp", lambda e, mT=mT, g=g, c8=c8: e.dma_start(
                        out=mT_d[2 * c8:2 * c8 + 2, :, g * 512:(g + 1) * 512].rearrange("k p n -> p k n"), in_=mT),
                        r=[mk_], w=[("mT_d", g, c8)])

        if STAGE >= 6:
            A.reset()
            Wo = A.bf16(16 * 2048).rearrange("p (k n) -> p k n", n=2048)
            mt_s = [A.bf16(16 * 128).rearrange("p (k n) -> p k n", n=128) for _ in range(2)]
            xr_s = [A.f32(D) for _ in range(2)]
            r_s = [A.f32(D) for _ in range(2)]
            G0, B0, G1, B1 = A.f32(D), A.f32(D), A.f32(D), A.f32(D)
            h1Tf = A.f32(16 * 128).rearrange("p (k n) -> p k n", n=128)
            h1Tb_s = [A.bf16(16 * 512).rearrange("p (k n) -> p k n", n=512) for _ in range(2)]
            wr = A.f32(16 * 64).rearrange("p (k n) -> p k n", n=64)
            rbt = A.f32(64)
            st = A.f32(24).rearrange("p (a b) -> p a b", b=6)
            mv = A.f32(2)
            rstd = A.f32(1)
            sgm, sel, eq, sel2, selm, msk, smm = [A.f32(64) for _ in range(7)]
            m1, m2, gs, top8, gm, pen, top8e = [A.f32(8) for _ in range(7)]
            ssum = A.f32(1)
            S.barrier()
            S.dma("pool", lambda e: e.dma_start(out=Wo, in_=w_out[:, :].rearrange("(k p) n -> p k n", p=128)), w=["F_Wo"])
            for i, t_ in enumerate((G0, B0, G1, B1)):
                S.dma("sp", lambda e, i=i, t_=t_: e.dma_start(out=t_, in_=bcd[i, :, :]), w=[f"F_bc{i}"])
            S.dma("sp", lambda e: e.dma_start(out=wr, in_=w_router[:, :].rearrange("(k p) n -> p k n", p=128)), w=["F_wr"])
            S.dma("sp", lambda e: e.dma_start(out=rbt, in_=rbd[:, :]), w=["F_rb"])
            S.pool(lambda e: e.memset(ewt[:, :], 1.0), w=["ew"])

            def ln_stats(src, keyin, pf):
                def stats(e):
                    for i in range(4):
                        ins = e.bn_stats(out=st[:, i, :], in_=src[:, i * 512:(i + 1) * 512])
                    return ins
                S.dve(stats, r=[keyin], w=[pf + "st"])
                S.dve(lambda e: e.bn_aggr(out=mv, in_=st), r=[pf + "st"], w=[pf + "mv"])
                S.act(lambda e: e.activation(out=rstd, in_=mv[:, 1:2], func=AF.Sqrt, bias=EPS, scale=1.0), r=[pf + "mv"], w=[pf + "rstd"])
                S.dve(lambda e: e.reciprocal(out=rstd, in_=rstd), r=[pf + "rstd"], w=[pf + "rstd"])
                S.dve(lambda e: e.tensor_scalar(out=src, in0=src, scalar1=mv[:, 0:1], scalar2=rstd[:, 0:1], op0=ALU.subtract, op1=ALU.mult),
                      r=[keyin], s=[pf + "rstd", pf + "mv"], w=[keyin])

            for tt in range(16):
                sl = tt % 2
                mt, xr, rr, h1Tb = mt_s[sl], xr_s[sl], r_s[sl], h1Tb_s[(tt // 4) % 2]
                g4, t4 = tt // 4, tt % 4
                S.dma("sp", lambda e, mt=mt, tt=tt: e.dma_start(out=mt, in_=mT_d[:, :, tt * 128:(tt + 1) * 128].rearrange("k p n -> p k n")),
                      r=[("mT_d", tt // 4, c8) for c8 in range(8)], w=[f"F_mt{sl}"])
                S.dma("sp", lambda e, xr=xr, tt=tt: e.dma_start(out=xr, in_=xo[tt * 128:(tt + 1) * 128, :]), w=[f"F_xr{sl}"])

                def mmo(e, mt=mt):
                    for cc in range(4):
                        for k in range(16):
                            ins = e.matmul(bank_f[cc][:, :], lhsT=mt[:, k, :], rhs=Wo[:, k, cc * 512:(cc + 1) * 512], start=(k == 0), stop=(k == 15))
                    return ins
                S.pe(mmo, r=[f"F_mt{sl}", "F_Wo"], w=["F_po"])
                ln_stats(xr, f"F_xr{sl}", "F_a")
                S.dve(lambda e, xr=xr: e.tensor_tensor(out=xr, in0=xr, in1=G0, op=ALU.mult), r=[f"F_xr{sl}", "F_bc0"], w=[f"F_xr{sl}"])
                S.pool(lambda e, xr=xr: e.tensor_tensor(out=xr, in0=xr, in1=B0, op=ALU.add), r=[f"F_xr{sl}", "F_bc1"], w=[f"F_xr{sl}"])

                def resid(e, xr=xr, rr=rr):
                    for cc in range(4):
                        ins = e.scalar_tensor_tensor(out=rr[:, cc * 512:(cc + 1) * 512], in0=xr[:, cc * 512:(cc + 1) * 512], scalar=ALPHA,
                                                     in1=bank_f[cc][:, :], op0=ALU.mult, op1=ALU.add)
                    return ins
                S.dve(resid, r=[f"F_xr{sl}", "F_po"], w=[f"F_r{sl}"])
                ln_stats(rr, f"F_r{sl}", "F_b")
                S.dve(lambda e, rr=rr: e.tensor_tensor(out=rr, in0=rr, in1=G1, op=ALU.mult), r=[f"F_r{sl}", "F_bc2"], w=[f"F_r{sl}"])
                S.pool(lambda e, rr=rr: e.tensor_tensor(out=rr, in0=rr, in1=B1, op=ALU.add), r=[f"F_r{sl}", "F_bc3"], w=[f"F_r{sl}"])
                S.dma("sp", lambda e, rr=rr, tt=tt: e.dma_start(out=h1_d[tt * 128:(tt + 1) * 128, :], in_=rr), r=[f"F_r{sl}"], w=[("h1_d", tt)])

                if SUB6 >= 1:
                    def trf(e, rr=rr):
                        for k in range(16):
                            ins = e.matmul(bank_f[4 + k // 4][:, (k % 4) * 128:(k % 4 + 1) * 128], lhsT=rr[:, k * 128:(k + 1) * 128], rhs=idf, start=True, stop=True)
                        return ins
                    S.pe(trf, r=[f"F_r{sl}", "idf"], w=["F_pt"])

                    def evb(e, h1Tb=h1Tb, t4=t4):
                        for q in range(4):
                            ins = e.activation(out=h1Tb[:, 4 * q:4 * q + 4, t4 * 128:(t4 + 1) * 128], in_=V3(bank_f[4 + q][:, :], 4), func=AF.Copy)
                        return ins
                    if SUB6B & 1:
                        S.act(evb, r=["F_pt"], w=[f"F_h1Tb{g4 % 2}"])

                    def evf(e):
                        for q in range(4):
                            ins = e.tensor_copy(out=h1Tf[:, 4 * q:4 * q + 4, :], in_=V3(bank_f[4 + q][:, :], 4))
                        return ins
                    if SUB6B & 2:
                        S.dve(evf, r=["F_pt", f"F_h1Tb{g4 % 2}"], w=["F_h1Tf"])
                    if (SUB6B & 4) and t4 == 3:
                        S.dma("sp", lambda e, h1Tb=h1Tb, g4=g4: e.dma_start(out=h1T_d[:, :, g4 * 512:(g4 + 1) * 512].rearrange("k p n -> p k n"), in_=h1Tb),
                              r=[f"F_h1Tb{g4 % 2}"], w=[("h1T_d", g4)])

                if SUB6 >= 2:
                    def mmr(e):
                        for k in range(16):
                            ins = e.matmul(bank_f[0][:, 0:64], lhsT=h1Tf[:, k, :], rhs=wr[:, k, :], start=(k == 0), stop=(k == 15))
                        return ins
                    S.pe(mmr, r=["F_h1Tf", "F_wr"], w=["F_po"])
                if SUB6 >= 3:
                    S.act(lambda e: e.activation(out=sgm, in_=bank_f[0][:, 0:64], func=AF.Sigmoid), r=["F_po"], w=["R_sgm"])
                    S.dve(lambda e: e.tensor_tensor(out=sel, in0=sgm, in1=rbt, op=ALU.add), r=["R_sgm", "F_rb"], w=["R_sel"])
                    S.dve(lambda e: e.tensor_reduce(out=m1, in_=V3(sel, 8), axis=AX.X, op=ALU.max), r=["R_sel"], w=["R_m1"])
                    S.dve(lambda e: e.tensor_tensor(out=V3(eq, 8), in0=V3(sel, 8), in1=m1.unsqueeze(2).to_broadcast([128, 8, 8]), op=ALU.is_equal),
                          r=["R_sel", "R_m1"], w=["R_eq"])
                    S.dve(lambda e: e.scalar_tensor_tensor(out=sel2, in0=eq, scalar=-1e30, in1=sel, op0=ALU.mult, op1=ALU.add),
                          r=["R_eq", "R_sel"], w=["R_sel2"])
                    S.dve(lambda e: e.tensor_reduce(out=m2, in_=V3(sel2, 8), axis=AX.X, op=ALU.max), r=["R_sel2"], w=["R_m2"])
                    S.dve(lambda e: e.tensor_tensor(out=gs, in0=m1, in1=m2, op=ALU.add), r=["R_m1", "R_m2"], w=["R_gs"])
                    S.dve(lambda e: e.max(out=top8, in_=gs), r=["R_gs"], w=["R_top8"])
                    S.dve(lambda e: e.tensor_tensor(out=gm, in0=gs, in1=top8[:, 3:4].to_broadcast([128, 8]), op=ALU.is_ge), r=["R_gs", "R_top8"], w=["R_gm"])
                    S.dve(lambda e: e.tensor_scalar(out=pen, in0=gm, scalar1=-1.0, scalar2=1e30, op0=ALU.add, op1=ALU.mult), r=["R_gm"], w=["R_pen"])
                    S.dve(lambda e: e.tensor_tensor(out=V3(selm, 8), in0=V3(sel, 8), in1=pen.unsqueeze(2).to_broadcast([128, 8, 8]), op=ALU.add),
                          r=["R_sel", "R_pen"], w=["R_selm"])
                    S.dve(lambda e: e.max(out=top8e, in_=selm), r=["R_selm"], w=["R_top8e"])
                    S.dve(lambda e: e.tensor_tensor(out=msk, in0=selm, in1=top8e[:, 7:8].to_broadcast([128, 64]), op=ALU.is_ge),
                          r=["R_selm", "R_top8e"], w=["R_msk"])
                    S.dve(lambda e: e.tensor_tensor(out=smm, in0=sgm, in1=msk, op=ALU.mult), r=["R_sgm", "R_msk"], w=["R_smm"])
                    S.dve(lambda e: e.tensor_reduce(out=ssum, in_=smm, axis=AX.X, op=ALU.add), r=["R_smm"], w=["R_ssum"])
                    S.dve(lambda e: e.reciprocal(out=ssum, in_=ssum), r=["R_ssum"], w=["R_ssum"])
                    S.dve(lambda e, tt=tt: e.scalar_tensor_tensor(out=ewt[:, tt * 65: tt * 65 + 64], in0=smm, scalar=2.5,
                                                                  in1=ssum[:, 0:1].to_broadcast([128, 64]), op0=ALU.mult, op1=ALU.mult),
                          r=["R_smm", "R_ssum"], w=["ew"])

            if DEBUG:
                S.dma("sp", lambda e: e.dma_start(out=ew_d[:, :], in_=ewt), r=["ew"], w=["ew_d"])

        if STAGE >= 7:
            experts = [(w_eg[e_], w_eu[e_], w_ed[e_]) for e_ in range(NE)] + [(w_sg, w_su, w_sd)]
            def moe_half(hf):
                A.reset()
                acc = A.f32(8 * D).rearrange("p (t n) -> p t n", n=D)
                h1T = A.bf16(16 * 1024).rearrange("p (k n) -> p k n", n=1024)
                Wgu_s = [A.bf16(16 * 1024).rearrange("p (k n) -> p k n", n=1024) for _ in range(2)]
                Wd_s = [A.bf16(4 * 2048).rearrange("p (k n) -> p k n", n=2048) for _ in range(2)]
                hid = A.bf16(4 * 1024).rearrange("p (k n) -> p k n", n=1024)
                sgt_one = A.f32(512)
                sgt = [sgt_one, sgt_one]
                pf = f"M{hf}"

                def acck(t):
                    return [(pf + "acc", t, c_) for c_ in range(4)]
                S.barrier()
                for t in range(8):
                    S.dma("sp", lambda e, t=t: e.dma_start(out=acc[:, t, :], in_=h1_d[(8 * hf + t) * 128:(8 * hf + t + 1) * 128, :]),
                          r=[("h1_d", 8 * hf + t)], w=acck(t))
                    S.pool(lambda e, t=t: e.tensor_scalar(out=acc[:, t, :], in0=acc[:, t, :], scalar1=ALPHA, scalar2=1.0, op0=ALU.mult, op1=ALU.mult),
                           r=acck(t), w=acck(t))
                S.dma("sp", lambda e: e.dma_start(out=h1T, in_=h1T_d[:, :, hf * 1024:(hf + 1) * 1024].rearrange("k p n -> p k n")),
                      r=[("h1T_d", 2 * hf), ("h1T_d", 2 * hf + 1)], w=[pf + "h1T"])
                for ei, (wg_, wu_, wd_) in enumerate(experts[:NEXP]):
                    ws = ei % 2
                    Wgu, Wd = Wgu_s[ws], Wd_s[ws]
                    S.dma("pool", lambda e, Wgu=Wgu, wg_=wg_: e.dma_start(out=Wgu[:, :, 0:512], in_=wg_.rearrange("(k p) n -> p k n", p=128)), w=[f"{pf}Wg{ws}"])
                    S.dma("pool", lambda e, Wgu=Wgu, wu_=wu_: e.dma_start(out=Wgu[:, :, 512:1024], in_=wu_.rearrange("(k p) n -> p k n", p=128)), w=[f"{pf}Wu{ws}"])
                    S.dma("pool", lambda e, Wd=Wd, wd_=wd_: e.dma_start(out=Wd, in_=wd_.rearrange("(k p) n -> p k n", p=128)), w=[f"{pf}Wd{ws}"])
                    for m in range(4):
                        for n in range(2):
                            b0 = 2 * ((2 * m + n) % 2)
                            bk = f"{pf}gu{b0}"

                            def mmgu(e, Wgu=Wgu, m=m, n=n, b0=b0):
                                for j in range(2):
                                    for k in range(16):
                                        ins = e.matmul(bank_f[b0 + j][:, :], lhsT=Wgu[:, k, j * 512 + m * 128: j * 512 + (m + 1) * 128],
                                                       rhs=h1T[:, k, n * 512:(n + 1) * 512], start=(k == 0), stop=(k == 15))
                                return ins
                            S.pe(mmgu, r=[f"{pf}Wg{ws}", f"{pf}Wu{ws}", pf + "h1T"], w=[bk])
                            sg_ = sgt[(2 * m + n) % 2]
                            S.act(lambda e, sg_=sg_, b0=b0: e.activation(out=sg_, in_=bank_f[b0][:, :], func=AF.Silu), r=[bk], w=[f"{pf}sg"])
                            S.dve(lambda e, sg_=sg_, b0=b0, m=m, n=n: e.tensor_tensor(out=hid[:, m, n * 512:(n + 1) * 512], in0=bank_f[b0 + 1][:, :],
                                                                                      in1=sg_, op=ALU.mult), r=[bk, f"{pf}sg"], w=[(pf + "hid", m, n)])
                    for t in range(8):
                        for cc in range(4):
                            yb = 4 + cc
                            S.pe(lambda e, t=t, cc=cc, yb=yb, Wd=Wd: [e.matmul(bank_f[yb][:, :], lhsT=hid[:, m, t * 128:(t + 1) * 128],
                                                                              rhs=Wd[:, m, cc * 512:(cc + 1) * 512], start=(m == 0), stop=(m == 3))
                                                                     for m in range(4)][-1],
                                 r=[(pf + "hid", m, t // 4) for m in range(4)] + [f"{pf}Wd{ws}"], w=[f"{pf}y{cc}"])
                            S.dve(lambda e, t=t, cc=cc, yb=yb, ei=ei: e.scalar_tensor_tensor(
                                out=acc[:, t, cc * 512:(cc + 1) * 512], in0=bank_f[yb][:, :],
                                scalar=ewt[:, (8 * hf + t) * 65 + ei:(8 * hf + t) * 65 + ei + 1], in1=acc[:, t, cc * 512:(cc + 1) * 512],
                                op0=ALU.mult, op1=ALU.add), r=[f"{pf}y{cc}", (pf + "acc", t, cc), "ew"], w=[(pf + "acc", t, cc)])
                S.barrier()
                A.off = 8 * D
                G2, B2 = A.f32(D), A.f32(D)
                st = A.f32(24).rearrange("p (a b) -> p a b", b=6)
                mv = A.f32(2)
                rstd = A.f32(1)
                S.dma("sp", lambda e: e.dma_start(out=G2, in_=bcd[4, :, :]), w=[pf + "G2"])
                S.dma("sp", lambda e: e.dma_start(out=B2, in_=bcd[5, :, :]), w=[pf + "B2"])
                for t in range(8):
                    src = acc[:, t, :]
                    kin = acck(t)

                    def stats(e, src=src):
                        for i in range(4):
                            ins = e.bn_stats(out=st[:, i, :], in_=src[:, i * 512:(i + 1) * 512])
                        return ins
                    S.dve(stats, r=kin, w=[pf + "st"])
                    S.dve(lambda e: e.bn_aggr(out=mv, in_=st), r=[pf + "st"], w=[pf + "mv"])
                    S.act(lambda e: e.activation(out=rstd, in_=mv[:, 1:2], func=AF.Sqrt, bias=EPS, scale=1.0), r=[pf + "mv"], w=[pf + "rstd"])
                    S.dve(lambda e: e.reciprocal(out=rstd, in_=rstd), r=[pf + "rstd"], w=[pf + "rstd"])
                    S.dve(lambda e, src=src: e.tensor_scalar(out=src, in0=src, scalar1=mv[:, 0:1], scalar2=rstd[:, 0:1], op0=ALU.subtract, op1=ALU.mult),
                          r=kin, s=[pf + "rstd", pf + "mv"], w=kin)
                    S.dve(lambda e, src=src: e.tensor_tensor(out=src, in0=src, in1=G2, op=ALU.mult), r=kin + [pf + "G2"], w=kin)
                    S.pool(lambda e, src=src: e.tensor_tensor(out=src, in0=src, in1=B2, op=ALU.add), r=kin + [pf + "B2"], w=kin)
                    S.dma("sp", lambda e, src=src, t=t: e.dma_start(out=out[(8 * hf + t) * 128:(8 * hf + t + 1) * 128, :], in_=src),
                          r=kin, w=[("out", hf, t)])

            if STAGE >= 7:
                for hf_ in range(2):
                    moe_half(hf_)

        S.barrier()
        S.emit(nc, sems)
    return nc


def _rope_tables():
    half = 32
    inv = (10000.0 ** (-np.arange(half, dtype=np.float32) / half)).astype(np.float32)
    pos = np.arange(L, dtype=np.float32)
    ang = pos[None, :] * inv[:, None]
    cos = np.cos(ang).astype(np.float32)
    sin = np.sin(ang).astype(np.float32)
    cos64 = np.concatenate([cos, cos], 0)
    sin64 = np.concatenate([-sin, sin], 0)
    return np.concatenate([cos64, cos64], 0), np.concatenate([sin64, sin64], 0)


def _perm_cols():
    idx = np.arange(1024).reshape(NH, 2, 2, 32)[:, :, ::-1, :].reshape(1024)
    return idx


_NC_CACHE = {}
_DECL = []


def kernel(**inputs):
    f = lambda k: np.asarray(inputs[k], np.float32)
    x = f("x")
    w_in = np.ascontiguousarray(f("w_in")[0])
    pidx = _perm_cols()
    w_qkp = np.ascontiguousarray(np.concatenate([w_in[:, 0:1024][:, pidx], w_in[:, 1024:2048][:, pidx]], axis=1))
    cosk, sink = _rope_tables()
    par = np.zeros((128, 64), np.float32)
    par[:, 0:16] = f("ln0_g").reshape(16, 128).T
    par[:, 16:32] = f("ln0_b").reshape(16, 128).T
    par[:, 32] = f("subln_g")[0]
    wc = f("w_conv")[0]
    for k in range(3):
        par[:, 33 + 8 * k: 41 + 8 * k] = wc[k].reshape(8, 128).T
    ident = np.eye(128, dtype=np.float32)
    meta = np.ascontiguousarray(f("meta_tokens"))
    lamv = np.ascontiguousarray(np.broadcast_to(np.concatenate(
        [f("lambda_q1")[0], f("lambda_q2")[0], f("lambda_k1")[0], f("lambda_k2")[0]])[None, :], (128, 256)))
    rbd = np.ascontiguousarray(np.broadcast_to(f("router_bias")[0][None, :], (128, NE)))
    bcd = np.ascontiguousarray(np.stack([np.broadcast_to(v[None, :], (128, D)) for v in
                                         (f("ln0_g"), f("ln0_b"), f("ln1_g")[0], f("ln1_b")[0], f("ln2_g")[0], f("ln2_b")[0])]))
    shared = dict(meta=meta, w_in=w_in, w_qkp=w_qkp, par=par, ident=ident, cosk=cosk, sink=sink, lamv=lamv, rbd=rbd, bcd=bcd,
                  w_pa=np.ascontiguousarray(f("w_proj_attn")[0]), w_pc=np.ascontiguousarray(f("w_proj_conv")[0]),
                  w_out=np.ascontiguousarray(f("w_out")[0]), w_router=np.ascontiguousarray(f("w_router")[0]),
                  w_eg=np.ascontiguousarray(f("w_exp_gate")[0]), w_eu=np.ascontiguousarray(f("w_exp_up")[0]),
                  w_ed=np.ascontiguousarray(f("w_exp_down")[0]), w_sg=np.ascontiguousarray(f("w_sh_gate")[0]),
                  w_su=np.ascontiguousarray(f("w_sh_up")[0]), w_sd=np.ascontiguousarray(f("w_sh_down")[0]))
    if "nc" not in _NC_CACHE:
        _NC_CACHE["nc"] = build_program()
    nc = _NC_CACHE["nc"]
    in_maps = []
    blocks_of = []
    for c in range(8):
        b, p = c // 2, c % 2
        blks = [8 * g + OFFS[p][i] for g in range(4) for i in range(4)]
        blocks_of.append(blks)
        xo = np.ascontiguousarray(np.concatenate([x[b, 128 * j:128 * (j + 1)] for j in blks], 0))
        seqx = x[b]
        xh = np.ascontiguousarray(np.concatenate([(meta[14:16] if j == 0 else seqx[128 * j - 2:128 * j]) for j in blks], 0))
        pos = np.concatenate([NMETA + 128 * j + np.arange(128) for j in blks])
        mask = np.zeros((128, 8, 4, 128), np.float32)
        r = np.arange(128)
        diag = ((r[:, None] // 64) <= (r[None, :] // 64)).astype(np.float32)
        for o in range(8):
            for i in range(4):
                oo = OFFS[p][i]
                if o < oo:
                    mask[:, o, i, :] = 1.0
                elif o == oo:
                    mask[:, o, i, :] = diag
        m = dict(shared)
        m.update(xs=np.ascontiguousarray(x[b]), xo=xo, xh=xh, cosq=np.ascontiguousarray(cosk[:, pos]),
                 sinq=np.ascontiguousarray(sink[:, pos]), maskd=((mask.reshape(128, 8, 512) - 1.0) * 10000.0).astype(ml_dtypes.bfloat16))
        in_maps.append({k: v for k, v in m.items() if k in _DECL})
    res = run_bass_kernel_spmd(nc, in_maps, core_ids=list(range(8)))
    kernel.last = res
    outp = np.zeros((4, SEQ, D), np.float32)
    for c in range(8):
        b = c // 2
        o = np.asarray(res.results[c]["out"], np.float32)
        for i, j in enumerate(blocks_of[c]):
            outp[b, 128 * j:128 * (j + 1)] = o[128 * i:128 * (i + 1)]
    return outp
```

```python
import numpy as np
import ml_dtypes
import concourse.bass as bass
import concourse.mybir as mybir
from concourse.bass_utils import run_bass_kernel_spmd

F32 = mybir.dt.float32
BF16 = mybir.dt.bfloat16
AF = mybir.ActivationFunctionType
ALU = mybir.AluOpType
AX = mybir.AxisListType

D = 2048
SEQ = 4096
NMETA = 16
L = SEQ + NMETA
NH = 8
NE = 64
DE = 512
EPS = 1e-5
ALPHA = 2.0 ** 0.25
LAM_INIT = 0.2
OFFS = ((0, 3, 4, 7), (1, 2, 5, 6))


class _Op:
    __slots__ = ("eng", "fn", "deps", "is_dma", "signal", "sem", "val", "idx", "pos")


class Sched:
    ENGS = ("pe", "act", "dve", "pool", "sp")
    RING = 16

    def __init__(self):
        self.ops = {e: [] for e in self.ENGS}
        self.last_w = {}
        self.readers = {}
        self.ndma = {"sp": 0, "pool": 0}
        self.dma_ops = {"sp": [], "pool": []}
        self.pending_barrier = {}
        self.n = 0

    def _add(self, eng, fn, r, w, is_dma, s=()):
        op = _Op()
        op.eng, op.fn, op.is_dma, op.signal, op.sem, op.val = eng, fn, is_dma, False, None, 0
        op.idx = self.n
        op.pos = len(self.ops[eng])
        self.n += 1
        deps = set()
        hard = set()
        for k in tuple(r) + tuple(s):
            lw = self.last_w.get(k)
            if lw is not None:
                deps.add(lw)
                if (not lw.is_dma) and (not is_dma) and lw.eng == eng and eng != "pe" and op.pos - lw.pos <= 2:
                    hard.add(lw)
        for k in w:
            lw = self.last_w.get(k)
            if lw is not None:
                deps.add(lw)
            rd = self.readers.get(k)
            if rd:
                deps.update(rd.values())
        r = tuple(r) + tuple(s)
        if eng in self.pending_barrier:
            deps.update(self.pending_barrier.pop(eng))
        if is_dma:
            n = self.ndma[eng]
            self.ndma[eng] = n + 1
            op.sem = (eng, n % self.RING)
            op.val = 16 * (n // self.RING + 1)
            if n >= self.RING:
                deps.add(self.dma_ops[eng][n - self.RING])
            self.dma_ops[eng].append(op)
        for k in r:
            d = self.readers.setdefault(k, {})
            d[("dma", op.idx) if is_dma else eng] = op
        for k in w:
            self.last_w[k] = op
            self.readers[k] = {}
        op.deps = [d for d in deps if (d.is_dma or is_dma or d.eng != eng)] + [d for d in hard if not (d.is_dma or is_dma or d.eng != eng)]
        for d in op.deps:
            if not d.is_dma:
                d.signal = True
        self.ops[eng].append(op)
        return op

    def pe(self, fn, r=(), w=(), s=()):
        return self._add("pe", fn, r, w, False, s)

    def act(self, fn, r=(), w=(), s=()):
        return self._add("act", fn, r, w, False, s)

    def dve(self, fn, r=(), w=(), s=()):
        return self._add("dve", fn, r, w, False, s)

    def pool(self, fn, r=(), w=(), s=()):
        return self._add("pool", fn, r, w, False, s)

    def dma(self, q, fn, r=(), w=()):
        return self._add(q, fn, r, w, True)

    def barrier(self):
        deps = []
        for e in self.ENGS:
            if self.ops[e]:
                deps.append(self.ops[e][-1])
        for q in ("sp", "pool"):
            deps.extend(self.dma_ops[q][-self.RING:])
        for e in self.ENGS:
            self.pending_barrier[e] = list(deps)

    def emit(self, nc, sems):
        for e in ("pe", "act", "dve", "pool"):
            c = 0
            for op in self.ops[e]:
                if op.is_dma:
                    continue
                if op.signal:
                    c += 1
                    op.sem = e
                    op.val = c
        final = {}
        for q in ("sp", "pool"):
            for op in self.dma_ops[q]:
                final[op.sem] = max(final.get(op.sem, 0), op.val)

        def run(eng_name, e):
            waited = {}
            for op in self.ops[eng_name]:
                need = {}
                for d in op.deps:
                    if d.val > need.get(d.sem, 0):
                        need[d.sem] = d.val
                for s, v in need.items():
                    if waited.get(s, 0) < v:
                        e.wait_ge(sems[s], v)
                        waited[s] = v
                ins = op.fn(e)
                if op.is_dma:
                    ins.then_inc(sems[op.sem], 16)
                elif op.signal:
                    ins.then_inc(sems[op.sem], 1)
            if eng_name == "sp":
                for s, v in final.items():
                    e.wait_ge(sems[s], v)

        with nc.Block() as block:
            @block.tensor
            def _(e):
                run("pe", e)

            @block.scalar
            def _(e):
                run("act", e)

            @block.vector
            def _(e):
                run("dve", e)

            @block.gpsimd
            def _(e):
                run("pool", e)

            @block.sync
            def _(e):
                run("sp", e)


class Arena:
    def __init__(self, t, nwords):
        self.t, self.n, self.off = t, nwords, 0

    def reset(self):
        self.off = 0

    def f32(self, n):
        assert self.off + n <= self.n, ("arena overflow", self.off, n, self.n)
        ap = self.t[:, self.off:self.off + n]
        self.off += n
        return ap

    def bf16(self, n):
        w = (n + 1) // 2
        assert self.off + w <= self.n, ("arena overflow", self.off, w, self.n)
        ap = self.t[:, self.off:self.off + w].bitcast(BF16)
        self.off += w
        return ap


ARENA_WORDS = 51800
NEXP = 65
SUB6 = 3
SUB6B = 7
STAGE = 99
DEBUG = False
NCHK = 9
PASSES = (0, 1)


def build_program():
    nc = bass.Bass("TRN2", target_bir_lowering=False)
    S = Sched()

    def dram(name, shape, dt, kind="ExternalInput"):
        if kind == "ExternalInput":
            if STAGE < 7 and name.startswith(("w_e", "w_s")):
                return None
            _DECL.append(name)
        return nc.dram_tensor(name, list(shape), dt, kind=kind).ap()

    scratch_kind = "ExternalOutput" if DEBUG else "Internal"
    xs = dram("xs", [SEQ, D], F32)
    meta = dram("meta", [NMETA, D], F32)
    w_in = dram("w_in", [D, 10240], F32)
    w_qkp = dram("w_qkp", [D, 2048], F32)
    par = dram("par", [128, 64], F32)
    ident_d = dram("ident", [128, 128], F32)
    cosk = dram("cosk", [128, L], F32)
    sink = dram("sink", [128, L], F32)
    xo = dram("xo", [2048, D], F32)
    xh = dram("xh", [32, D], F32)
    cosq = dram("cosq", [128, 2048], F32)
    sinq = dram("sinq", [128, 2048], F32)
    maskd = dram("maskd", [128, 8, 512], BF16)
    lamv = dram("lamv", [128, 256], F32)
    w_pa = dram("w_pa", [1024, D], F32)
    w_pc = dram("w_pc", [1024, D], F32)
    w_out = dram("w_out", [D, D], F32)
    w_router = dram("w_router", [D, NE], F32)
    rbd = dram("rbd", [128, NE], F32)
    bcd = dram("bcd", [6, 128, D], F32)
    w_eg = dram("w_eg", [NE, D, DE], F32)
    w_eu = dram("w_eu", [NE, D, DE], F32)
    w_ed = dram("w_ed", [NE, DE, D], F32)
    w_sg = dram("w_sg", [D, DE], F32)
    w_su = dram("w_su", [D, DE], F32)
    w_sd = dram("w_sd", [DE, D], F32)
    KT_d = dram("KT_d", [NH, 128, L], BF16, scratch_kind)
    V_d = dram("V_d", [L, 1024], BF16, scratch_kind)
    hTo_d = dram("hTo_d", [16, 128, 2048], BF16, scratch_kind)
    hTh_d = dram("hTh_d", [16, 128, 32], BF16, scratch_kind)
    QT_d = dram("QT_d", [NH, 128, 2048], BF16, scratch_kind)
    yT_d = dram("yT_d", [8, 128, 2048], BF16, scratch_kind)
    OnT_d = dram("OnT_d", [NH, 128, 2048], BF16, scratch_kind)
    mT_d = dram("mT_d", [16, 128, 2048], BF16, scratch_kind)
    h1_d = dram("h1_d", [2048, D], F32, scratch_kind)
    h1T_d = dram("h1T_d", [16, 128, 2048], BF16, scratch_kind)
    ew_d = dram("ew_d", [128, 16 * 65], F32, scratch_kind)
    out = dram("out", [2048, D], F32, "ExternalOutput")

    sem_names = ["pe", "act", "dve", "pool"] + [(q, i) for q in ("sp", "pool") for i in range(Sched.RING)]

    import contextlib
    with contextlib.ExitStack() as es:
        arena_t = es.enter_context(nc.sbuf_tensor("arena", [128, ARENA_WORDS], F32))
        A = Arena(arena_t, ARENA_WORDS)
        cst = es.enter_context(nc.sbuf_tensor("cst", [128, 64 + 128], F32))
        idb_t = es.enter_context(nc.sbuf_tensor("idb", [128, 128], BF16))
        ewt_t = es.enter_context(nc.sbuf_tensor("ewt", [128, 16 * 65], F32))
        ewt = ewt_t[:]
        banks = [es.enter_context(nc.psum_tensor(f"ps{i}", [128, 512], F32)) for i in range(8)]
        sems = {}
        for sn in sem_names:
            nm = sn if isinstance(sn, str) else f"{sn[0]}{sn[1]}"
            sems[sn] = es.enter_context(nc.semaphore("s_" + nm))

        parv = cst[:, 0:64]
        idf = cst[:, 64:192]
        idb = idb_t[:]
        G0T, B0T = 0, 16

        S.dma("sp", lambda e: e.dma_start(out=parv, in_=par[:, :]), w=["par"])
        S.dma("sp", lambda e: e.dma_start(out=idf, in_=ident_d[:, :]), w=["idf"])
        S.dve(lambda e: e.tensor_copy(out=idb, in_=idf), r=["idf"], w=["idb"])

        def ln_tile(src_ap, R, xr, xn, st, mv, rstd, tpb, hT_dst, keys, c0):
            S.dma("sp", lambda e: e.dma_start(out=xr[:R, :], in_=src_ap), w=[keys["xr"]])

            def stats(e):
                for i in range(4):
                    ins = e.bn_stats(out=st[:R, i, :], in_=xr[:R, i * 512:(i + 1) * 512])
                return ins
            S.dve(stats, r=[keys["xr"]], w=[keys["st"]])
            S.dve(lambda e: e.bn_aggr(out=mv[:R, :], in_=st[:R, :, :]), r=[keys["st"]], w=[keys["mv"]])
            S.act(lambda e: e.activation(out=rstd[:R, :], in_=mv[:R, 1:2], func=AF.Sqrt, bias=EPS, scale=1.0),
                  r=[keys["mv"]], w=[keys["rstd"]])

            S.dve(lambda e: e.reciprocal(out=rstd[:R, :], in_=rstd[:R, :]), r=[keys["rstd"]], w=[keys["rstd"]])
            S.dve(lambda e: e.tensor_scalar(out=xn[:R, :], in0=xr[:R, :], scalar1=mv[:R, 0:1], scalar2=rstd[:R, 0:1],
                                            op0=ALU.subtract, op1=ALU.mult),
                  r=[keys["xr"]], s=[keys["rstd"], keys["mv"]], w=[keys["xn"]])

            def tr(e):
                for kt in range(16):
                    bk = tpb[kt // 8]
                    ins = e.transpose(out=bk[:, (kt % 8) * 128:(kt % 8) * 128 + R], in_=xn[:R, kt * 128:(kt + 1) * 128],
                                      identity=idb[:R, :R])
                return ins
            S.pe(tr, r=[keys["xn"], "idb"], w=[keys["tp"]])

            def ev(e):
                for kt in range(16):
                    bk = tpb[kt // 8]
                    ins = e.activation(out=hT_dst(kt), in_=bk[:, (kt % 8) * 128:(kt % 8) * 128 + R], func=AF.Identity,
                                       scale=parv[:, G0T + kt:G0T + kt + 1], bias=parv[:, B0T + kt:B0T + kt + 1])
                return ins
            S.act(ev, r=[keys["tp"], "par"], w=[keys["hT"]])

        bank_bf = [b[:].bitcast(BF16) for b in banks]
        bank_f = [b[:] for b in banks]


        def V3(ap, a):
            return ap.rearrange("p (a b) -> p a b", a=a)

        def qk_pass(pfx, chunk_list, wcol0, cos_d, sin_d, out_d, hT_store=None, vmode=False):
            A.reset()
            xr_s = [A.f32(D) for _ in range(3)]
            xn_s = [A.bf16(D) for _ in range(2)]
            hT_s = [A.bf16(16 * 512).rearrange("p (k n) -> p k n", n=512) for _ in range(2)]
            st = A.f32(24).rearrange("p (a b) -> p a b", b=6)
            mv = A.f32(2)
            rstd = A.f32(1)
            W = A.bf16(16 * 2048).rearrange("p (k n) -> p k n", n=2048)
            cs_s = [(A.f32(512), A.f32(512)) for _ in range(2)]
            t_s = [(A.f32(512), A.f32(512)) for _ in range(2)]
            ko_s = [A.bf16(512) for _ in range(2)]
            vo_s = [A.bf16(1024) for _ in range(2)]
            S.barrier()
            if out_d is not None:
                S.dma("pool", lambda e: e.dma_start(out=W[:, :, 0:1024],
                                                    in_=w_in[:, wcol0:wcol0 + 1024].rearrange("(k p) n -> p k n", p=128)), w=[pfx + "W0"])
            if not vmode and out_d is not None:
                qc = wcol0
                S.dma("pool", lambda e: e.dma_start(out=W[:, :, 1024:2048],
                                                    in_=w_qkp[:, qc:qc + 1024].rearrange("(k p) n -> p k n", p=128)), w=[pfx + "W1"])
            tile_ctr = 0
            for ci, (col0, ntok, tiles) in enumerate(chunk_list):
                hT = hT_s[ci % 2]
                hk = f"{pfx}hT{ci % 2}"
                for tl, (src, R) in enumerate(tiles):
                    sl = tile_ctr % 2
                    keys = dict(xr=f"{pfx}xr{tile_ctr % 3}", xn=f"{pfx}xn{sl}", mv=pfx + "mv", rstd=pfx + "rstd", st=pfx + "st",
                                tp=f"{pfx}tp{sl}", hT=hk)
                    tpb = (bank_bf[2 * sl], bank_bf[2 * sl + 1])
                    ln_tile(src, R, xr_s[tile_ctr % 3], xn_s[sl], st, mv, rstd, tpb,
                            (lambda kt, hT=hT, tl=tl, R=R: hT[:, kt, tl * 128: tl * 128 + R]), keys, 0)
                    tile_ctr += 1
                N = ntok
                if hT_store is not None:
                    hT_store(hT, hk, col0, N)
                if out_d is None:
                    continue
                if not vmode:
                    cs = cs_s[ci % 2]
                    ck = f"{pfx}cs{ci % 2}"
                    S.dma("sp", lambda e, cs=cs, col0=col0, N=N: e.dma_start(out=cs[0][:, :N], in_=cos_d[:, col0:col0 + N]), w=[ck + "c"])
                    S.dma("sp", lambda e, cs=cs, col0=col0, N=N: e.dma_start(out=cs[1][:, :N], in_=sin_d[:, col0:col0 + N]), w=[ck + "s"])
                    for h in range(NH):
                        par2 = h % 2
                        pk, pkp = bank_f[4 + 2 * par2], bank_f[5 + 2 * par2]

                        def mm(e, h=h, hT=hT, N=N, pk=pk, pkp=pkp):
                            for v, pb in ((0, pk), (1, pkp)):
                                for kt in range(16):
                                    ins = e.matmul(pb[:, :N], lhsT=W[:, kt, v * 1024 + h * 128: v * 1024 + (h + 1) * 128],
                                                   rhs=hT[:, kt, :N], start=(kt == 0), stop=(kt == 15))
                            return ins
                        S.pe(mm, r=[hk, pfx + "W0", pfx + "W1"], w=[f"{pfx}pk{par2}"])
                        t1, t2 = t_s[par2]
                        ko = ko_s[par2]

                        def rope(e, pk=pk, pkp=pkp, cs=cs, N=N, t1=t1, t2=t2):
                            e.tensor_tensor(out=t1[:, :N], in0=pk[:, :N], in1=cs[0][:, :N], op=ALU.mult)
                            return e.tensor_tensor(out=t2[:, :N], in0=pkp[:, :N], in1=cs[1][:, :N], op=ALU.mult)
                        S.dve(rope, r=[f"{pfx}pk{par2}", ck + "c", ck + "s"], w=[f"{pfx}t{par2}"])
                        S.pool(lambda e, t1=t1, t2=t2, ko=ko, N=N: e.tensor_tensor(out=ko[:, :N], in0=t1[:, :N], in1=t2[:, :N], op=ALU.add),
                               r=[f"{pfx}t{par2}"], w=[f"{pfx}ko{par2}"])
                        S.dma("pool", lambda e, ko=ko, h=h, col0=col0, N=N: e.dma_start(out=out_d[h, :, col0:col0 + N], in_=ko[:, :N]),
                              r=[f"{pfx}ko{par2}"], w=[(pfx + "out", h, ci)])
                else:
                    for tl, (src, R) in enumerate(tiles):
                        vo = vo_s[tl % 2]
                        for half in range(2):
                            pb = bank_f[4 + half + 2 * (tl % 2)]

                            def mmv(e, hT=hT, tl=tl, R=R, half=half, pb=pb):
                                for kt in range(16):
                                    ins = e.matmul(pb[:R, :], lhsT=hT[:, kt, tl * 128: tl * 128 + R],
                                                   rhs=W[:, kt, half * 512:(half + 1) * 512], start=(kt == 0), stop=(kt == 15))
                                return ins
                            S.pe(mmv, r=[hk, pfx + "W0"], w=[f"{pfx}pv{half}{tl % 2}"])
                            S.act(lambda e, vo=vo, pb=pb, R=R, half=half: e.activation(out=vo[:R, half * 512:(half + 1) * 512], in_=pb[:R, :], func=AF.Copy),
                                  r=[f"{pfx}pv{half}{tl % 2}"], w=[f"{pfx}vo{tl % 2}{half}"])
                        p0 = col0 + tl * 128
                        S.dma("pool", lambda e, vo=vo, p0=p0, R=R: e.dma_start(out=out_d[p0:p0 + R, :], in_=vo[:R, :]),
                              r=[f"{pfx}vo{tl % 2}0", f"{pfx}vo{tl % 2}1"], w=[(pfx + "out", p0)])

        seq_chunks = [(0, NMETA, [(meta[:, :], NMETA)])] + \
                     [(NMETA + 512 * i, 512, [(xs[i * 512 + t * 128: i * 512 + (t + 1) * 128, :], 128) for t in range(4)]) for i in range(8)]
        own_chunks = [(512 * g, 512, [(xo[g * 512 + t * 128: g * 512 + (t + 1) * 128, :], 128) for t in range(4)]) for g in range(4)]
        halo_chunk = [(0, 32, [(xh[:, :], 32)])]

        if STAGE >= 1:
            qk_pass("A0", seq_chunks, 1024, cosk, sink, KT_d)
            qk_pass("A1", seq_chunks, 2048, None, None, V_d, vmode=True)

        def store_hTo(hT, hk, col0, N):
            S.dma("sp", lambda e: e.dma_start(out=hTo_d[:, :, col0:col0 + N].rearrange("k p n -> p k n"), in_=hT[:, :, :N]),
                  r=[hk], w=[("hTo_d", col0)])

        def store_hTh(hT, hk, col0, N):
            S.dma("sp", lambda e: e.dma_start(out=hTh_d[:, :, 0:32].rearrange("k p n -> p k n"), in_=hT[:, :, :32]),
                  r=[hk], w=["hTh_d"])

        if STAGE >= 2:
            qk_pass("B0", own_chunks, 0, cosq, sinq, QT_d, hT_store=store_hTo)
            qk_pass("Bh", halo_chunk, 0, None, None, None, hT_store=store_hTh)

        if STAGE >= 3:
            A.reset()
            Wc = A.bf16(16 * 3072).rearrange("p (k n) -> p k n", n=3072)
            hT_s = [A.bf16(16 * 512).rearrange("p (k n) -> p k n", n=512) for _ in range(2)]
            hTh = A.bf16(16 * 32).rearrange("p (k n) -> p k n", n=32)
            cxs = A.f32(512)
            cxhs = A.f32(8)
            zext = A.f32(4 * 130).rearrange("p (a b) -> p a b", b=130)
            c1 = A.f32(512)
            yT_s = [A.bf16(8 * 512).rearrange("p (k n) -> p k n", n=512) for _ in range(2)]
            S.barrier()
            for j in range(3):
                S.dma("pool", lambda e, j=j: e.dma_start(out=Wc[:, :, j * 1024:(j + 1) * 1024],
                                                         in_=w_in[:, 3072 + j * 1024: 4096 + j * 1024].rearrange("(k p) n -> p k n", p=128)),
                      w=[f"Wc{j}"])
            S.dma("sp", lambda e: e.dma_start(out=hTh, in_=hTh_d[:, :, :].rearrange("k p n -> p k n")), r=["hTh_d"], w=["hTh"])
            for g in range(4):
                hT = hT_s[g % 2]
                hk = f"C_hT{g % 2}"
                S.dma("sp", lambda e, hT=hT, g=g: e.dma_start(out=hT, in_=hTo_d[:, :, g * 512:(g + 1) * 512].rearrange("k p n -> p k n")),
                      r=[("hTo_d", g * 512)], w=[hk])
                yT = yT_s[g % 2]
                yk = f"C_yT{g % 2}"
                for ct in range(8):
                    b0 = 4 * (ct % 2)
                    bx, bc_, bb, bh = bank_f[b0], bank_f[b0 + 1], bank_f[b0 + 2], bank_f[b0 + 3]
                    bkk = f"C_bk{ct % 2}"

                    def mmc(e, hT=hT, ct=ct, g=g, bx=bx, bc_=bc_, bb=bb, bh=bh):
                        for j, pb in ((0, bx), (1, bc_), (2, bb)):
                            for kt in range(16):
                                ins = e.matmul(pb[:, :], lhsT=Wc[:, kt, j * 1024 + ct * 128: j * 1024 + (ct + 1) * 128],
                                               rhs=hT[:, kt, :], start=(kt == 0), stop=(kt == 15))
                        for j in (0, 1):
                            for kt in range(16):
                                ins = e.matmul(bh[:, j * 8:(j + 1) * 8], lhsT=Wc[:, kt, j * 1024 + ct * 128: j * 1024 + (ct + 1) * 128],
                                               rhs=hTh[:, kt, g * 8:(g + 1) * 8], start=(kt == 0), stop=(kt == 15))
                        return ins
                    S.pe(mmc, r=[hk, "hTh", "Wc0", "Wc1", "Wc2"], w=[bkk])

                    def cpx(e, bx=bx, bh=bh):
                        e.activation(out=cxs, in_=bx[:, :], func=AF.Copy)
                        return e.activation(out=cxhs, in_=bh[:, 0:8], func=AF.Copy)
                    S.act(cpx, r=[bkk], w=["C_cxs"])

                    def zmk(e, bc_=bc_, bh=bh):
                        e.tensor_tensor(out=zext[:, :, 2:130], in0=V3(bc_[:, :], 4), in1=V3(cxs, 4), op=ALU.mult)
                        return e.tensor_tensor(out=zext[:, :, 0:2], in0=V3(bh[:, 8:16], 4), in1=V3(cxhs, 4), op=ALU.mult)
                    S.dve(zmk, r=[bkk, "C_cxs"], w=["C_z"])
                    wc = 33
                    S.dve(lambda e, ct=ct: e.tensor_scalar(out=V3(c1, 4), in0=zext[:, :, 0:128], scalar1=parv[:, 33 + ct:34 + ct], scalar2=None,
                                                           op0=ALU.mult), r=["C_z", "par"], w=["C_c1"])
                    S.dve(lambda e, ct=ct: e.scalar_tensor_tensor(out=V3(c1, 4), in0=zext[:, :, 1:129], scalar=parv[:, 41 + ct:42 + ct],
                                                                  in1=V3(c1, 4), op0=ALU.mult, op1=ALU.add), r=["C_z", "C_c1", "par"], w=["C_c1"])
                    S.dve(lambda e, ct=ct: e.scalar_tensor_tensor(out=V3(c1, 4), in0=zext[:, :, 2:130], scalar=parv[:, 49 + ct:50 + ct],
                                                                  in1=V3(c1, 4), op0=ALU.mult, op1=ALU.add), r=["C_z", "C_c1", "par"], w=["C_c1"])
                    S.dve(lambda e, bb=bb, yT=yT, ct=ct: e.tensor_tensor(out=yT[:, ct, :], in0=bb[:, :], in1=c1, op=ALU.mult),
                          r=[bkk, "C_c1"], w=[yk])
                S.dma("sp", lambda e, yT=yT, g=g: e.dma_start(out=yT_d[:, :, g * 512:(g + 1) * 512].rearrange("k p n -> p k n"), in_=yT),
                      r=[yk], w=[("yT_d", g)])

        if STAGE >= 4:
            A.reset()
            lamt = A.f32(256)
            lprod = A.f32(128)
            lsum = A.f32(2)
            lex = A.f32(2)
            nlam = A.f32(1)
            sgs = A.f32(1)
            ones_b = A.bf16(128)
            ones_f = A.f32(128)
            mk = A.bf16(8 * 512).rearrange("p (k n) -> p k n", n=512)
            QT_s = [A.bf16(512) for _ in range(2)]
            KT_s = [A.bf16(L) for _ in range(2)]
            V_s = [A.bf16(33 * 128).rearrange("p (k n) -> p k n", n=128) for _ in range(2)]
            E_s = [[A.bf16(512) for _ in range(2)] for _ in range(2)]
            rz = [A.f32(512) for _ in range(2)]
            tt_ = [A.f32(512) for _ in range(2)]
            ocomb = A.f32(512)
            sq = A.f32(512)
            rst = A.f32(512)
            on_s = [A.bf16(512) for _ in range(2)]
            S.barrier()
            S.dma("sp", lambda e: e.dma_start(out=lamt, in_=lamv[:, :]), w=["lamt"])
            S.dma("sp", lambda e: e.dma_start(out=mk, in_=maskd[:, :, :]), w=["mk"])
            S.pool(lambda e: e.memset(ones_b, 1.0), w=["ones_b"])
            S.pool(lambda e: e.memset(ones_f, 1.0), w=["ones_f"])
            S.dve(lambda e: e.tensor_tensor(out=lprod, in0=lamt[:, 0:128], in1=lamt[:, 128:256], op=ALU.mult),
                  r=["lamt"], w=["lprod"])
            S.dve(lambda e: e.tensor_reduce(out=lsum, in_=V3(lprod, 2), axis=AX.X, op=ALU.add), r=["lprod"], w=["lsum"])
            S.act(lambda e: e.activation(out=lex, in_=lsum, func=AF.Exp), r=["lsum"], w=["lex"])
            S.dve(lambda e: e.tensor_tensor(out=nlam, in0=lex[:, 1:2], in1=lex[:, 0:1], op=ALU.subtract), r=["lex"], w=["nlam"])
            S.pool(lambda e: e.tensor_scalar(out=nlam, in0=nlam, scalar1=-LAM_INIT, scalar2=1.0, op0=ALU.add, op1=ALU.mult), r=["nlam"], w=["nlam"])
            S.pool(lambda e: e.tensor_scalar(out=sgs, in0=parv[:, 32:33], scalar1=1.0 - LAM_INIT, scalar2=1.0, op0=ALU.mult, op1=ALU.mult),
                   r=["par"], w=["sgs"])
            heads = [(g, h) for g in range(4) for h in range(NH)]

            def slot_of(n):
                sl = n % 2
                return sl, QT_s[sl], KT_s[sl], V_s[sl], f"D_Q{sl}", f"D_K{sl}", f"D_V{sl}"

            def issue_loads(n):
                g, h = heads[n]
                ntile = 8 * g + 8
                nk = NMETA + 128 * ntile
                sl, QT, KT, Vh, kq, kk, kv = slot_of(n)
                S.dma("sp", lambda e: e.dma_start(out=QT, in_=QT_d[h, :, g * 512:(g + 1) * 512]), r=[("B0out", h, g)], w=[kq])
                S.dma("sp", lambda e: e.dma_start(out=KT[:, :nk], in_=KT_d[h, :, 0:nk]), r=[("A0out", h, c) for c in range(9)], w=[kk])
                S.dma("sp", lambda e: e.dma_start(out=Vh[:NMETA, 0, :], in_=V_d[0:NMETA, h * 128:(h + 1) * 128]), r=[("A1out", 0)], w=[kv + "m"])
                S.dma("sp", lambda e: e.dma_start(
                    out=Vh[:, 1:1 + ntile, :], in_=V_d[NMETA:NMETA + 128 * ntile, h * 128:(h + 1) * 128].rearrange("(t p) n -> p t n", p=128)),
                    r=[("A1out", NMETA + 128 * t) for t in range(32)], w=[kv])

            def s_part(n, t):
                g, h = heads[n]
                sl, QT, KT, Vh, kq, kk, kv = slot_of(n)
                R = NMETA if t == 0 else 128
                kc0 = 0 if t == 0 else NMETA + 128 * (t - 1)
                tp_ = t % 2
                jj = t - 1 - 8 * g
                masked = (t >= 1 and jj >= 0)
                for m in range(2):
                    sb = bank_f[2 * tp_ + m]
                    Em = E_s[tp_][m]

                    def smm(e, sb=sb, m=m):
                        ins = e.matmul(sb[:R, :], lhsT=KT[m * 64:(m + 1) * 64, kc0:kc0 + R], rhs=QT[m * 64:(m + 1) * 64, :],
                                       start=True, stop=(not masked))
                        if masked:
                            ins = e.matmul(sb[:R, :], lhsT=idb, rhs=mk[:, jj, :], start=False, stop=True)
                        return ins
                    S.pe(smm, r=[kq, kk, "mk", "idb"], w=[f"D_S{tp_}{m}"])
                    S.act(lambda e, sb=sb, Em=Em: e.activation(out=Em[:R, :], in_=sb[:R, :], func=AF.Exp, scale=0.125),
                          r=[f"D_S{tp_}{m}"], w=[f"D_E{tp_}{m}"])

            def av_part(n, t):
                g, h = heads[n]
                ntile = 8 * g + 8
                sl, QT, KT, Vh, kq, kk, kv = slot_of(n)
                R = NMETA if t == 0 else 128
                tp_ = t % 2
                for m in range(2):
                    Em = E_s[tp_][m]

                    def av(e, Em=Em, m=m):
                        e.matmul(bank_f[4 + m][:, :], lhsT=Vh[:R, t, :], rhs=Em[:R, :], start=(t == 0), stop=(t == ntile))
                        return e.matmul(bank_f[6 + m][:, :], lhsT=ones_b[:R, :], rhs=Em[:R, :], start=(t == 0), stop=(t == ntile))
                    S.pe(av, r=[f"D_E{tp_}{m}", kv, kv + "m", "ones_b"], w=[f"D_O{m}"])

            def fin_a(n):
                for m in range(2):
                    S.dve(lambda e, m=m: e.reciprocal(out=rz[m], in_=bank_f[6 + m][:, :]), r=[f"D_O{m}"], w=[f"D_rz{m}"])
                    S.dve(lambda e, m=m: e.tensor_tensor(out=tt_[m], in0=bank_f[4 + m][:, :], in1=rz[m], op=ALU.mult),
                          r=[f"D_O{m}", f"D_rz{m}"], w=[f"D_t{m}"])

            def fin_b(n):
                g, h = heads[n]
                sl = n % 2
                S.dve(lambda e: e.scalar_tensor_tensor(out=ocomb, in0=tt_[1], scalar=nlam[:, 0:1], in1=tt_[0], op0=ALU.mult, op1=ALU.add),
                      r=["D_t0", "D_t1", "nlam"], w=["D_oc"])
                S.pool(lambda e: e.tensor_tensor(out=sq, in0=ocomb, in1=ocomb, op=ALU.mult), r=["D_oc"], w=["D_sq"])
                S.pe(lambda e: e.matmul(bank_f[2][:, :], lhsT=ones_f, rhs=sq, start=True, stop=True), r=["D_sq", "ones_f"], w=["D_S10"])
                S.act(lambda e: e.activation(out=rst, in_=bank_f[2][:, :], func=AF.Sqrt, scale=1.0 / 128.0, bias=EPS), r=["D_S10"], w=["D_rst"])
                S.dve(lambda e: e.reciprocal(out=rst, in_=rst), r=["D_rst"], w=["D_rst"])
                S.dve(lambda e: e.tensor_tensor(out=ocomb, in0=ocomb, in1=rst, op=ALU.mult), r=["D_oc", "D_rst"], w=["D_oc"])
                on = on_s[sl]
                S.pool(lambda e: e.tensor_scalar(out=on, in0=ocomb, scalar1=sgs[:, 0:1], scalar2=1.0, op0=ALU.mult, op1=ALU.mult),
                       r=["D_oc", "sgs"], w=[f"D_on{sl}"])
                S.dma("sp", lambda e: e.dma_start(out=OnT_d[h, :, g * 512:(g + 1) * 512], in_=on), r=[f"D_on{sl}"], w=[("OnT_d", h, g)])

            issue_loads(0)
            for n in range(len(heads)):
                if n + 1 < len(heads):
                    issue_loads(n + 1)
                nt = 8 * heads[n][0] + 8
                s_part(n, 0)
                for t in range(nt + 1):
                    if t + 1 <= nt:
                        s_part(n, t + 1)
                    av_part(n, t)
                    if t == 2 and n >= 1:
                        fin_b(n - 1)
                fin_a(n)
            fin_b(len(heads) - 1)

        if STAGE >= 5:
            A.reset()
            hT_a = A.bf16(16 * 2048).rearrange("p (k n) -> p k n", n=2048)
            on_a = A.bf16(8 * 2048).rearrange("p (k n) -> p k n", n=2048)
            y_a = A.bf16(8 * 2048).rearrange("p (k n) -> p k n", n=2048)
            Wg_s = [A.bf16(16 * 512).rearrange("p (k n) -> p k n", n=512) for _ in range(2)]
            Wp_s = [A.bf16(8 * 512).rearrange("p (k n) -> p k n", n=512) for _ in range(2)]
            mT_s = [A.bf16(2 * 512).rearrange("p (k n) -> p k n", n=512) for _ in range(2)]
            sg_t = [A.f32(512) for _ in range(2)]
            t12 = [A.f32(512) for _ in range(2)]
            S.barrier()
            for g in range(4):
                S.dma("sp", lambda e, g=g: e.dma_start(out=hT_a[:, :, g * 512:(g + 1) * 512], in_=hTo_d[:, :, g * 512:(g + 1) * 512].rearrange("k p n -> p k n")),
                      r=[("hTo_d", g * 512)], w=[f"E_hT{g}"])
                S.dma("sp", lambda e, g=g: e.dma_start(out=on_a[:, :, g * 512:(g + 1) * 512], in_=OnT_d[:, :, g * 512:(g + 1) * 512].rearrange("k p n -> p k n")),
                      r=[("OnT_d", h, g) for h in range(NH)], w=[f"E_on{g}"])
                S.dma("sp", lambda e, g=g: e.dma_start(out=y_a[:, :, g * 512:(g + 1) * 512], in_=yT_d[:, :, g * 512:(g + 1) * 512].rearrange("k p n -> p k n")),
                      r=[("yT_d", g)], w=[f"E_y{g}"])
            octr = 0
            for c8 in range(8):
                ws = c8 % 2
                Wg, Wp = Wg_s[ws], Wp_s[ws]
                S.dma("pool", lambda e, Wg=Wg, c8=c8: e.dma_start(out=Wg[:, :, 0:256], in_=w_in[:, 6144 + c8 * 256: 6144 + (c8 + 1) * 256].rearrange("(k p) n -> p k n", p=128)), w=[f"E_Wga{ws}"])
                S.dma("pool", lambda e, Wg=Wg, c8=c8: e.dma_start(out=Wg[:, :, 256:512], in_=w_in[:, 8192 + c8 * 256: 8192 + (c8 + 1) * 256].rearrange("(k p) n -> p k n", p=128)), w=[f"E_Wgc{ws}"])
                S.dma("pool", lambda e, Wp=Wp, c8=c8: e.dma_start(out=Wp[:, :, 0:256], in_=w_pa[:, c8 * 256:(c8 + 1) * 256].rearrange("(k p) n -> p k n", p=128)), w=[f"E_Wpa{ws}"])
                S.dma("pool", lambda e, Wp=Wp, c8=c8: e.dma_start(out=Wp[:, :, 256:512], in_=w_pc[:, c8 * 256:(c8 + 1) * 256].rearrange("(k p) n -> p k n", p=128)), w=[f"E_Wpc{ws}"])
                for g in range(4):
                    mT = mT_s[octr % 2]
                    mk_ = f"E_mT{octr % 2}"
                    octr += 1
                    for k2 in range(2):
                        b0 = 4 * (k2 % 2)
                        bk = f"E_bk{k2 % 2}"

                        def mmg(e, Wg=Wg, Wp=Wp, g=g, k2=k2, b0=b0):
                            for j in range(2):
                                for k in range(16):
                                    e.matmul(bank_f[b0 + j][:, :], lhsT=Wg[:, k, j * 256 + k2 * 128: j * 256 + (k2 + 1) * 128],
                                             rhs=hT_a[:, k, g * 512:(g + 1) * 512], start=(k == 0), stop=(k == 15))
                            for k in range(8):
                                e.matmul(bank_f[b0 + 2][:, :], lhsT=Wp[:, k, k2 * 128:(k2 + 1) * 128], rhs=on_a[:, k, g * 512:(g + 1) * 512],
                                         start=(k == 0), stop=(k == 7))
                            for k in range(8):
                                ins = e.matmul(bank_f[b0 + 3][:, :], lhsT=Wp[:, k, 256 + k2 * 128: 256 + (k2 + 1) * 128],
                                               rhs=y_a[:, k, g * 512:(g + 1) * 512], start=(k == 0), stop=(k == 7))
                            return ins
                        S.pe(mmg, r=[f"E_Wga{ws}", f"E_Wgc{ws}", f"E_Wpa{ws}", f"E_Wpc{ws}", f"E_hT{g}", f"E_on{g}", f"E_y{g}"], w=[bk])
                        for j in range(2):
                            S.act(lambda e, j=j, b0=b0: e.activation(out=sg_t[j], in_=bank_f[b0 + j][:, :], func=AF.Sigmoid), r=[bk], w=[f"E_sg{j}"])
                            S.dve(lambda e, j=j, b0=b0: e.tensor_tensor(out=t12[j], in0=bank_f[b0 + 2 + j][:, :], in1=sg_t[j], op=ALU.mult),
                                  r=[bk, f"E_sg{j}"], w=[f"E_t{j}"])
                        S.pool(lambda e, mT=mT, k2=k2: e.tensor_tensor(out=mT[:, k2, :], in0=t12[0], in1=t12[1], op=ALU.add), r=["E_t0", "E_t1"], w=[mk_])
                    S.dma("sp", lambda e, mT=mT, g=g, c8=c8: e.dma_start(
                        out=mT_d[2 * c8:2 * c8 + 2, :, g * 512:(g + 1) * 512].rearrange("k p n -> p k n"), in_=mT),
                        r=[mk_], w=[("mT_d", g, c8)])

        if STAGE >= 6:
            A.reset()
            Wo = A.bf16(16 * 2048).rearrange("p (k n) -> p k n", n=2048)
            mt_s = [A.bf16(16 * 128).rearrange("p (k n) -> p k n", n=128) for _ in range(2)]
            xr_s = [A.f32(D) for _ in range(2)]
            r_s = [A.f32(D) for _ in range(2)]
            G0, B0, G1, B1 = A.f32(D), A.f32(D), A.f32(D), A.f32(D)
            h1Tf = A.f32(16 * 128).rearrange("p (k n) -> p k n", n=128)
            h1Tb_s = [A.bf16(16 * 512).rearrange("p (k n) -> p k n", n=512) for _ in range(2)]
            wr = A.f32(16 * 64).rearrange("p (k n) -> p k n", n=64)
            rbt = A.f32(64)
            st = A.f32(24).rearrange("p (a b) -> p a b", b=6)
            mv = A.f32(2)
            rstd = A.f32(1)
            sgm, sel, eq, sel2, selm, msk, smm = [A.f32(64) for _ in range(7)]
            m1, m2, gs, top8, gm, pen, top8e = [A.f32(8) for _ in range(7)]
            ssum = A.f32(1)
            S.barrier()
            S.dma("pool", lambda e: e.dma_start(out=Wo, in_=w_out[:, :].rearrange("(k p) n -> p k n", p=128)), w=["F_Wo"])
            for i, t_ in enumerate((G0, B0, G1, B1)):
                S.dma("sp", lambda e, i=i, t_=t_: e.dma_start(out=t_, in_=bcd[i, :, :]), w=[f"F_bc{i}"])
            S.dma("sp", lambda e: e.dma_start(out=wr, in_=w_router[:, :].rearrange("(k p) n -> p k n", p=128)), w=["F_wr"])
            S.dma("sp", lambda e: e.dma_start(out=rbt, in_=rbd[:, :]), w=["F_rb"])
            S.pool(lambda e: e.memset(ewt[:, :], 1.0), w=["ew"])

            def ln_stats(src, keyin, pf):
                def stats(e):
                    for i in range(4):
                        ins = e.bn_stats(out=st[:, i, :], in_=src[:, i * 512:(i + 1) * 512])
                    return ins
                S.dve(stats, r=[keyin], w=[pf + "st"])
                S.dve(lambda e: e.bn_aggr(out=mv, in_=st), r=[pf + "st"], w=[pf + "mv"])
                S.act(lambda e: e.activation(out=rstd, in_=mv[:, 1:2], func=AF.Sqrt, bias=EPS, scale=1.0), r=[pf + "mv"], w=[pf + "rstd"])
                S.dve(lambda e: e.reciprocal(out=rstd, in_=rstd), r=[pf + "rstd"], w=[pf + "rstd"])
                S.dve(lambda e: e.tensor_scalar(out=src, in0=src, scalar1=mv[:, 0:1], scalar2=rstd[:, 0:1], op0=ALU.subtract, op1=ALU.mult),
                      r=[keyin], s=[pf + "rstd", pf + "mv"], w=[keyin])

            for tt in range(16):
                sl = tt % 2
                mt, xr, rr, h1Tb = mt_s[sl], xr_s[sl], r_s[sl], h1Tb_s[(tt // 4) % 2]
                g4, t4 = tt // 4, tt % 4
                S.dma("sp", lambda e, mt=mt, tt=tt: e.dma_start(out=mt, in_=mT_d[:, :, tt * 128:(tt + 1) * 128].rearrange("k p n -> p k n")),
                      r=[("mT_d", tt // 4, c8) for c8 in range(8)], w=[f"F_mt{sl}"])
                S.dma("sp", lambda e, xr=xr, tt=tt: e.dma_start(out=xr, in_=xo[tt * 128:(tt + 1) * 128, :]), w=[f"F_xr{sl}"])

                def mmo(e, mt=mt):
                    for cc in range(4):
                        for k in range(16):
                            ins = e.matmul(bank_f[cc][:, :], lhsT=mt[:, k, :], rhs=Wo[:, k, cc * 512:(cc + 1) * 512], start=(k == 0), stop=(k == 15))
                    return ins
                S.pe(mmo, r=[f"F_mt{sl}", "F_Wo"], w=["F_po"])
                ln_stats(xr, f"F_xr{sl}", "F_a")
                S.dve(lambda e, xr=xr: e.tensor_tensor(out=xr, in0=xr, in1=G0, op=ALU.mult), r=[f"F_xr{sl}", "F_bc0"], w=[f"F_xr{sl}"])
                S.pool(lambda e, xr=xr: e.tensor_tensor(out=xr, in0=xr, in1=B0, op=ALU.add), r=[f"F_xr{sl}", "F_bc1"], w=[f"F_xr{sl}"])

                def resid(e, xr=xr, rr=rr):
                    for cc in range(4):
                        ins = e.scalar_tensor_tensor(out=rr[:, cc * 512:(cc + 1) * 512], in0=xr[:, cc * 512:(cc + 1) * 512], scalar=ALPHA,
                                                     in1=bank_f[cc][:, :], op0=ALU.mult, op1=ALU.add)
                    return ins
                S.dve(resid, r=[f"F_xr{sl}", "F_po"], w=[f"F_r{sl}"])
                ln_stats(rr, f"F_r{sl}", "F_b")
                S.dve(lambda e, rr=rr: e.tensor_tensor(out=rr, in0=rr, in1=G1, op=ALU.mult), r=[f"F_r{sl}", "F_bc2"], w=[f"F_r{sl}"])
                S.pool(lambda e, rr=rr: e.tensor_tensor(out=rr, in0=rr, in1=B1, op=ALU.add), r=[f"F_r{sl}", "F_bc3"], w=[f"F_r{sl}"])
                S.dma("sp", lambda e, rr=rr, tt=tt: e.dma_start(out=h1_d[tt * 128:(tt + 1) * 128, :], in_=rr), r=[f"F_r{sl}"], w=[("h1_d", tt)])

                if SUB6 >= 1:
                    def trf(e, rr=rr):
                        for k in range(16):
                            ins = e.matmul(bank_f[4 + k // 4][:, (k % 4) * 128:(k % 4 + 1) * 128], lhsT=rr[:, k * 128:(k + 1) * 128], rhs=idf, start=True, stop=True)
                        return ins
                    S.pe(trf, r=[f"F_r{sl}", "idf"], w=["F_pt"])

                    def evb(e, h1Tb=h1Tb, t4=t4):
                        for q in range(4):
                            ins = e.activation(out=h1Tb[:, 4 * q:4 * q + 4, t4 * 128:(t4 + 1) * 128], in_=V3(bank_f[4 + q][:, :], 4), func=AF.Copy)
                        return ins
                    if SUB6B & 1:
                        S.act(evb, r=["F_pt"], w=[f"F_h1Tb{g4 % 2}"])

                    def evf(e):
                        for q in range(4):
                            ins = e.tensor_copy(out=h1Tf[:, 4 * q:4 * q + 4, :], in_=V3(bank_f[4 + q][:, :], 4))
                        return ins
                    if SUB6B & 2:
                        S.dve(evf, r=["F_pt", f"F_h1Tb{g4 % 2}"], w=["F_h1Tf"])
                    if (SUB6B & 4) and t4 == 3:
                        S.dma("sp", lambda e, h1Tb=h1Tb, g4=g4: e.dma_start(out=h1T_d[:, :, g4 * 512:(g4 + 1) * 512].rearrange("k p n -> p k n"), in_=h1Tb),
                              r=[f"F_h1Tb{g4 % 2}"], w=[("h1T_d", g4)])

                if SUB6 >= 2:
                    def mmr(e):
                        for k in range(16):
                            ins = e.matmul(bank_f[0][:, 0:64], lhsT=h1Tf[:, k, :], rhs=wr[:, k, :], start=(k == 0), stop=(k == 15))
                        return ins
                    S.pe(mmr, r=["F_h1Tf", "F_wr"], w=["F_po"])
                if SUB6 >= 3:
                    S.act(lambda e: e.activation(out=sgm, in_=bank_f[0][:, 0:64], func=AF.Sigmoid), r=["F_po"], w=["R_sgm"])
                    S.dve(lambda e: e.tensor_tensor(out=sel, in0=sgm, in1=rbt, op=ALU.add), r=["R_sgm", "F_rb"], w=["R_sel"])
                    S.dve(lambda e: e.tensor_reduce(out=m1, in_=V3(sel, 8), axis=AX.X, op=ALU.max), r=["R_sel"], w=["R_m1"])
                    S.dve(lambda e: e.tensor_tensor(out=V3(eq, 8), in0=V3(sel, 8), in1=m1.unsqueeze(2).to_broadcast([128, 8, 8]), op=ALU.is_equal),
                          r=["R_sel", "R_m1"], w=["R_eq"])
                    S.dve(lambda e: e.scalar_tensor_tensor(out=sel2, in0=eq, scalar=-1e30, in1=sel, op0=ALU.mult, op1=ALU.add),
                          r=["R_eq", "R_sel"], w=["R_sel2"])
                    S.dve(lambda e: e.tensor_reduce(out=m2, in_=V3(sel2, 8), axis=AX.X, op=ALU.max), r=["R_sel2"], w=["R_m2"])
                    S.dve(lambda e: e.tensor_tensor(out=gs, in0=m1, in1=m2, op=ALU.add), r=["R_m1", "R_m2"], w=["R_gs"])
                    S.dve(lambda e: e.max(out=top8, in_=gs), r=["R_gs"], w=["R_top8"])
                    S.dve(lambda e: e.tensor_tensor(out=gm, in0=gs, in1=top8[:, 3:4].to_broadcast([128, 8]), op=ALU.is_ge), r=["R_gs", "R_top8"], w=["R_gm"])
                    S.dve(lambda e: e.tensor_scalar(out=pen, in0=gm, scalar1=-1.0, scalar2=1e30, op0=ALU.add, op1=ALU.mult), r=["R_gm"], w=["R_pen"])
                    S.dve(lambda e: e.tensor_tensor(out=V3(selm, 8), in0=V3(sel, 8), in1=pen.unsqueeze(2).to_broadcast([128, 8, 8]), op=ALU.add),
                          r=["R_sel", "R_pen"], w=["R_selm"])
                    S.dve(lambda e: e.max(out=top8e, in_=selm), r=["R_selm"], w=["R_top8e"])
                    S.dve(lambda e: e.tensor_tensor(out=msk, in0=selm, in1=top8e[:, 7:8].to_broadcast([128, 64]), op=ALU.is_ge),
                          r=["R_selm", "R_top8e"], w=["R_msk"])
                    S.dve(lambda e: e.tensor_tensor(out=smm, in0=sgm, in1=msk, op=ALU.mult), r=["R_sgm", "R_msk"], w=["R_smm"])
                    S.dve(lambda e: e.tensor_reduce(out=ssum, in_=smm, axis=AX.X, op=ALU.add), r=["R_smm"], w=["R_ssum"])
                    S.dve(lambda e: e.reciprocal(out=ssum, in_=ssum), r=["R_ssum"], w=["R_ssum"])
                    S.dve(lambda e, tt=tt: e.scalar_tensor_tensor(out=ewt[:, tt * 65: tt * 65 + 64], in0=smm, scalar=2.5,
                                                                  in1=ssum[:, 0:1].to_broadcast([128, 64]), op0=ALU.mult, op1=ALU.mult),
                          r=["R_smm", "R_ssum"], w=["ew"])

            if DEBUG:
                S.dma("sp", lambda e: e.dma_start(out=ew_d[:, :], in_=ewt), r=["ew"], w=["ew_d"])

        if STAGE >= 7:
            experts = [(w_eg[e_], w_eu[e_], w_ed[e_]) for e_ in range(NE)] + [(w_sg, w_su, w_sd)]
            def moe_half(hf):
                A.reset()
                acc = A.f32(8 * D).rearrange("p (t n) -> p t n", n=D)
                h1T = A.bf16(16 * 1024).rearrange("p (k n) -> p k n", n=1024)
                Wgu_s = [A.bf16(16 * 1024).rearrange("p (k n) -> p k n", n=1024) for _ in range(2)]
                Wd_s = [A.bf16(4 * 2048).rearrange("p (k n) -> p k n", n=2048) for _ in range(2)]
                hid = A.bf16(4 * 1024).rearrange("p (k n) -> p k n", n=1024)
                sgt_one = A.f32(512)
                sgt = [sgt_one, sgt_one]
                pf = f"M{hf}"

                def acck(t):
                    return [(pf + "acc", t, c_) for c_ in range(4)]
                S.barrier()
                for t in range(8):
                    S.dma("sp", lambda e, t=t: e.dma_start(out=acc[:, t, :], in_=h1_d[(8 * hf + t) * 128:(8 * hf + t + 1) * 128, :]),
                          r=[("h1_d", 8 * hf + t)], w=acck(t))
                    S.pool(lambda e, t=t: e.tensor_scalar(out=acc[:, t, :], in0=acc[:, t, :], scalar1=ALPHA, scalar2=1.0, op0=ALU.mult, op1=ALU.mult),
                           r=acck(t), w=acck(t))
                S.dma("sp", lambda e: e.dma_start(out=h1T, in_=h1T_d[:, :, hf * 1024:(hf + 1) * 1024].rearrange("k p n -> p k n")),
                      r=[("h1T_d", 2 * hf), ("h1T_d", 2 * hf + 1)], w=[pf + "h1T"])
                for ei, (wg_, wu_, wd_) in enumerate(experts[:NEXP]):
                    ws = ei % 2
                    Wgu, Wd = Wgu_s[ws], Wd_s[ws]
                    S.dma("pool", lambda e, Wgu=Wgu, wg_=wg_: e.dma_start(out=Wgu[:, :, 0:512], in_=wg_.rearrange("(k p) n -> p k n", p=128)), w=[f"{pf}Wg{ws}"])
                    S.dma("pool", lambda e, Wgu=Wgu, wu_=wu_: e.dma_start(out=Wgu[:, :, 512:1024], in_=wu_.rearrange("(k p) n -> p k n", p=128)), w=[f"{pf}Wu{ws}"])
                    S.dma("pool", lambda e, Wd=Wd, wd_=wd_: e.dma_start(out=Wd, in_=wd_.rearrange("(k p) n -> p k n", p=128)), w=[f"{pf}Wd{ws}"])
                    for m in range(4):
                        for n in range(2):
                            b0 = 2 * ((2 * m + n) % 2)
                            bk = f"{pf}gu{b0}"

                            def mmgu(e, Wgu=Wgu, m=m, n=n, b0=b0):
                                for j in range(2):
                                    for k in range(16):
                                        ins = e.matmul(bank_f[b0 + j][:, :], lhsT=Wgu[:, k, j * 512 + m * 128: j * 512 + (m + 1) * 128],
                                                       rhs=h1T[:, k, n * 512:(n + 1) * 512], start=(k == 0), stop=(k == 15))
                                return ins
                            S.pe(mmgu, r=[f"{pf}Wg{ws}", f"{pf}Wu{ws}", pf + "h1T"], w=[bk])
                            sg_ = sgt[(2 * m + n) % 2]
                            S.act(lambda e, sg_=sg_, b0=b0: e.activation(out=sg_, in_=bank_f[b0][:, :], func=AF.Silu), r=[bk], w=[f"{pf}sg"])
                            S.dve(lambda e, sg_=sg_, b0=b0, m=m, n=n: e.tensor_tensor(out=hid[:, m, n * 512:(n + 1) * 512], in0=bank_f[b0 + 1][:, :],
                                                                                      in1=sg_, op=ALU.mult), r=[bk, f"{pf}sg"], w=[(pf + "hid", m, n)])
                    for t in range(8):
                        for cc in range(4):
                            yb = 4 + cc
                            S.pe(lambda e, t=t, cc=cc, yb=yb, Wd=Wd: [e.matmul(bank_f[yb][:, :], lhsT=hid[:, m, t * 128:(t + 1) * 128],
                                                                              rhs=Wd[:, m, cc * 512:(cc + 1) * 512], start=(m == 0), stop=(m == 3))
                                                                     for m in range(4)][-1],
                                 r=[(pf + "hid", m, t // 4) for m in range(4)] + [f"{pf}Wd{ws}"], w=[f"{pf}y{cc}"])
                            S.dve(lambda e, t=t, cc=cc, yb=yb, ei=ei: e.scalar_tensor_tensor(
                                out=acc[:, t, cc * 512:(cc + 1) * 512], in0=bank_f[yb][:, :],
                                scalar=ewt[:, (8 * hf + t) * 65 + ei:(8 * hf + t) * 65 + ei + 1], in1=acc[:, t, cc * 512:(cc + 1) * 512],
                                op0=ALU.mult, op1=ALU.add), r=[f"{pf}y{cc}", (pf + "acc", t, cc), "ew"], w=[(pf + "acc", t, cc)])
                S.barrier()
                A.off = 8 * D
                G2, B2 = A.f32(D), A.f32(D)
                st = A.f32(24).rearrange("p (a b) -> p a b", b=6)
                mv = A.f32(2)
                rstd = A.f32(1)
                S.dma("sp", lambda e: e.dma_start(out=G2, in_=bcd[4, :, :]), w=[pf + "G2"])
                S.dma("sp", lambda e: e.dma_start(out=B2, in_=bcd[5, :, :]), w=[pf + "B2"])
                for t in range(8):
                    src = acc[:, t, :]
                    kin = acck(t)

                    def stats(e, src=src):
                        for i in range(4):
                            ins = e.bn_stats(out=st[:, i, :], in_=src[:, i * 512:(i + 1) * 512])
                        return ins
                    S.dve(stats, r=kin, w=[pf + "st"])
                    S.dve(lambda e: e.bn_aggr(out=mv, in_=st), r=[pf + "st"], w=[pf + "mv"])
                    S.act(lambda e: e.activation(out=rstd, in_=mv[:, 1:2], func=AF.Sqrt, bias=EPS, scale=1.0), r=[pf + "mv"], w=[pf + "rstd"])
                    S.dve(lambda e: e.reciprocal(out=rstd, in_=rstd), r=[pf + "rstd"], w=[pf + "rstd"])
                    S.dve(lambda e, src=src: e.tensor_scalar(out=src, in0=src, scalar1=mv[:, 0:1], scalar2=rstd[:, 0:1], op0=ALU.subtract, op1=ALU.mult),
                          r=kin, s=[pf + "rstd", pf + "mv"], w=kin)
                    S.dve(lambda e, src=src: e.tensor_tensor(out=src, in0=src, in1=G2, op=ALU.mult), r=kin + [pf + "G2"], w=kin)
                    S.pool(lambda e, src=src: e.tensor_tensor(out=src, in0=src, in1=B2, op=ALU.add), r=kin + [pf + "B2"], w=kin)
                    S.dma("sp", lambda e, src=src, t=t: e.dma_start(out=out[(8 * hf + t) * 128:(8 * hf + t + 1) * 128, :], in_=src),
                          r=kin, w=[("out", hf, t)])

            if STAGE >= 7:
                for hf_ in range(2):
                    moe_half(hf_)

        S.barrier()
        S.emit(nc, sems)
    return nc


def _rope_tables():
    half = 32
    inv = (10000.0 ** (-np.arange(half, dtype=np.float32) / half)).astype(np.float32)
    pos = np.arange(L, dtype=np.float32)
    ang = pos[None, :] * inv[:, None]
    cos = np.cos(ang).astype(np.float32)
    sin = np.sin(ang).astype(np.float32)
    cos64 = np.concatenate([cos, cos], 0)
    sin64 = np.concatenate([-sin, sin], 0)
    return np.concatenate([cos64, cos64], 0), np.concatenate([sin64, sin64], 0)


def _perm_cols():
    idx = np.arange(1024).reshape(NH, 2, 2, 32)[:, :, ::-1, :].reshape(1024)
    return idx


_NC_CACHE = {}
_DECL = []


def kernel(**inputs):
    f = lambda k: np.asarray(inputs[k], np.float32)
    x = f("x")
    w_in = np.ascontiguousarray(f("w_in")[0])
    pidx = _perm_cols()
    w_qkp = np.ascontiguousarray(np.concatenate([w_in[:, 0:1024][:, pidx], w_in[:, 1024:2048][:, pidx]], axis=1))
    cosk, sink = _rope_tables()
    par = np.zeros((128, 64), np.float32)
    par[:, 0:16] = f("ln0_g").reshape(16, 128).T
    par[:, 16:32] = f("ln0_b").reshape(16, 128).T
    par[:, 32] = f("subln_g")[0]
    wc = f("w_conv")[0]
    for k in range(3):
        par[:, 33 + 8 * k: 41 + 8 * k] = wc[k].reshape(8, 128).T
    ident = np.eye(128, dtype=np.float32)
    meta = np.ascontiguousarray(f("meta_tokens"))
    lamv = np.ascontiguousarray(np.broadcast_to(np.concatenate(
        [f("lambda_q1")[0], f("lambda_q2")[0], f("lambda_k1")[0], f("lambda_k2")[0]])[None, :], (128, 256)))
    rbd = np.ascontiguousarray(np.broadcast_to(f("router_bias")[0][None, :], (128, NE)))
    bcd = np.ascontiguousarray(np.stack([np.broadcast_to(v[None, :], (128, D)) for v in
                                         (f("ln0_g"), f("ln0_b"), f("ln1_g")[0], f("ln1_b")[0], f("ln2_g")[0], f("ln2_b")[0])]))
    shared = dict(meta=meta, w_in=w_in, w_qkp=w_qkp, par=par, ident=ident, cosk=cosk, sink=sink, lamv=lamv, rbd=rbd, bcd=bcd,
                  w_pa=np.ascontiguousarray(f("w_proj_attn")[0]), w_pc=np.ascontiguousarray(f("w_proj_conv")[0]),
                  w_out=np.ascontiguousarray(f("w_out")[0]), w_router=np.ascontiguousarray(f("w_router")[0]),
                  w_eg=np.ascontiguousarray(f("w_exp_gate")[0]), w_eu=np.ascontiguousarray(f("w_exp_up")[0]),
                  w_ed=np.ascontiguousarray(f("w_exp_down")[0]), w_sg=np.ascontiguousarray(f("w_sh_gate")[0]),
                  w_su=np.ascontiguousarray(f("w_sh_up")[0]), w_sd=np.ascontiguousarray(f("w_sh_down")[0]))
    if "nc" not in _NC_CACHE:
        _NC_CACHE["nc"] = build_program()
    nc = _NC_CACHE["nc"]
    in_maps = []
    blocks_of = []
    for c in range(8):
        b, p = c // 2, c % 2
        blks = [8 * g + OFFS[p][i] for g in range(4) for i in range(4)]
        blocks_of.append(blks)
        xo = np.ascontiguousarray(np.concatenate([x[b, 128 * j:128 * (j + 1)] for j in blks], 0))
        seqx = x[b]
        xh = np.ascontiguousarray(np.concatenate([(meta[14:16] if j == 0 else seqx[128 * j - 2:128 * j]) for j in blks], 0))
        pos = np.concatenate([NMETA + 128 * j + np.arange(128) for j in blks])
        mask = np.zeros((128, 8, 4, 128), np.float32)
        r = np.arange(128)
        diag = ((r[:, None] // 64) <= (r[None, :] // 64)).astype(np.float32)
        for o in range(8):
            for i in range(4):
                oo = OFFS[p][i]
                if o < oo:
                    mask[:, o, i, :] = 1.0
                elif o == oo:
                    mask[:, o, i, :] = diag
        m = dict(shared)
        m.update(xs=np.ascontiguousarray(x[b]), xo=xo, xh=xh, cosq=np.ascontiguousarray(cosk[:, pos]),
                 sinq=np.ascontiguousarray(sink[:, pos]), maskd=((mask.reshape(128, 8, 512) - 1.0) * 10000.0).astype(ml_dtypes.bfloat16))
        in_maps.append({k: v for k, v in m.items() if k in _DECL})
    res = run_bass_kernel_spmd(nc, in_maps, core_ids=list(range(8)))
    kernel.last = res
    outp = np.zeros((4, SEQ, D), np.float32)
    for c in range(8):
        b = c // 2
        o = np.asarray(res.results[c]["out"], np.float32)
        for i, j in enumerate(blocks_of[c]):
            outp[b, 128 * j:128 * (j + 1)] = o[128 * i:128 * (i + 1)]
    return outp
```
